# Optimizing a Trainium2 kernel written in Bass

```python
import math
import jax, jax.numpy as jnp
from jax import lax
import numpy as np

D_MODEL = 1024
BATCH = 2
SEQ = 8192
DEPTH = 2

HEAD_DIM = 64
ROPE_DIM = HEAD_DIM // 4
ROPE_THETA = 500000.0
Q_BLOCK = 128
NSA_HEADS = (D_MODEL // 2) // HEAD_DIM
NSA_KV_GROUPS = 2
CMP_BLOCK = 32
CMP_STRIDE = 16
CMP_HIDDEN = 4 * HEAD_DIM
SLC_BLOCK = 64
N_SELECT = 16
WINDOW = 512
SB_HEADS = (D_MODEL // 2) // HEAD_DIM
DIFF_HEADS = D_MODEL // (2 * HEAD_DIM)
D_FF = ((8 * D_MODEL // 3 + 255) // 256) * 256
CONV_WIDTH = 3
N_EVEN = (DEPTH + 1) // 2
N_ODD = DEPTH // 2
NSA_Q_W = NSA_HEADS * HEAD_DIM
NSA_KV_W = NSA_KV_GROUPS * HEAD_DIM
SB_W = SB_HEADS * HEAD_DIM
HYB_IN_SIZES = (NSA_Q_W,) + (NSA_KV_W,) * 6 + (3 * NSA_HEADS,) + (SB_W,) * 3
HYB_IN_W = sum(HYB_IN_SIZES)
HYB_OUT_W = NSA_Q_W + SB_W
DIFF_W = 2 * DIFF_HEADS * HEAD_DIM
RMS_EPS = 1e-6
NEG_INF = -1e30
FORCE = 1e9

kernel_name = 'hybrid_nsa_stickbreak_diffattn_convffn'


def rms_norm(x, g):
    xf = x.astype(jnp.float32)
    y = xf * lax.rsqrt(jnp.mean(xf * xf, axis=-1, keepdims=True) + RMS_EPS) * g.astype(jnp.float32)
    return y.astype(x.dtype)


def modulate(h, shift, scale):
    return h * (1.0 + scale[:, None, :]) + shift[:, None, :]


def rope_partial(x, pos):
    inv = ROPE_THETA ** (-jnp.arange(0, ROPE_DIM, 2, dtype=jnp.float32) / ROPE_DIM)
    ang = pos.astype(jnp.float32)[..., None] * inv
    cos = jnp.cos(ang)[:, :, None, :]
    sin = jnp.sin(ang)[:, :, None, :]
    half = ROPE_DIM // 2
    xf = x.astype(jnp.float32)
    x1, x2 = xf[..., :half], xf[..., half:ROPE_DIM]
    out = jnp.concatenate([x1 * cos - x2 * sin, x2 * cos + x1 * sin, xf[..., ROPE_DIM:]], axis=-1)
    return out.astype(x.dtype)


def masked_softmax(s, mask):
    p = jax.nn.softmax(jnp.where(mask, s, NEG_INF), axis=-1)
    return jnp.where(mask, p, 0.0)


def split_cols(a, sizes):
    out, start = [], 0
    for s in sizes:
        out.append(a[..., start:start + s])
        start += s
    return out


def compress(kv, pos_emb, w1, w2):
    B, T, G, dk = kv.shape
    nc = (T - CMP_BLOCK) // CMP_STRIDE + 1
    idx = (jnp.arange(nc) * CMP_STRIDE)[:, None] + jnp.arange(CMP_BLOCK)[None, :]
    blk = kv[:, idx] + pos_emb[None, None, :, None, :]
    blk = jnp.moveaxis(blk, 3, 2).reshape(B, nc, G, CMP_BLOCK * dk)
    return jax.nn.silu(blk @ w1) @ w2


def nsa_attention(q, k_c, v_c, k_s, v_s, k_w, v_w, gates):
    B, T, H, dk = q.shape
    G = NSA_KV_GROUPS
    R = H // G
    nc = k_c.shape[1]
    ns = T // SLC_BLOCK
    n_sel = min(N_SELECT, ns)
    scale = dk ** -0.5
    qg = q.reshape(B, T, G, R, dk)
    gg = gates.reshape(B, T, G, R, 3)
    cmp_start = jnp.arange(nc) * CMP_STRIDE
    cmp_end = cmp_start + CMP_BLOCK - 1
    slc_start = jnp.arange(ns) * SLC_BLOCK
    overlap = ((cmp_start[:, None] < slc_start[None, :] + SLC_BLOCK)
               & (cmp_start[:, None] + CMP_BLOCK > slc_start[None, :])).astype(jnp.float32)
    ks_blocks = k_s.reshape(B, ns, SLC_BLOCK, G, dk).transpose(0, 3, 1, 2, 4)
    vs_blocks = v_s.reshape(B, ns, SLC_BLOCK, G, dk).transpose(0, 3, 1, 2, 4)
    kw_pad = jnp.pad(k_w, ((0, 0), (WINDOW, 0), (0, 0), (0, 0)))
    vw_pad = jnp.pad(v_w, ((0, 0), (WINDOW, 0), (0, 0), (0, 0)))
    b_ix = jnp.arange(B)[:, None, None, None]
    g_ix = jnp.arange(G)[None, :, None, None]
    blk_ids = jnp.arange(ns)

    def block(qb):
        q0 = qb * Q_BLOCK
        t = q0 + jnp.arange(Q_BLOCK)
        qc = lax.dynamic_slice_in_dim(qg, q0, Q_BLOCK, axis=1)
        s_c = jnp.einsum('bqgrd,bcgd->bgrqc', qc, k_c).astype(jnp.float32) * scale
        p_c = masked_softmax(s_c, cmp_end[None, :] <= t[:, None])
        o_c = jnp.einsum('bgrqc,bcgd->bqgrd', p_c.astype(v_c.dtype), v_c)
        imp = jnp.einsum('bgrqc,cn->bgqn', p_c, overlap)
        cur = t // SLC_BLOCK
        forced = (blk_ids[None, :] == 0) | (blk_ids[None, :] == cur[:, None]) | (blk_ids[None, :] == cur[:, None] - 1)
        valid = blk_ids[None, :] <= cur[:, None]
        score = jnp.where(forced, FORCE, jnp.where(valid, imp, -FORCE))
        _, sel = lax.top_k(score, n_sel)
        k_sel = ks_blocks[b_ix, g_ix, sel]
        v_sel = vs_blocks[b_ix, g_ix, sel]
        tok = sel[..., None] * SLC_BLOCK + jnp.arange(SLC_BLOCK)
        m_s = (tok <= t[None, None, :, None, None]).reshape(B, G, 1, Q_BLOCK, n_sel * SLC_BLOCK)
        s_s = jnp.einsum('bqgrd,bgqnkd->bgrqnk', qc, k_sel).astype(jnp.float32) * scale
        p_s = masked_softmax(s_s.reshape(B, G, R, Q_BLOCK, n_sel * SLC_BLOCK), m_s)
        p_s = p_s.reshape(B, G, R, Q_BLOCK, n_sel, SLC_BLOCK)
        o_s = jnp.einsum('bgrqnk,bgqnkd->bqgrd', p_s.astype(v_sel.dtype), v_sel)
        kw = lax.dynamic_slice_in_dim(kw_pad, q0, Q_BLOCK + WINDOW, axis=1)
        vw = lax.dynamic_slice_in_dim(vw_pad, q0, Q_BLOCK + WINDOW, axis=1)
        kp = q0 - WINDOW + jnp.arange(Q_BLOCK + WINDOW)
        m_w = (kp[None, :] <= t[:, None]) & (kp[None, :] > t[:, None] - WINDOW) & (kp[None, :] >= 0)
        s_w = jnp.einsum('bqgrd,bkgd->bgrqk', qc, kw).astype(jnp.float32) * scale
        p_w = masked_softmax(s_w, m_w)
        o_w = jnp.einsum('bgrqk,bkgd->bqgrd', p_w.astype(vw.dtype), vw)
        g = jax.nn.sigmoid(lax.dynamic_slice_in_dim(gg, q0, Q_BLOCK, axis=1).astype(jnp.float32))
        o = g[..., 0:1] * o_c + g[..., 1:2] * o_s + g[..., 2:3] * o_w
        return o.astype(q.dtype).reshape(B, Q_BLOCK, H * dk)

    out = lax.map(block, jnp.arange(T // Q_BLOCK))
    return jnp.moveaxis(out, 0, 1).reshape(B, T, H * dk)


def stick_breaking_attention(q, k, v):
    B, T, H, dk = q.shape
    scale = dk ** -0.5
    key_pos = jnp.arange(T)

    def block(qb):
        q0 = qb * Q_BLOCK
        t = q0 + jnp.arange(Q_BLOCK)
        qc = lax.dynamic_slice_in_dim(q, q0, Q_BLOCK, axis=1)
        z = jnp.einsum('bqhd,bkhd->bhqk', qc, k).astype(jnp.float32) * scale
        m = key_pos[None, :] < t[:, None]
        log_keep = jnp.where(m, jax.nn.log_sigmoid(-z), 0.0)
        log_later = lax.cumsum(log_keep, axis=3, reverse=True) - log_keep
        a = jnp.where(m, jnp.exp(jax.nn.log_sigmoid(z) + log_later), 0.0)
        return jnp.einsum('bhqk,bkhd->bqhd', a.astype(v.dtype), v)

    out = lax.map(block, jnp.arange(T // Q_BLOCK))
    return jnp.moveaxis(out, 0, 1).reshape(B, T, H * dk)


def diff_attention(q1, q2, k1, k2, v, lam):
    B, T, H, dk = q1.shape
    scale = dk ** -0.5
    key_pos = jnp.arange(T)

    def block(qb):
        q0 = qb * Q_BLOCK
        t = q0 + jnp.arange(Q_BLOCK)
        m = key_pos[None, :] <= t[:, None]
        q1c = lax.dynamic_slice_in_dim(q1, q0, Q_BLOCK, axis=1)
        q2c = lax.dynamic_slice_in_dim(q2, q0, Q_BLOCK, axis=1)
        p1 = masked_softmax(jnp.einsum('bqhd,bkhd->bhqk', q1c, k1).astype(jnp.float32) * scale, m)
        p2 = masked_softmax(jnp.einsum('bqhd,bkhd->bhqk', q2c, k2).astype(jnp.float32) * scale, m)
        w = p1 - lam * p2
        return jnp.einsum('bhqk,bkhe->bqhe', w.astype(v.dtype), v)

    out = lax.map(block, jnp.arange(T // Q_BLOCK))
    return jnp.moveaxis(out, 0, 1).reshape(B, T, H, 2 * dk)


def hybrid_nsa_sb_mixer(h, positions, w_in, pos_k, pos_v, ck_w1, ck_w2, cv_w1, cv_w2, w_out):
    B, T, _ = h.shape
    G = NSA_KV_GROUPS
    (q_n, kc, vc, ks, vs, kw, vw, gl, q_s, k_s, v_s) = split_cols(h @ w_in, HYB_IN_SIZES)
    hd = lambda a, n: a.reshape(B, T, n, HEAD_DIM)
    q_n = rope_partial(hd(q_n, NSA_HEADS), positions)
    ks = rope_partial(hd(ks, G), positions)
    kw = rope_partial(hd(kw, G), positions)
    k_cmp = compress(hd(kc, G), pos_k, ck_w1, ck_w2)
    v_cmp = compress(hd(vc, G), pos_v, cv_w1, cv_w2)
    nc = k_cmp.shape[1]
    cmp_end = jnp.arange(nc) * CMP_STRIDE + CMP_BLOCK - 1
    k_cmp = rope_partial(k_cmp, positions[:, cmp_end])
    o_nsa = nsa_attention(q_n, k_cmp, v_cmp, ks, hd(vs, G), kw, hd(vw, G),
                          gl.reshape(B, T, NSA_HEADS, 3))
    o_sb = stick_breaking_attention(hd(q_s, SB_HEADS), hd(k_s, SB_HEADS), hd(v_s, SB_HEADS))
    return jnp.concatenate([o_nsa, o_sb], axis=-1) @ w_out


def diff_mixer(h, positions, w_qkv, lq1, lk1, lq2, lk2, subln, w_out, layer_idx):
    B, T, _ = h.shape
    H = DIFF_HEADS
    q, k, v = jnp.split(h @ w_qkv, 3, axis=-1)
    q = rope_partial(q.reshape(B, T, 2 * H, HEAD_DIM), positions).reshape(B, T, H, 2, HEAD_DIM)
    k = rope_partial(k.reshape(B, T, 2 * H, HEAD_DIM), positions).reshape(B, T, H, 2, HEAD_DIM)
    v = v.reshape(B, T, H, 2 * HEAD_DIM)
    lambda_init = 0.8 - 0.6 * math.exp(-0.3 * layer_idx)
    lam = (jnp.exp(jnp.sum(lq1.astype(jnp.float32) * lk1.astype(jnp.float32)))
           - jnp.exp(jnp.sum(lq2.astype(jnp.float32) * lk2.astype(jnp.float32))) + lambda_init)
    o = diff_attention(q[..., 0, :], q[..., 1, :], k[..., 0, :], k[..., 1, :], v, lam)
    o = rms_norm(o, subln) * (1.0 - lambda_init)
    return o.reshape(B, T, DIFF_W) @ w_out


def conv_ffn(h, w_gate, w_up, conv_w, conv_b, w_down):
    g = h @ w_gate
    g = lax.conv_general_dilated(g, conv_w[:, None, :].astype(g.dtype), window_strides=(1,),
                                 padding=[(CONV_WIDTH - 1, 0)],
                                 dimension_numbers=('NWC', 'WIO', 'NWC'),
                                 feature_group_count=g.shape[-1]) + conv_b
    return (jax.nn.silu(g) * (h @ w_up)) @ w_down


def setup_inputs(seed: int = 0) -> dict:
    key = jax.random.key(seed)
    ks = iter(jax.random.split(key, 32))
    D = D_MODEL

    def nrm(shape, scale):
        return scale * jax.random.normal(next(ks), shape, jnp.float32)

    def gain(shape):
        return 1.0 + 0.05 * jax.random.normal(next(ks), shape, jnp.float32)

    x = nrm((BATCH, SEQ, D), 1.0)
    c = nrm((BATCH, D), 1.0)
    positions = (jax.random.randint(next(ks), (BATCH, 1), 0, 1024, jnp.int32)
                 + jnp.arange(SEQ, dtype=jnp.int32)[None, :])
    return {
        'x': x, 'c': c, 'positions': positions,
        'mod_w': nrm((DEPTH, D, 6 * D), D ** -0.5),
        'mod_b': nrm((DEPTH, 6 * D), 0.02),
        'norm_mix': gain((DEPTH, D)),
        'norm_ffn': gain((DEPTH, D)),
        'ffn_w_gate': nrm((DEPTH, D, D_FF), D ** -0.5),
        'ffn_w_up': nrm((DEPTH, D, D_FF), D ** -0.5),
        'ffn_conv_w': nrm((DEPTH, CONV_WIDTH, D_FF), CONV_WIDTH ** -0.5),
        'ffn_conv_b': nrm((DEPTH, D_FF), 0.02),
        'ffn_w_down': nrm((DEPTH, D_FF, D), D_FF ** -0.5),
        'hyb_w_in': nrm((N_EVEN, D, HYB_IN_W), D ** -0.5),
        'nsa_pos_k': nrm((N_EVEN, CMP_BLOCK, HEAD_DIM), 0.1),
        'nsa_pos_v': nrm((N_EVEN, CMP_BLOCK, HEAD_DIM), 0.1),
        'nsa_ck_w1': nrm((N_EVEN, CMP_BLOCK * HEAD_DIM, CMP_HIDDEN), (CMP_BLOCK * HEAD_DIM) ** -0.5),
        'nsa_ck_w2': nrm((N_EVEN, CMP_HIDDEN, HEAD_DIM), CMP_HIDDEN ** -0.5),
        'nsa_cv_w1': nrm((N_EVEN, CMP_BLOCK * HEAD_DIM, CMP_HIDDEN), (CMP_BLOCK * HEAD_DIM) ** -0.5),
        'nsa_cv_w2': nrm((N_EVEN, CMP_HIDDEN, HEAD_DIM), CMP_HIDDEN ** -0.5),
        'hyb_w_out': nrm((N_EVEN, HYB_OUT_W, D), HYB_OUT_W ** -0.5),
        'diff_w_qkv': nrm((N_ODD, D, 3 * DIFF_W), D ** -0.5),
        'diff_lq1': nrm((N_ODD, HEAD_DIM), 0.1),
        'diff_lk1': nrm((N_ODD, HEAD_DIM), 0.1),
        'diff_lq2': nrm((N_ODD, HEAD_DIM), 0.1),
        'diff_lk2': nrm((N_ODD, HEAD_DIM), 0.1),
        'diff_subln': gain((N_ODD, 2 * HEAD_DIM)),
        'diff_w_out': nrm((N_ODD, DIFF_W, D), DIFF_W ** -0.5),
        'norm_f': gain((D,)),
    }


def reference(x, c, positions, mod_w, mod_b, norm_mix, norm_ffn, ffn_w_gate, ffn_w_up,
              ffn_conv_w, ffn_conv_b, ffn_w_down, hyb_w_in, nsa_pos_k, nsa_pos_v,
              nsa_ck_w1, nsa_ck_w2, nsa_cv_w1, nsa_cv_w2, hyb_w_out, diff_w_qkv,
              diff_lq1, diff_lk1, diff_lq2, diff_lk2, diff_subln, diff_w_out, norm_f):
    cond = jax.nn.silu(c)
    for i in range(DEPTH):
        mod = cond @ mod_w[i] + mod_b[i]
        sh_m, sc_m, g_m, sh_f, sc_f, g_f = jnp.split(mod, 6, axis=-1)
        h = modulate(rms_norm(x, norm_mix[i]), sh_m, sc_m)
        if i % 2 == 0:
            j = i // 2
            y = hybrid_nsa_sb_mixer(h, positions, hyb_w_in[j], nsa_pos_k[j], nsa_pos_v[j],
                                    nsa_ck_w1[j], nsa_ck_w2[j], nsa_cv_w1[j], nsa_cv_w2[j],
                                    hyb_w_out[j])
        else:
            j = i // 2
            y = diff_mixer(h, positions, diff_w_qkv[j], diff_lq1[j], diff_lk1[j], diff_lq2[j],
                           diff_lk2[j], diff_subln[j], diff_w_out[j], i)
        x = x + g_m[:, None, :] * y
        h = modulate(rms_norm(x, norm_ffn[i]), sh_f, sc_f)
        x = x + g_f[:, None, :] * conv_ffn(h, ffn_w_gate[i], ffn_w_up[i], ffn_conv_w[i],
                                           ffn_conv_b[i], ffn_w_down[i])
    return rms_norm(x, norm_f)
```

```python
import math
from contextlib import ExitStack
import numpy as np
import ml_dtypes
import concourse.bass as bass
import concourse.mybir as mybir
from concourse.bass_utils import run_bass_kernel_spmd

F32 = mybir.dt.float32
BF16 = mybir.dt.bfloat16
I32 = mybir.dt.int32
AF = mybir.ActivationFunctionType
ALU = mybir.AluOpType
AX = mybir.AxisListType
NPBF = ml_dtypes.bfloat16

SAME_ENGINE_SYNC = True
N_DMA_SEMS = 16


class Res:
    __slots__ = ("name", "w", "rs")

    def __init__(self, name):
        self.name = name
        self.w = None
        self.rs = []


class Tile:
    def __init__(self, h, name):
        self.h = h
        self.r = Res(name)
        self._subs = {}
        self.name = name

    def __getitem__(self, k):
        return self.h[k]

    def sub(self, key):
        s = self._subs.get(key)
        if s is None:
            s = Res(f"{self.name}/{key}")
            self._subs[key] = s
        return s


class Sched:
    ENG = ("pe", "act", "dve", "pool", "sp")

    def __init__(self, nc, stack):
        self.nc = nc
        self.stack = stack
        self.ops = {e: [] for e in self.ENG}
        self.cnt = {e: 0 for e in self.ENG}
        self.known = {e: {} for e in self.ENG}
        self.dma_cnt = [0] * N_DMA_SEMS
        self.dma_rr = 0
        self.sems = {}
        for e in ("pe", "act", "dve", "pool"):
            self.sems[e] = stack.enter_context(nc.semaphore("s_" + e))
        for i in range(N_DMA_SEMS):
            self.sems[("d", i)] = stack.enter_context(nc.semaphore(f"s_d{i}"))
        self.out_events = []
        self.n_names = 0

    def sb(self, shape, dtype, name=None):
        self.n_names += 1
        name = f"{name or 't'}_{self.n_names}"
        h = self.stack.enter_context(self.nc.sbuf_tensor(name, list(shape), dtype))
        return Tile(h, name)

    def ps(self, shape, dtype, name=None):
        self.n_names += 1
        name = f"{name or 'p'}_{self.n_names}"
        h = self.stack.enter_context(self.nc.psum_tensor(name, list(shape), dtype))
        return Tile(h, name)

    def _deps(self, eng, r, w):
        deps = {}
        def add(ev):
            if ev is None:
                return
            k, v = ev
            if deps.get(k, 0) < v:
                deps[k] = v
        for x in r:
            add(x.w)
        for x in w:
            add(x.w)
            for ev in x.rs:
                add(ev)
        waits = []
        kn = self.known[eng]
        for k, v in deps.items():
            if k == eng and (eng == "pe" or not SAME_ENGINE_SYNC):
                continue
            if kn.get(k, 0) >= v:
                continue
            kn[k] = v
            waits.append((k, v))
        return waits

    def _commit(self, ev, r, w):
        for x in r:
            x.rs.append(ev)
        for x in w:
            x.w = ev
            x.rs = []

    @staticmethod
    def _res(lst):
        out = []
        for x in lst:
            out.append(x.r if isinstance(x, Tile) else x)
        return out

    def op(self, eng, fn, r=(), w=()):
        r = self._res(r)
        w = self._res(w)
        waits = self._deps(eng, r, w)
        self.cnt[eng] += 1
        ev = (eng, self.cnt[eng])
        self.ops[eng].append((waits, fn, (eng, 1)))
        self._commit(ev, r, w)
        return ev

    def dma(self, eng, out, in_, r=(), w=(), is_output=False, **kw):
        r = self._res(r)
        w = self._res(w)
        waits = self._deps(eng, r, w)
        si = self.dma_rr
        self.dma_rr = (self.dma_rr + 1) % N_DMA_SEMS
        key = ("d", si)
        prev = 16 * self.dma_cnt[si]
        kn = self.known[eng]
        if prev > 0 and kn.get(key, 0) < prev:
            kn[key] = prev
            waits.append((key, prev))
        self.dma_cnt[si] += 1
        ev = (key, 16 * self.dma_cnt[si])
        def fn(e, out=out, in_=in_, kw=kw):
            return e.dma_start(out=out, in_=in_, **kw)
        self.ops[eng].append((waits, fn, (key, 16)))
        self._commit(ev, r, w)
        if is_output:
            self.out_events.append(ev)
        return ev

    def barrier(self):
        evs = [(e, self.cnt[e]) for e in ("pe", "act", "dve", "pool") if self.cnt[e] > 0]
        evs += [(("d", i), 16 * self.dma_cnt[i]) for i in range(N_DMA_SEMS) if self.dma_cnt[i] > 0]
        for eng in self.ENG:
            kn = self.known[eng]
            waits = []
            for (k, v) in evs:
                if kn.get(k, 0) >= v:
                    continue
                kn[k] = v
                waits.append((k, v))
            if waits:
                self.ops[eng].append((waits, None, None))

    def finish(self):
        final = {}
        for (k, v) in self.out_events:
            if final.get(k, 0) < v:
                final[k] = v
        waits = [(k, v) for k, v in final.items()]
        self.ops["sp"].append((waits, None, None))

    def emit(self):
        nc = self.nc
        sems = self.sems
        ops = self.ops
        def run(engname, e):
            for (waits, fn, inc) in ops[engname]:
                for (k, v) in waits:
                    e.wait_ge(sems[k], v)
                if fn is not None:
                    ins = fn(e)
                    ins.then_inc(sems[inc[0]], inc[1])
        with nc.Block() as block:
            @block.tensor
            def _(e):
                run("pe", e)
            @block.scalar
            def _(e):
                run("act", e)
            @block.vector
            def _(e):
                run("dve", e)
            @block.gpsimd
            def _(e):
                run("pool", e)
            @block.sync
            def _(e):
                run("sp", e)


D = 1024
DFF = 2816
NFC = DFF // 128
EPS = 1e-6
NEG = -30000.0
ROPE_THETA = 500000.0
LAMBDA_INIT = 0.8 - 0.6 * math.exp(-0.3 * 1)
C1 = 6.28125
C2 = 2 * math.pi - 6.28125


def new_nc():
    return bass.Bass("TRN2", target_bir_lowering=False)


def din(nc, name, shape, dt):
    return nc.dram_tensor(name, list(shape), dt, kind="ExternalInput").ap()


def dout(nc, name, shape, dt):
    return nc.dram_tensor(name, list(shape), dt, kind="ExternalOutput").ap()


def host_consts():
    c = {}
    c["ident"] = np.eye(128, dtype=np.float32).astype(NPBF)
    p = np.arange(128)
    sw = np.zeros((128, 128), np.float32)
    for m in range(128):
        r = m % 64
        if r < 8:
            sw[m + 8, m] = 1
        elif r < 16:
            sw[m - 8, m] = 1
    c["pswap"] = sw.astype(NPBF)
    rc = np.zeros((128, 2), np.float32)
    for m in range(128):
        r = m % 64
        if r < 16:
            rc[m, 0] = ROPE_THETA ** (-(2 * (r % 8)) / 16.0)
            rc[m, 1] = -1.0 if r < 8 else 1.0
    c["ropec"] = rc
    n = np.arange(512)[None, :]
    pp = p[:, None]
    mle = np.stack([np.where(n >= 128 * d + pp, 0.0, NEG) for d in range(4)])
    mlt = np.stack([np.where(n > 128 * d + pp, 0.0, NEG) for d in range(4)])
    mwin = np.stack([np.where(n < 128 * d + pp, 0.0, NEG) for d in range(4)])
    c["mle"] = mle.astype(NPBF)
    c["mlt"] = mlt.astype(NPBF)
    c["mwin"] = mwin.astype(NPBF)
    tri = np.where(p[:, None] >= p[None, :], -1.0, 0.0)
    c["negtri"] = tri.astype(NPBF)
    c["negones"] = (-np.ones((128, 128), np.float32)).astype(NPBF)
    return c


def rope_tables(S, pos_ap, ntok, ropec, name):
    Ct = S.sb([128, ntok], F32, name + "C")
    St = S.sb([128, ntok], F32, name + "S")
    with ExitStack() as st:
        old = S.stack
        S.stack = st
        posi = S.sb([128, ntok], I32, "posi")
        ang = S.sb([128, ntok], F32, "ang")
        u = S.sb([128, ntok], F32, "u")
        ki = S.sb([128, ntok], I32, "ki")
        kf = S.sb([128, ntok], F32, "kf")
        S.dma("sp", posi[:], pos_ap.partition_broadcast(128), w=[posi])
        S.op("dve", lambda e: e.tensor_copy(out=ang[:], in_=posi[:]), r=[posi], w=[ang])
        S.op("dve", lambda e: e.tensor_scalar(out=ang[:], in0=ang[:], scalar1=ropec[:, 0:1], scalar2=None, op0=ALU.mult, op1=ALU.bypass), r=[ang, ropec], w=[ang])
        for (off, dst, sgn) in ((0.5 * math.pi, Ct, False), (0.0, St, True)):
            S.op("dve", lambda e, off=off: e.tensor_single_scalar(out=u[:], in_=ang[:], scalar=off, op=ALU.add), r=[ang], w=[u])
            S.op("dve", lambda e: e.tensor_single_scalar(out=ki[:], in_=u[:], scalar=1.0 / (2 * math.pi), op=ALU.mult), r=[u], w=[ki])
            S.op("dve", lambda e: e.tensor_copy(out=kf[:], in_=ki[:]), r=[ki], w=[kf])
            S.op("dve", lambda e: e.scalar_tensor_tensor(out=u[:], in0=kf[:], scalar=-C1, in1=u[:], op0=ALU.mult, op1=ALU.add), r=[kf, u], w=[u])
            S.op("dve", lambda e: e.scalar_tensor_tensor(out=u[:], in0=kf[:], scalar=-C2, in1=u[:], op0=ALU.mult, op1=ALU.add), r=[kf, u], w=[u])
            S.op("dve", lambda e: e.tensor_scalar(out=kf[:], in0=u[:], scalar1=math.pi, scalar2=2 * math.pi, op0=ALU.is_gt, op1=ALU.mult), r=[u], w=[kf])
            S.op("dve", lambda e: e.tensor_sub(out=u[:], in0=u[:], in1=kf[:]), r=[u, kf], w=[u])
            S.op("dve", lambda e: e.tensor_scalar(out=u[:], in0=u[:], scalar1=math.pi, scalar2=-math.pi, op0=ALU.min, op1=ALU.max), r=[u], w=[u])
            S.op("act", lambda e, dst=dst: e.activation(out=dst[:], in_=u[:], func=AF.Sin), r=[u], w=[dst])
            if sgn:
                S.op("dve", lambda e, dst=dst: e.tensor_scalar(out=dst[:], in0=dst[:], scalar1=ropec[:, 1:2], scalar2=None, op0=ALU.mult, op1=ALU.bypass), r=[dst, ropec], w=[dst])
        S.barrier()
        S.stack = old
    return Ct, St


def load_const(S, ap, shape, dt, name):
    t = S.sb(shape, dt, name)
    S.dma("sp", t[:], ap, w=[t])
    return t


def rstd_from_ssq(S, ssq, rstd, n, ntok=128, cols=None):
    sl = (slice(0, ntok), slice(None) if cols is None else cols)
    S.op("dve", lambda e: e.tensor_scalar(out=rstd[sl], in0=ssq[sl], scalar1=1.0 / n, scalar2=EPS, op0=ALU.mult, op1=ALU.add), r=[ssq], w=[rstd])
    S.op("act", lambda e: e.activation(out=rstd[sl], in_=rstd[sl], func=AF.Ln), r=[rstd], w=[rstd])
    S.op("act", lambda e: e.activation(out=rstd[sl], in_=rstd[sl], func=AF.Exp, scale=-0.5), r=[rstd], w=[rstd])


def load_weight_bf16(S, Wb, w_ap, nk, ncols, stage_cols=1024, col0=0, name="wst"):
    stg = [S.sb([128, stage_cols], F32, name) for _ in range(2)]
    i = 0
    for k in range(nk):
        for c0 in range(0, ncols, stage_cols):
            cw = min(stage_cols, ncols - c0)
            s = stg[i % 2]
            i += 1
            S.dma("sp", s[:, 0:cw], w_ap[k * 128:(k + 1) * 128, col0 + c0:col0 + c0 + cw], w=[s])
            S.op("pool", lambda e, s=s, k=k, c0=c0, cw=cw: e.tensor_copy(out=Wb[:, k, c0:c0 + cw], in_=s[:, 0:cw]), r=[s], w=[Wb])


def norm_to_hT(S, x_t, ntok, tok0, hT, a_t, sh_t, ident, scr):
    junk, ssq, rstd, xn, pT = scr
    S.op("act", lambda e: e.activation(out=junk[0:ntok, :], in_=x_t[0:ntok, :], func=AF.Square, accum_out=ssq[0:ntok, :]), r=[x_t], w=[junk, ssq])
    rstd_from_ssq(S, ssq, rstd, D, ntok)
    S.op("act", lambda e: e.activation(out=xn[0:ntok, :], in_=x_t[0:ntok, :], func=AF.Copy, scale=rstd[0:ntok, :]), r=[x_t, rstd], w=[xn])
    for k in range(8):
        S.op("pe", lambda e, k=k: e.transpose(out=pT[:, k * 128:k * 128 + ntok], in_=xn[0:ntok, k * 128:(k + 1) * 128], identity=ident[0:ntok, 0:ntok]), r=[xn, ident], w=[pT])
    for k in range(8):
        eng = "dve" if k % 2 == 0 else "pool"
        eng = "dve"
        S.op(eng, lambda e, k=k: e.tensor_scalar(out=hT[:, k, tok0:tok0 + ntok], in0=pT[:, k * 128:k * 128 + ntok], scalar1=a_t[:, k:k + 1], scalar2=sh_t[:, k:k + 1], op0=ALU.mult, op1=ALU.add), r=[pT, a_t, sh_t], w=[hT.sub(tok0)])


def norm_scratch(S):
    return [(S.sb([128, D], BF16, "junk"), S.sb([128, 1], F32, "ssq"), S.sb([128, 1], F32, "rstd"),
             S.sb([128, D], BF16, "xn"), S.ps([128, D], BF16, "pT")) for _ in range(2)]


def mod_vectors(S, mv_ap):
    mv = S.sb([128, 3, 8], F32, "mv")
    S.dma("sp", mv[:], mv_ap.rearrange("r (k p) -> p r k", p=128), w=[mv], allow_slow_non_contiguous=True)
    a = S.sb([128, 8], F32, "a")
    S.op("dve", lambda e: e.tensor_single_scalar(out=a[:], in_=mv[:, 1, :], scalar=1.0, op=ALU.add), r=[mv], w=[a])
    S.op("dve", lambda e: e.tensor_mul(out=a[:], in0=a[:], in1=mv[:, 0, :]), r=[a, mv], w=[a])
    sh = S.sb([128, 8], F32, "sh")
    S.op("dve", lambda e: e.tensor_copy(out=sh[:], in_=mv[:, 2, :]), r=[mv], w=[sh])
    return a, sh


def emit_pre(S, T_loc, colspec, NC, x_ap, pos_ap, mv_ap, w_ap, fm_ap, tm_ap, cst, x_res=None):
    NT = T_loc // 128
    TG = min(512, T_loc)
    NTG = T_loc // TG
    ident, pswap, ropec = cst["ident"], cst["pswap"], cst["ropec"]
    a, sh = mod_vectors(S, mv_ap)
    hT = S.sb([128, 8, T_loc], BF16, "hT")
    Wb = S.sb([128, 8, NC], BF16, "Wb")
    need_rope = any(c.get("rope") for c in colspec)
    if need_rope:
        Ct, St = rope_tables(S, pos_ap, T_loc, ropec, "rp")
    load_weight_bf16(S, Wb, w_ap, 8, NC)
    scr = norm_scratch(S)
    xt = [S.sb([128, D], F32, "xt") for _ in range(2)]
    for tt in range(NT):
        x_t = xt[tt % 2]
        S.dma("sp", x_t[:], x_ap[tt * 128:(tt + 1) * 128, :], r=[x_res] if x_res else [], w=[x_t])
        norm_to_hT(S, x_t, 128, tt * 128, hT, a, sh, ident, scr[tt % 2])
    hT_all = [hT.sub(tt * 128) for tt in range(NT)]
    psA = [S.ps([128, 512], F32, "psA") for _ in range(2)]
    psB = S.ps([128, 512], F32, "psB")
    xb = [S.sb([128, 512], BF16, "xb") for _ in range(2)]
    t1 = [S.sb([128, 512], F32, "t1") for _ in range(2)]
    t2 = [S.sb([128, 512], F32, "t2") for _ in range(2)]
    ob = [S.sb([128, 512], BF16, "ob") for _ in range(3)]
    it = 0
    fmi = 0
    tmoff = 0
    for c in colspec:
        if c["kind"] == "fm":
            c0 = c["col"]
            for tg in range(NTG):
                it += 1
                ps = psA[it % 2]
                tsl = slice(tg * TG, (tg + 1) * TG)
                for k in range(8):
                    S.op("pe", lambda e, ps=ps, k=k, c0=c0, tsl=tsl: e.matmul(ps[:, 0:TG], lhsT=Wb[:, k, c0:c0 + 128], rhs=hT[:, k, tsl], start=(k == 0), stop=(k == 7)),
                         r=[Wb] + hT_all[tg * (TG // 128):(tg + 1) * (TG // 128)], w=[ps])
                o = ob[it % 3]
                if not c.get("rope"):
                    S.op("act", lambda e, ps=ps, o=o, sc=c.get("scale", 1.0): e.activation(out=o[:, 0:TG], in_=ps[:, 0:TG], func=AF.Copy, scale=sc), r=[ps], w=[o])
                else:
                    b = xb[it % 2]; u1 = t1[it % 2]; u2 = t2[it % 2]
                    S.op("act", lambda e, ps=ps, b=b: e.activation(out=b[:, 0:TG], in_=ps[:, 0:TG], func=AF.Copy), r=[ps], w=[b])
                    S.op("pe", lambda e, b=b: e.matmul(psB[:, 0:TG], lhsT=pswap[:], rhs=b[:, 0:TG], start=True, stop=True), r=[b, pswap], w=[psB])
                    S.op("dve", lambda e, b=b, u1=u1, tsl=tsl: e.tensor_mul(out=u1[:, 0:TG], in0=b[:, 0:TG], in1=Ct[:, tsl]), r=[b, Ct], w=[u1])
                    S.op("dve", lambda e, u2=u2, tsl=tsl: e.tensor_mul(out=u2[:, 0:TG], in0=psB[:, 0:TG], in1=St[:, tsl]), r=[psB, St], w=[u2])
                    S.op("pool", lambda e, o=o, u1=u1, u2=u2: e.tensor_add(out=o[:, 0:TG], in0=u1[:, 0:TG], in1=u2[:, 0:TG]), r=[u1, u2], w=[o])
                S.dma("sp", fm_ap[fmi, :, tsl], o[:, 0:TG], r=[o], is_output=True)
            fmi += 1
        else:
            c0, n = c["col"], c["n"]
            for tt in range(NT):
                for cc in range(0, n, 512):
                    cw = min(512, n - cc)
                    it += 1
                    ps = psA[it % 2]
                    for k in range(8):
                        S.op("pe", lambda e, ps=ps, k=k, tt=tt, cc=cc, cw=cw, c0=c0: e.matmul(ps[:, 0:cw], lhsT=hT[:, k, tt * 128:(tt + 1) * 128], rhs=Wb[:, k, c0 + cc:c0 + cc + cw], start=(k == 0), stop=(k == 7)),
                             r=[Wb, hT_all[tt]], w=[ps])
                    o = ob[it % 3]
                    S.op("act", lambda e, ps=ps, o=o, cw=cw: e.activation(out=o[:, 0:cw], in_=ps[:, 0:cw], func=AF.Copy), r=[ps], w=[o])
                    S.dma("sp", tm_ap[tt * 128:(tt + 1) * 128, tmoff + cc:tmoff + cc + cw], o[:, 0:cw], r=[o], is_output=True)
            tmoff += n


def colspec_l0():
    cs = []
    for i in range(4):
        cs.append(dict(kind="fm", col=128 * i, rope=True))
    cs.append(dict(kind="fm", col=512))
    cs.append(dict(kind="fm", col=640))
    cs.append(dict(kind="fm", col=768, rope=True))
    cs.append(dict(kind="fm", col=1024, rope=True))
    for i in range(4):
        cs.append(dict(kind="fm", col=1304 + 128 * i, scale=0.125))
    for i in range(4):
        cs.append(dict(kind="fm", col=1816 + 128 * i))
    cs.append(dict(kind="tm", col=896, n=128))
    cs.append(dict(kind="tm", col=1152, n=128))
    cs.append(dict(kind="tm", col=1280, n=24))
    cs.append(dict(kind="tm", col=2328, n=512))
    return cs, 16, 792


def colspec_l1():
    cs = []
    for i in range(8):
        cs.append(dict(kind="fm", col=128 * i, rope=True))
    for i in range(8):
        cs.append(dict(kind="fm", col=1024 + 128 * i, rope=True))
    cs.append(dict(kind="tm", col=2048, n=1024))
    return cs, 16, 1024


def load_csts(S, nc, names):
    hc = host_consts()
    out = {}
    for n in names:
        arr = hc[n]
        dt = BF16 if arr.dtype == NPBF else F32
        ap = din(nc, "c_" + n, arr.shape, dt)
        if arr.ndim == 3:
            t = S.sb([arr.shape[1], arr.shape[0], arr.shape[2]], dt, n)
            S.dma("sp", t[:], ap.rearrange("d p n -> p d n"), w=[t])
        else:
            t = S.sb(list(arr.shape), dt, n)
            S.dma("sp", t[:], ap, w=[t])
        out[n] = t
    return out, {"c_" + n: hc[n] for n in names}


def build_pre(T_loc, layer):
    cs, nfm, ntm = colspec_l0() if layer == 0 else colspec_l1()
    NC = 2840 if layer == 0 else 3072
    nc = new_nc()
    x = din(nc, "x", [T_loc, D], F32)
    pos = din(nc, "pos", [1, T_loc], I32)
    mv = din(nc, "mv", [3, D], F32)
    w = din(nc, "w", [D, NC], F32)
    fm = dout(nc, "fm", [nfm, 128, T_loc], BF16)
    tm = dout(nc, "tm", [T_loc, ntm], BF16)
    with ExitStack() as st:
        S = Sched(nc, st)
        cst, cmap = load_csts(S, nc, ["ident", "pswap", "ropec"])
        emit_pre(S, T_loc, cs, NC, x, pos, mv, w, fm, tm, cst)
        S.finish(); S.emit()
    return nc, cmap


def emit_post(S, T_loc, oT_ap, x_ap, wo_ap, mvec_ap, mvf_ap, wg_ap, wu_ap, wd_ap, cw_ap, cb_ap, flag_ap, xo_ap, cst, final_g_ap=None):
    TE = T_loc + 2
    NT = T_loc // 128
    ident = cst["ident"]
    tiles = [(0, 2)] + [(2 + 128 * i, 128) for i in range(NT)]
    a_f, sh_f = mod_vectors(S, mvf_ap)
    gm_b = S.sb([128, D], F32, "gm_b"); gf_b = S.sb([128, D], F32, "gf_b")
    S.dma("sp", gm_b[:], mvec_ap[0:1, :].partition_broadcast(128), w=[gm_b])
    S.dma("sp", gf_b[:], mvec_ap[1:2, :].partition_broadcast(128), w=[gf_b])
    if final_g_ap is not None:
        nf_b = S.sb([128, D], F32, "nf_b")
        S.dma("sp", nf_b[:], final_g_ap.partition_broadcast(128), w=[nf_b])
    flag = S.sb([128, 1], F32, "flag")
    S.dma("sp", flag[:], flag_ap.partition_broadcast(128), w=[flag])
    cw = S.sb([128, 3, NFC], F32, "cw"); cb = S.sb([128, NFC], F32, "cb")
    S.dma("sp", cw[:], cw_ap.rearrange("r (c p) -> p r c", p=128), w=[cw], allow_slow_non_contiguous=True)
    S.dma("sp", cb[:], cb_ap.rearrange("r (c p) -> p (r c)", p=128), w=[cb], allow_slow_non_contiguous=True)
    hT = S.sb([128, 8, TE], BF16, "hT")
    psA = [S.ps([128, 512], F32, "psA") for _ in range(2)]
    psB = [S.ps([128, 512], F32, "psB") for _ in range(2)]
    with ExitStack() as st:
        old = S.stack; S.stack = st
        oT = S.sb([128, 8, TE], BF16, "oT")
        S.dma("sp", oT[:], oT_ap.rearrange("(k p) t -> p k t", p=128), w=[oT])
        Wo = S.sb([128, 8, D], BF16, "Wo")
        load_weight_bf16(S, Wo, wo_ap, 8, D)
        scr = norm_scratch(S)
        xt = [S.sb([128, D], F32, "xt") for _ in range(2)]
        tmp = [S.sb([128, D], F32, "tmp") for _ in range(2)]
        for ti, (r0, n) in enumerate(tiles):
            x_t = xt[ti % 2]; t_t = tmp[ti % 2]
            S.dma("sp", x_t[0:n, :], x_ap[r0:r0 + n, :], w=[x_t])
            for half in range(2):
                ps = psA[half]
                for k in range(8):
                    S.op("pe", lambda e, ps=ps, k=k, r0=r0, n=n, half=half: e.matmul(ps[0:n, :], lhsT=oT[:, k, r0:r0 + n], rhs=Wo[:, k, half * 512:(half + 1) * 512], start=(k == 0), stop=(k == 7)), r=[oT, Wo], w=[ps])
                S.op("dve", lambda e, ps=ps, n=n, half=half, t_t=t_t: e.tensor_mul(out=t_t[0:n, half * 512:(half + 1) * 512], in0=ps[0:n, :], in1=gm_b[0:n, half * 512:(half + 1) * 512]), r=[ps, gm_b], w=[t_t])
            S.op("pool", lambda e, n=n, x_t=x_t, t_t=t_t: e.tensor_add(out=x_t[0:n, :], in0=x_t[0:n, :], in1=t_t[0:n, :]), r=[t_t, x_t], w=[x_t])
            if ti > 0:
                S.dma("sp", xo_ap[r0 - 2:r0 - 2 + n, :], x_t[0:n, :], r=[x_t], w=[S_xo(S)], is_output=True)
            norm_to_hT(S, x_t, n, r0, hT, a_f, sh_f, ident, scr[ti % 2])
        S.barrier()
        S.stack = old
    hT_all = [hT.sub(r0) for (r0, n) in tiles]
    actT = S.sb([128, NFC, T_loc], BF16, "actT")
    Wd = S.sb([128, NFC, D], BF16, "Wd")
    with ExitStack() as st:
        old = S.stack; S.stack = st
        wst = [S.sb([128, 8, 256], F32, "wst") for _ in range(1)]
        Wgu = [S.sb([128, 8, 256], BF16, "Wgu") for _ in range(2)]
        gx = [S.sb([128, 514], F32, "gx") for _ in range(2)]
        tc_ = [S.sb([128, 512], F32, "tc") for _ in range(2)]
        sg = [S.sb([128, 512], F32, "sg") for _ in range(2)]
        wdst = [S.sb([128, 512], F32, "wdst") for _ in range(1)]
        TG = min(512, T_loc)
        groups = [(0, 2)] + [(2 + TG * i, TG) for i in range(T_loc // TG)]
        it = 0
        for fc in range(NFC):
            ws = wst[0]; wb = Wgu[fc % 2]
            S.dma("sp", ws[:, :, 0:128], wg_ap[:, fc * 128:(fc + 1) * 128].rearrange("(k p) c -> p k c", p=128), w=[ws])
            S.dma("sp", ws[:, :, 128:256], wu_ap[:, fc * 128:(fc + 1) * 128].rearrange("(k p) c -> p k c", p=128), w=[ws])
            S.op("pool", lambda e, ws=ws, wb=wb: e.tensor_copy(out=wb[:], in_=ws[:]), r=[ws], w=[wb])
            wd_s = wdst[0]
            for hh in range(2):
                S.dma("sp", wd_s[:], wd_ap[fc * 128:(fc + 1) * 128, hh * 512:(hh + 1) * 512], w=[wd_s])
                S.op("pool", lambda e, wd_s=wd_s, fc=fc, hh=hh: e.tensor_copy(out=Wd[:, fc, hh * 512:(hh + 1) * 512], in_=wd_s[:]), r=[wd_s], w=[Wd.sub((fc, hh))])
            for gi, (r0, n) in enumerate(groups):
                it += 1
                pg = psA[it % 2]; pu = psB[it % 2]
                g = gx[it % 2]; gprev = gx[(it - 1) % 2]
                hdeps = [hT_all[i] for i, (tr0, tn) in enumerate(tiles) if tr0 >= r0 and tr0 < r0 + n]
                for k in range(8):
                    S.op("pe", lambda e, pg=pg, k=k, r0=r0, n=n, wb=wb: e.matmul(pg[:, 0:n], lhsT=wb[:, k, 0:128], rhs=hT[:, k, r0:r0 + n], start=(k == 0), stop=(k == 7)), r=[wb] + hdeps, w=[pg])
                if gi == 0:
                    gnext = gx[(it + 1) % 2]
                    S.op("dve", lambda e, pg=pg, gnext=gnext: e.tensor_scalar(out=gnext[:, 0:2], in0=pg[:, 0:2], scalar1=flag[:, 0:1], scalar2=None, op0=ALU.mult, op1=ALU.bypass), r=[pg, flag], w=[gnext])
                    continue
                for k in range(8):
                    S.op("pe", lambda e, pu=pu, k=k, r0=r0, n=n, wb=wb: e.matmul(pu[:, 0:n], lhsT=wb[:, k, 128:256], rhs=hT[:, k, r0:r0 + n], start=(k == 0), stop=(k == 7)), r=[wb] + hdeps, w=[pu])
                S.op("act", lambda e, pg=pg, g=g, n=n: e.activation(out=g[:, 2:2 + n], in_=pg[:, 0:n], func=AF.Copy), r=[pg], w=[g])
                if gi < len(groups) - 1:
                    gnext = gx[(it + 1) % 2]
                    S.op("pool", lambda e, g=g, gnext=gnext, n=n: e.tensor_copy(out=gnext[:, 0:2], in_=g[:, n:n + 2]), r=[g], w=[gnext])
                t = tc_[it % 2]; s = sg[it % 2]
                S.op("dve", lambda e, g=g, t=t, n=n, fc=fc: e.tensor_scalar(out=t[:, 0:n], in0=g[:, 2:2 + n], scalar1=cw[:, 2, fc:fc + 1], scalar2=cb[:, fc:fc + 1], op0=ALU.mult, op1=ALU.add), r=[g, cw, cb], w=[t])
                S.op("dve", lambda e, g=g, t=t, n=n, fc=fc: e.scalar_tensor_tensor(out=t[:, 0:n], in0=g[:, 1:1 + n], scalar=cw[:, 1, fc:fc + 1], in1=t[:, 0:n], op0=ALU.mult, op1=ALU.add), r=[g, cw, t], w=[t])
                S.op("dve", lambda e, g=g, t=t, n=n, fc=fc: e.scalar_tensor_tensor(out=t[:, 0:n], in0=g[:, 0:n], scalar=cw[:, 0, fc:fc + 1], in1=t[:, 0:n], op0=ALU.mult, op1=ALU.add), r=[g, cw, t], w=[t])
                S.op("act", lambda e, t=t, s=s, n=n: e.activation(out=s[:, 0:n], in_=t[:, 0:n], func=AF.Silu), r=[t], w=[s])
                S.op("dve", lambda e, s=s, pu=pu, n=n, fc=fc, r0=r0: e.tensor_mul(out=actT[:, fc, r0 - 2:r0 - 2 + n], in0=pu[:, 0:n], in1=s[:, 0:n]), r=[pu, s], w=[actT.sub((fc, r0))])
        S.barrier()
        S.stack = old
    xm = [S.sb([128, D], F32, "xm") for _ in range(2)]
    tmp = [S.sb([128, D], F32, "tmp2") for _ in range(2)]
    junk = S.sb([128, D], BF16, "junk2"); ssq = S.sb([128, 1], F32, "ssq2"); rstd = S.sb([128, 1], F32, "rstd2")
    for tt in range(NT):
        x_t = xm[tt % 2]; t_t = tmp[tt % 2]
        S.dma("sp", x_t[:], xo_ap[tt * 128:(tt + 1) * 128, :], r=[S_xo(S)], w=[x_t])
        for half in range(2):
            ps = psA[half]
            for fc in range(NFC):
                S.op("pe", lambda e, ps=ps, fc=fc, tt=tt, half=half: e.matmul(ps[:], lhsT=actT[:, fc, tt * 128:(tt + 1) * 128], rhs=Wd[:, fc, half * 512:(half + 1) * 512], start=(fc == 0), stop=(fc == NFC - 1)), r=[actT, Wd] + [Wd.sub((fc, half))], w=[ps])
            S.op("dve", lambda e, ps=ps, half=half, t_t=t_t: e.tensor_mul(out=t_t[:, half * 512:(half + 1) * 512], in0=ps[:], in1=gf_b[:, half * 512:(half + 1) * 512]), r=[ps, gf_b], w=[t_t])
        S.op("pool", lambda e, x_t=x_t, t_t=t_t: e.tensor_add(out=t_t[:], in0=x_t[:], in1=t_t[:]), r=[t_t, x_t], w=[t_t])
        if final_g_ap is not None:
            S.op("act", lambda e, t_t=t_t: e.activation(out=junk[:], in_=t_t[:], func=AF.Square, accum_out=ssq[:]), r=[t_t], w=[junk, ssq])
            rstd_from_ssq(S, ssq, rstd, D)
            S.op("dve", lambda e, t_t=t_t: e.scalar_tensor_tensor(out=t_t[:], in0=t_t[:], scalar=rstd[:, 0:1], in1=nf_b[:], op0=ALU.mult, op1=ALU.mult), r=[t_t, rstd, nf_b], w=[t_t])
        S.dma("sp", xo_ap[tt * 128:(tt + 1) * 128, :], t_t[:], r=[t_t], w=[S_xo(S)], is_output=True)


def S_xo(S):
    if not hasattr(S, "_xo"):
        S._xo = Res("xo_dram")
    return S._xo


def build_post(T_loc, final):
    nc = new_nc()
    oT = din(nc, "oT", [D, T_loc + 2], BF16); x = din(nc, "x", [T_loc + 2, D], F32)
    wo = din(nc, "wo", [D, D], F32); mvec = din(nc, "mvec", [2, D], F32); mvf = din(nc, "mvf", [3, D], F32)
    wg = din(nc, "wg", [D, DFF], F32); wu = din(nc, "wu", [D, DFF], F32); wd = din(nc, "wd", [DFF, D], F32)
    cw = din(nc, "cw", [3, DFF], F32); cb = din(nc, "cb", [1, DFF], F32); flag = din(nc, "flag", [1, 1], F32)
    fg = din(nc, "fg", [1, D], F32) if final else None
    xo = dout(nc, "xo", [T_loc, D], F32)
    with ExitStack() as st:
        S = Sched(nc, st)
        cst, cmap = load_csts(S, nc, ["ident"])
        emit_post(S, T_loc, oT, x, wo, mvec, mvf, wg, wu, wd, cw, cb, flag, xo, cst, final_g_ap=fg)
        S.finish(); S.emit()
    return nc, cmap


def emit_diff(S, T, qT_ap, kT_ap, v_ap, lam_ap, subln_ap, oT_ap, cst):
    NKB = T // 128
    NQT = T // 512
    ident, mle = cst["ident"], cst["mle"]
    qT = [S.sb([64, T], BF16, "qT") for _ in range(4)]
    kT = [S.sb([64, T], BF16, "kT") for _ in range(4)]
    for i in range(4):
        S.dma("sp", qT[i][:], qT_ap[i], w=[qT[i]])
        S.dma("sp", kT[i][:], kT_ap[i], w=[kT[i]])
    Va = S.sb([128, NKB, 2, 129], BF16, "Va")
    S.op("pool", lambda e: e.memset(Va[:], 1.0), w=[Va])
    for h in range(2):
        S.dma("sp", Va[:, :, h, 0:128], v_ap[:, h * 128:(h + 1) * 128].rearrange("(kb p) d -> p kb d", p=128), w=[Va])
    lv = S.sb([128, 4, 64], F32, "lv")
    S.dma("sp", lv[:], lam_ap.rearrange("a d -> (a d)").partition_broadcast(128).rearrange("p (a d) -> p a d", a=4), w=[lv])
    pr = S.sb([128, 2, 64], F32, "pr")
    S.op("dve", lambda e: e.tensor_mul(out=pr[:, 0, :], in0=lv[:, 0, :], in1=lv[:, 1, :]), r=[lv], w=[pr])
    S.op("dve", lambda e: e.tensor_mul(out=pr[:, 1, :], in0=lv[:, 2, :], in1=lv[:, 3, :]), r=[lv, pr], w=[pr])
    sm = S.sb([128, 2], F32, "sm")
    S.op("dve", lambda e: e.tensor_reduce(out=sm[:], in_=pr[:], axis=AX.X, op=ALU.add), r=[pr], w=[sm])
    S.op("act", lambda e: e.activation(out=sm[:], in_=sm[:], func=AF.Exp), r=[sm], w=[sm])
    nlam = S.sb([128, 1], F32, "nlam")
    S.op("dve", lambda e: e.tensor_sub(out=nlam[:], in0=sm[:, 1:2], in1=sm[:, 0:1]), r=[sm], w=[nlam])
    S.op("dve", lambda e: e.tensor_single_scalar(out=nlam[:], in_=nlam[:], scalar=-LAMBDA_INIT, op=ALU.add), r=[nlam], w=[nlam])
    gsub = S.sb([128, 128], F32, "gsub")
    S.dma("sp", gsub[:], subln_ap.partition_broadcast(128), w=[gsub])
    S.op("dve", lambda e: e.tensor_single_scalar(out=gsub[:], in_=gsub[:], scalar=1.0 - LAMBDA_INIT, op=ALU.mult), r=[gsub], w=[gsub])

    ps_s = [S.ps([128, 512], F32, "ps_s") for _ in range(2)]
    ps_o = [S.ps([128, 2, 129], F32, "ps_o") for _ in range(4)]
    ps_t = S.ps([128, 512], BF16, "ps_t")
    pT = [S.sb([128, 512], BF16, "pT") for _ in range(3)]
    osb = [S.sb([128, 4, 128], F32, "osb") for _ in range(2)]
    rl = S.sb([128, 8], F32, "rl")
    od = S.sb([128, 4, 128], F32, "od")
    junk = S.sb([128, 128], BF16, "junk")
    ssq = S.sb([128, 4], F32, "ssq")
    rstd = S.sb([128, 4], F32, "rstd")
    onb = S.sb([128, 4, 128], BF16, "onb")
    ost = [S.sb([128, 512], BF16, "ost") for _ in range(2)]
    it = 0
    for h in range(2):
        for qt in range(NQT):
            qsl = slice(qt * 512, (qt + 1) * 512)
            for j in range(2):
                q_t, k_t = qT[h * 2 + j], kT[h * 2 + j]
                nkb = 4 * qt + 4
                started = [False, False]
                for kb in range(nkb):
                    d = kb - 4 * qt
                    it += 1
                    ps = ps_s[it % 2]
                    p_t = pT[it % 3]
                    S.op("pe", lambda e, ps=ps, k_t=k_t, q_t=q_t, kb=kb, qsl=qsl, d=d: e.matmul(ps[:], lhsT=k_t[:, kb * 128:(kb + 1) * 128], rhs=q_t[:, qsl], start=True, stop=(d < 0)), r=[k_t, q_t], w=[ps])
                    if d >= 0:
                        S.op("pe", lambda e, ps=ps, d=d: e.matmul(ps[:], lhsT=ident[:], rhs=mle[:, d, :], start=False, stop=True), r=[ident, mle], w=[ps])
                    S.op("act", lambda e, ps=ps, p_t=p_t: e.activation(out=p_t[:], in_=ps[:], func=AF.Exp, scale=0.125), r=[ps], w=[p_t])
                    for sub in range(max(d, 0), 4):
                        bank = ps_o[j * 2 + sub // 2]
                        st = not started[sub // 2]
                        started[sub // 2] = True
                        S.op("pe", lambda e, bank=bank, sub=sub, p_t=p_t, kb=kb, st=st, h=h: e.matmul(bank[:, sub % 2, :], lhsT=p_t[:, sub * 128:(sub + 1) * 128], rhs=Va[:, kb, h, :], start=st, stop=(kb == 4 * qt + sub), skip_group_check=True), r=[p_t, Va], w=[bank])
                for sub in range(4):
                    bank = ps_o[j * 2 + sub // 2]
                    c = j * 4 + sub
                    S.op("dve", lambda e, bank=bank, sub=sub, c=c: e.reciprocal(out=rl[:, c:c + 1], in_=bank[:, sub % 2, 128:129]), r=[bank], w=[rl])
                    S.op("dve", lambda e, bank=bank, sub=sub, c=c, j=j: e.tensor_scalar(out=osb[j][:, sub, :], in0=bank[:, sub % 2, 0:128], scalar1=rl[:, c:c + 1], scalar2=None, op0=ALU.mult, op1=ALU.bypass), r=[bank, rl], w=[osb[j]])
            S.op("dve", lambda e: e.scalar_tensor_tensor(out=od[:], in0=osb[1][:], scalar=nlam[:, 0:1], in1=osb[0][:], op0=ALU.mult, op1=ALU.add), r=[osb[0], osb[1], nlam], w=[od])
            for sub in range(4):
                S.op("act", lambda e, sub=sub: e.activation(out=junk[:], in_=od[:, sub, :], func=AF.Square, accum_out=ssq[:, sub:sub + 1]), r=[od], w=[junk, ssq])
            rstd_from_ssq(S, ssq, rstd, 128)
            for sub in range(4):
                S.op("dve", lambda e, sub=sub: e.scalar_tensor_tensor(out=onb[:, sub, :], in0=od[:, sub, :], scalar=rstd[:, sub:sub + 1], in1=gsub[:], op0=ALU.mult, op1=ALU.mult), r=[od, rstd, gsub], w=[onb])
            for sub in range(4):
                S.op("pe", lambda e, sub=sub: e.transpose(out=ps_t[:, sub * 128:(sub + 1) * 128], in_=onb[:, sub, :], identity=ident[:]), r=[onb, ident], w=[ps_t])
            o_s = ost[(h * NQT + qt) % 2]
            S.op("act", lambda e, o_s=o_s: e.activation(out=o_s[:], in_=ps_t[:], func=AF.Copy), r=[ps_t], w=[o_s])
            S.dma("sp", oT_ap[h * 128:(h + 1) * 128, qsl], o_s[:], r=[o_s], is_output=True)


def build_diff(T):
    nc = new_nc()
    qT = din(nc, "qT", [4, 64, T], BF16); kT = din(nc, "kT", [4, 64, T], BF16)
    v = din(nc, "v", [T, 256], BF16); lam = din(nc, "lam", [4, 64], F32); subln = din(nc, "subln", [1, 128], F32)
    oT = dout(nc, "oT", [256, T], BF16)
    with ExitStack() as st:
        S = Sched(nc, st)
        cst, cmap = load_csts(S, nc, ["ident", "mle"])
        emit_diff(S, T, qT, kT, v, lam, subln, oT, cst)
        S.finish(); S.emit()
    return nc, cmap


def emit_sb(S, T, qT_ap, kT_ap, v_ap, oT_ap, cst, banks=None):
    NKB = T // 128
    NQT = T // 512
    ident, mlt, negtri, negones = cst["ident"], cst["mlt"], cst["negtri"], cst["negones"]
    qT = [S.sb([64, T], BF16, "sqT") for _ in range(2)]
    kT = [S.sb([64, T], BF16, "skT") for _ in range(2)]
    for i in range(2):
        S.dma("sp", qT[i][:], qT_ap[i], w=[qT[i]])
        S.dma("sp", kT[i][:], kT_ap[i], w=[kT[i]])
    V = S.sb([128, NKB, 128], BF16, "sV")
    S.dma("sp", V[:], v_ap.rearrange("(kb p) d -> p kb d", p=128), w=[V])
    ps_z = [S.ps([128, 512], F32, "ps_z") for _ in range(2)]
    ps_c = [S.ps([128, 512], F32, "ps_c") for _ in range(2)]
    ps_o = [S.ps([128, 4, 64], F32, "ps_o") for _ in range(2)]
    ps_t = S.ps([128, 512], BF16, "ps_t")
    E = [S.sb([128, 512], F32, "E") for _ in range(2)]
    sp = [S.sb([128, 512], BF16, "sp") for _ in range(2)]
    Racc = [S.sb([128, 512], BF16, "Racc") for _ in range(2)]
    aT = [S.sb([128, 512], BF16, "aT") for _ in range(2)]
    ob = S.sb([128, 4, 64], BF16, "ob")
    ost = [S.sb([64, 512], BF16, "ost") for _ in range(2)]
    it = 0
    for j in range(2):
        for qt in range(NQT):
            qsl = slice(qt * 512, (qt + 1) * 512)
            po = ps_o[(j * NQT + qt) % 2]
            first_o = True
            ri = 0
            for kb in range(4 * qt + 3, -1, -1):
                d = kb - 4 * qt
                it += 1
                pz = ps_z[it % 2]; pc = ps_c[it % 2]; e_t = E[it % 2]; s_t = sp[it % 2]; a_t = aT[it % 2]
                first = (kb == 4 * qt + 3)
                def zmm(e, ps, last, kb=kb, qsl=qsl, d=d, j=j):
                    pass
                S.op("pe", lambda e, pz=pz, kb=kb, qsl=qsl, d=d, j=j: e.matmul(pz[:], lhsT=kT[j][:, kb * 128:(kb + 1) * 128], rhs=qT[j][:, qsl], start=True, stop=(d < 0)), r=[kT[j], qT[j]], w=[pz])
                if d >= 0:
                    S.op("pe", lambda e, pz=pz, d=d: e.matmul(pz[:], lhsT=ident[:], rhs=mlt[:, d, :], start=False, stop=True), r=[ident, mlt], w=[pz])
                S.op("act", lambda e, pz=pz, e_t=e_t: e.activation(out=e_t[:], in_=pz[:], func=AF.Exp), r=[pz], w=[e_t])
                S.op("act", lambda e, e_t=e_t, s_t=s_t: e.activation(out=s_t[:], in_=e_t[:], func=AF.Ln, bias=1.0), r=[e_t], w=[s_t])
                S.op("pe", lambda e, pc=pc, kb=kb, qsl=qsl, j=j: e.matmul(pc[:], lhsT=kT[j][:, kb * 128:(kb + 1) * 128], rhs=qT[j][:, qsl], start=True, stop=False), r=[kT[j], qT[j]], w=[pc])
                if d >= 0:
                    S.op("pe", lambda e, pc=pc, d=d: e.matmul(pc[:], lhsT=ident[:], rhs=mlt[:, d, :], start=False, stop=False), r=[ident, mlt], w=[pc])
                S.op("pe", lambda e, pc=pc, s_t=s_t, first=first: e.matmul(pc[:], lhsT=negtri[:], rhs=s_t[:], start=False, stop=first), r=[negtri, s_t], w=[pc])
                if not first:
                    rc = Racc[ri % 2]
                    S.op("pe", lambda e, pc=pc, rc=rc: e.matmul(pc[:], lhsT=negones[:], rhs=rc[:], start=False, stop=True), r=[negones, rc], w=[pc])
                    if kb > 0:
                        rn = Racc[(ri + 1) % 2]
                        S.op("pool", lambda e, rc=rc, rn=rn, s_t=s_t: e.tensor_add(out=rn[:], in0=rc[:], in1=s_t[:]), r=[rc, s_t], w=[rn])
                        ri += 1
                else:
                    rn = Racc[ri % 2]
                    S.op("pool", lambda e, rn=rn, s_t=s_t: e.tensor_copy(out=rn[:], in_=s_t[:]), r=[s_t], w=[rn])
                S.op("act", lambda e, pc=pc, a_t=a_t: e.activation(out=a_t[:], in_=pc[:], func=AF.Exp), r=[pc], w=[a_t])
                for sub in range(max(d, 0), 4):
                    S.op("pe", lambda e, po=po, sub=sub, a_t=a_t, kb=kb, st=first_o, j=j: e.matmul(po[:, sub, :], lhsT=a_t[:, sub * 128:(sub + 1) * 128], rhs=V[:, kb, j * 64:(j + 1) * 64], start=st, stop=(kb == 0), skip_group_check=True), r=[a_t, V], w=[po])
                    first_o = False
            S.op("act", lambda e, po=po: e.activation(out=ob[:], in_=po[:], func=AF.Copy), r=[po], w=[ob])
            for sub in range(4):
                S.op("pe", lambda e, sub=sub: e.transpose(out=ps_t[0:64, sub * 128:(sub + 1) * 128], in_=ob[:, sub, :], identity=ident[:]), r=[ob, ident], w=[ps_t])
            o_s = ost[(j * NQT + qt) % 2]
            S.op("dve", lambda e, o_s=o_s: e.tensor_copy(out=o_s[:], in_=ps_t[0:64, :]), r=[ps_t], w=[o_s])
            S.dma("sp", oT_ap[j * 64:(j + 1) * 64, qsl], o_s[:], r=[o_s], is_output=True)


def build_sb(T):
    nc = new_nc()
    qT = din(nc, "qT", [2, 64, T], BF16); kT = din(nc, "kT", [2, 64, T], BF16)
    v = din(nc, "v", [T, 128], BF16)
    oT = dout(nc, "oT", [128, T], BF16)
    with ExitStack() as st:
        S = Sched(nc, st)
        cst, cmap = load_csts(S, nc, ["ident", "mlt", "negtri", "negones"])
        emit_sb(S, T, qT, kT, v, oT, cst)
        S.finish(); S.emit()
    return nc, cmap


def nsa_consts(T):
    c = {}
    p = np.arange(128)[:, None]
    cc = np.arange(512)[None, :]
    c["mbase"] = (16.0 * cc + 31.0 - p).astype(np.float32)
    n = np.arange(128)[None, :]
    c["dmat"] = (n - (p >= 64)).astype(np.float32)
    c["col0"] = np.broadcast_to((n == 0), (128, 128)).astype(np.float32)
    key = np.arange(T)[None, :]
    c["eall"] = ((key // 64) == p).astype(np.float32).astype(NPBF)
    return c


def emit_nsa(S, T, A, cst):
    NKB = T // 128
    NQT = T // 512
    NCc = (T - 32) // 16 + 1
    NCB = (NCc + 127) // 128
    ident, pswap, ropec, mle, mwin = cst["ident"], cst["pswap"], cst["ropec"], cst["mle"], cst["mwin"]
    mbase, dmat, col0, eall = cst["mbase"], cst["dmat"], cst["col0"], cst["eall"]
    ps_s = [S.ps([128, 512], F32, "ps_s") for _ in range(2)]
    ps_os = S.ps([128, 4, 65], F32, "ps_os")
    ps_ow = S.ps([128, 4, 65], F32, "ps_ow")
    ps_oc = S.ps([128, 2, 65], F32, "ps_oc")
    ps_t = S.ps([128, 512], BF16, "ps_t")
    ps_m = S.ps([128, 512], F32, "ps_m")
    kcmpT = S.sb([64, 512], BF16, "kcmpT")
    vcmp = S.sb([128, 4, 65], BF16, "vcmp")
    S.op("pool", lambda e: e.memset(kcmpT[:], 0.0), w=[kcmpT])
    S.op("pool", lambda e: e.memset(vcmp[:], 1.0), w=[vcmp])
    with ExitStack() as st:
        old = S.stack; S.stack = st
        Cc, Sc = rope_tables(S, A["posc"], 512, ropec, "rc")
        def _cmp(which):
            xT = S.sb([64, T], BF16, "cxT")
            S.dma("sp", xT[:], A["kcT"] if which == 0 else A["vcT"], w=[xT])
            W1 = S.sb([64, 32, 256], BF16, "W1")
            w1st = [S.sb([64, 4, 256], F32, "w1st") for _ in range(2)]
            w1v = (A["w1k"] if which == 0 else A["w1v"]).rearrange("(l d) h -> d l h", d=64)
            for li in range(8):
                s = w1st[li % 2]
                S.dma("sp", s[:], w1v[:, li * 4:(li + 1) * 4, :], w=[s])
                S.op("pool", lambda e, s=s, li=li: e.tensor_copy(out=W1[:, li * 4:(li + 1) * 4, :], in_=s[:]), r=[s], w=[W1])
            posf = S.sb([64, 32], F32, "posf"); posb = S.sb([64, 32], BF16, "posb")
            S.dma("sp", posf[:], (A["posk"] if which == 0 else A["posv"]).rearrange("l d -> d l"), w=[posf], allow_slow_non_contiguous=True)
            S.op("dve", lambda e: e.tensor_copy(out=posb[:], in_=posf[:]), r=[posf], w=[posb])
            w2f = S.sb([128, 2, 64], F32, "w2f"); W2 = S.sb([128, 2, 64], BF16, "W2")
            S.dma("sp", w2f[:], (A["w2k"] if which == 0 else A["w2v"]).rearrange("(c p) d -> p c d", p=128), w=[w2f])
            S.op("dve", lambda e: e.tensor_copy(out=W2[:], in_=w2f[:]), r=[w2f], w=[W2])
            hidT = S.sb([128, 2, 512], BF16, "hidT")
            S.op("pool", lambda e: e.memset(hidT[:], 0.0), w=[hidT])
            b1 = S.sb([128, 2], F32, "b1")
            for hc in range(2):
                for l in range(32):
                    S.op("pe", lambda e, l=l, hc=hc: e.matmul(ps_m[:, 0:1], lhsT=W1[:, l, hc * 128:(hc + 1) * 128], rhs=posb[:, l:l + 1], start=(l == 0), stop=(l == 31)), r=[W1, posb], w=[ps_m])
                S.op("dve", lambda e, hc=hc: e.tensor_copy(out=b1[:, hc:hc + 1], in_=ps_m[:, 0:1]), r=[ps_m], w=[b1])
                ps = ps_s[hc]
                for l in range(32):
                    S.op("pe", lambda e, l=l, hc=hc, ps=ps: e.matmul(ps[:, 0:NCc], lhsT=W1[:, l, hc * 128:(hc + 1) * 128], rhs=xT[:, l:l + 16 * (NCc - 1) + 1:16], start=(l == 0), stop=(l == 31)), r=[W1, xT], w=[ps])
                S.op("act", lambda e, hc=hc, ps=ps: e.activation(out=hidT[:, hc, 0:NCc], in_=ps[:, 0:NCc], func=AF.Silu, bias=b1[:, hc:hc + 1]), r=[ps, b1], w=[hidT])
            if "dbg_kcmp" in A:
                hf = S.sb([128, 2, 512], F32, "hf")
                S.op("dve", lambda e: e.tensor_copy(out=hf[:], in_=hidT[:]), r=[hidT], w=[hf])
                S.dma("sp", A["dbg_hid"][which], hf[:], r=[hf], is_output=True)
                S.dma("sp", A["dbg_b1"][which], b1[:], r=[b1], is_output=True)
            if which == 0:
                ktok = S.sb([128, 4, 64], BF16, "ktok")
                S.op("pool", lambda e: e.memset(ktok[:], 0.0), w=[ktok])
                for cb in range(NCB):
                    nb = min(128, NCc - cb * 128)
                    for hc in range(2):
                        S.op("pe", lambda e, hc=hc, cb=cb, nb=nb: e.matmul(ps_m[0:nb, 0:64], lhsT=hidT[:, hc, cb * 128:cb * 128 + nb], rhs=W2[:, hc, :], start=(hc == 0), stop=(hc == 1)), r=[W2, hidT], w=[ps_m])
                    S.op("act", lambda e, cb=cb, nb=nb: e.activation(out=ktok[0:nb, cb, :], in_=ps_m[0:nb, 0:64], func=AF.Copy), r=[ps_m], w=[ktok])
                for cb in range(4):
                    S.op("pe", lambda e, cb=cb: e.transpose(out=ps_t[0:64, cb * 128:(cb + 1) * 128], in_=ktok[:, cb, :], identity=ident[:]), r=[ktok, ident], w=[ps_t])
                kb_ = S.sb([64, 512], BF16, "kb_"); u1 = S.sb([64, 512], F32, "u1"); u2 = S.sb([64, 512], F32, "u2")
                S.op("act", lambda e: e.activation(out=kb_[:], in_=ps_t[0:64, :], func=AF.Copy), r=[ps_t], w=[kb_])
                S.op("pe", lambda e: e.matmul(ps_s[0][:, :], lhsT=pswap[0:64, :], rhs=kb_[:], start=True, stop=True), r=[pswap, kb_], w=[ps_s[0]])
                S.op("dve", lambda e: e.tensor_mul(out=u1[:], in0=kb_[:], in1=Cc[0:64, :]), r=[kb_, Cc], w=[u1])
                S.op("dve", lambda e: e.tensor_mul(out=u2[:], in0=ps_s[0][0:64, :], in1=Sc[0:64, :]), r=[ps_s[0], Sc], w=[u2])
                S.op("pool", lambda e: e.tensor_add(out=kcmpT[:, 0:NCc], in0=u1[:, 0:NCc], in1=u2[:, 0:NCc]), r=[u1, u2], w=[kcmpT])
                if "dbg_kcmp" in A:
                    S.dma("sp", A["dbg_u1"], u1[:], r=[u1], is_output=True)
                    S.dma("sp", A["dbg_u2"], u2[:], r=[u2], is_output=True)
                    S.dma("sp", A["dbg_cc"], Cc[0:64, :], r=[Cc], is_output=True)
                    S.dma("sp", A["dbg_sc"], Sc[0:64, :], r=[Sc], is_output=True)
            else:
                for cb in range(NCB):
                    nb = min(128, NCc - cb * 128)
                    for hc in range(2):
                        S.op("pe", lambda e, hc=hc, cb=cb, nb=nb: e.matmul(ps_m[0:nb, 0:64], lhsT=hidT[:, hc, cb * 128:cb * 128 + nb], rhs=W2[:, hc, :], start=(hc == 0), stop=(hc == 1)), r=[W2, hidT], w=[ps_m])
                    S.op("act", lambda e, cb=cb, nb=nb: e.activation(out=vcmp[0:nb, cb, 0:64], in_=ps_m[0:nb, 0:64], func=AF.Copy), r=[ps_m], w=[vcmp])
            S.barrier()
        for _w in (0, 1):
            _cmp(_w)
        S.stack = old
    if "dbg_kcmp" in A:
        kcf = S.sb([64, 512], F32, "kcf"); vcf = S.sb([128, 4, 65], F32, "vcf")
        S.op("dve", lambda e: e.tensor_copy(out=kcf[:], in_=kcmpT[:]), r=[kcmpT], w=[kcf])
        S.op("dve", lambda e: e.tensor_copy(out=vcf[:], in_=vcmp[:]), r=[vcmp], w=[vcf])
        S.dma("sp", A["dbg_kcmp"], kcf[:], r=[kcf], is_output=True)
        S.dma("sp", A["dbg_vcmp"], vcf[:], r=[vcf], is_output=True)
    qn = [S.sb([64, T], BF16, "qn") for _ in range(4)]
    for i in range(4):
        S.dma("sp", qn[i][:], A["qnT"][i], w=[qn[i]])
    ksT = S.sb([64, T], BF16, "ksT"); kwT = S.sb([64, T], BF16, "kwT")
    S.dma("sp", ksT[:], A["ksT"], w=[ksT]); S.dma("sp", kwT[:], A["kwT"], w=[kwT])
    vsa = S.sb([128, NKB, 65], BF16, "vsa"); vwa = S.sb([128, NKB, 65], BF16, "vwa")
    S.op("pool", lambda e: e.memset(vsa[:], 1.0), w=[vsa]); S.op("pool", lambda e: e.memset(vwa[:], 1.0), w=[vwa])
    S.dma("sp", vsa[:, :, 0:64], A["vs"].rearrange("(kb p) d -> p kb d", p=128), w=[vsa])
    S.dma("sp", vwa[:, :, 0:64], A["vw"].rearrange("(kb p) d -> p kb d", p=128), w=[vwa])
    madd = S.sb([128, 512], F32, "madd")
    sm = [S.sb([128, 512], F32, "sm") for _ in range(2)]
    P = [S.sb([128, 512], F32, "P") for _ in range(2)]
    Pb = [S.sb([128, 512], BF16, "Pb") for _ in range(2)]
    PT = [S.sb([128, 512], BF16, "PT") for _ in range(2)]
    lc = S.sb([128, 4], F32, "lc"); rlc = S.sb([128, 4], F32, "rlc")
    Ps4 = S.sb([128, 512], F32, "Ps4")
    imp = S.sb([128, 128], F32, "imp")
    v_ = S.sb([128, 128], F32, "v_"); f_ = S.sb([128, 128], F32, "f_"); vf_ = S.sb([128, 128], F32, "vf_"); ad_ = S.sb([128, 128], F32, "ad_")
    score = S.sb([128, 128], F32, "score"); sc2 = S.sb([128, 128], F32, "sc2"); m8 = S.sb([128, 16], F32, "m8")
    selb = S.sb([128, 128], BF16, "selb")
    selT = [S.sb([128, 512], BF16, "selT") for _ in range(2)]
    ocs = [S.sb([128, 4, 2, 65], F32, "ocs") for _ in range(2)]
    pT = [S.sb([128, 512], BF16, "pT") for _ in range(3)]
    oss = S.sb([128, 4, 65], F32, "oss")
    glb = S.sb([128, 4, 6], BF16, "glb"); gg = S.sb([128, 4, 6], F32, "gg")
    ww = S.sb([128, 4, 3], F32, "ww")
    oacc = S.sb([128, 4, 64], F32, "oacc"); ob = S.sb([128, 4, 64], BF16, "ob")
    ost = [S.sb([64, 512], BF16, "ost") for _ in range(2)]
    itc = [0]
    def _qt(qt):
        qsl = slice(qt * 512, (qt + 1) * 512)
        oc_t = ocs[qt % 2]; sT = selT[qt % 2]
        def _sub(sub):
            qs = qt * 4 + sub
            q1 = slice(qs * 128, (qs + 1) * 128)
            S.op("dve", lambda e, qs=qs: e.tensor_scalar(out=madd[:], in0=mbase[:], scalar1=float(128 * qs), scalar2=NEG, op0=ALU.is_gt, op1=ALU.mult), r=[mbase], w=[madd])
            for i in range(4):
                itc[0] += 1; it = itc[0]
                ps = ps_s[it % 2]; s_ = sm[it % 2]; p_ = P[it % 2]
                S.op("pe", lambda e, ps=ps, i=i, q1=q1: e.matmul(ps[:], lhsT=qn[i][:, q1], rhs=kcmpT[:], start=True, stop=True), r=[qn[i], kcmpT], w=[ps])
                S.op("dve", lambda e, ps=ps, s_=s_: e.scalar_tensor_tensor(out=s_[:], in0=ps[:], scalar=0.125, in1=madd[:], op0=ALU.mult, op1=ALU.add), r=[ps, madd], w=[s_])
                S.op("act", lambda e, s_=s_, p_=p_, i=i: e.activation(out=p_[:], in_=s_[:], func=AF.Exp, accum_out=lc[:, i:i + 1]), r=[s_], w=[p_, lc])
                S.op("dve", lambda e, i=i: e.tensor_single_scalar(out=rlc[:, i:i + 1], in_=lc[:, i:i + 1], scalar=1e-30, op=ALU.max), r=[lc], w=[rlc])
                S.op("dve", lambda e, i=i: e.reciprocal(out=rlc[:, i:i + 1], in_=rlc[:, i:i + 1]), r=[rlc], w=[rlc])
                if i == 0:
                    S.op("dve", lambda e, p_=p_, i=i: e.tensor_scalar(out=Ps4[:], in0=p_[:], scalar1=rlc[:, i:i + 1], scalar2=None, op0=ALU.mult, op1=ALU.bypass), r=[p_, rlc], w=[Ps4])
                else:
                    S.op("dve", lambda e, p_=p_, i=i: e.scalar_tensor_tensor(out=Ps4[:], in0=p_[:], scalar=rlc[:, i:i + 1], in1=Ps4[:], op0=ALU.mult, op1=ALU.add), r=[p_, rlc, Ps4], w=[Ps4])
                if i < 2:
                    pb = Pb[i]; pt = PT[i]
                    S.op("pool", lambda e, p_=p_, pb=pb: e.tensor_copy(out=pb[:], in_=p_[:]), r=[p_], w=[pb])
                    for cb in range(NCB):
                        S.op("pe", lambda e, pb=pb, cb=cb: e.transpose(out=ps_t[:, cb * 128:(cb + 1) * 128], in_=pb[:, cb * 128:(cb + 1) * 128], identity=ident[:]), r=[pb, ident], w=[ps_t])
                    S.op("act", lambda e, pt=pt: e.activation(out=pt[:, 0:NCB * 128], in_=ps_t[:, 0:NCB * 128], func=AF.Copy), r=[ps_t], w=[pt])
                    for cb in range(NCB):
                        S.op("pe", lambda e, pt=pt, cb=cb, i=i: e.matmul(ps_oc[:, i, :], lhsT=pt[:, cb * 128:(cb + 1) * 128], rhs=vcmp[:, cb, :], start=(cb == 0), stop=(cb == NCB - 1)), r=[pt, vcmp], w=[ps_oc])
                    S.op("act", lambda e, i=i, sub=sub, oc_t=oc_t: e.activation(out=oc_t[:, sub, i, :], in_=ps_oc[:, i, :], func=AF.Copy), r=[ps_oc], w=[oc_t])
            S.op("dve", lambda e: e.tensor_reduce(out=imp[:], in_=Ps4[:].rearrange("p (n f) -> p n f", f=4), axis=AX.X, op=ALU.add), r=[Ps4], w=[imp])
            S.op("dve", lambda e: e.tensor_add(out=imp[:, 1:128], in0=imp[:, 1:128], in1=Ps4[:, 3:508:4]), r=[imp, Ps4], w=[imp])
            S.op("dve", lambda e, qs=qs: e.tensor_single_scalar(out=v_[:], in_=dmat[:], scalar=float(2 * qs), op=ALU.is_le), r=[dmat], w=[v_])
            S.op("dve", lambda e, qs=qs: e.scalar_tensor_tensor(out=f_[:], in0=dmat[:], scalar=float(2 * qs - 1), in1=v_[:], op0=ALU.is_ge, op1=ALU.mult), r=[dmat, v_], w=[f_])
            S.op("dve", lambda e: e.tensor_max(out=f_[:], in0=f_[:], in1=col0[:]), r=[f_, col0], w=[f_])
            S.op("dve", lambda e: e.tensor_sub(out=vf_[:], in0=v_[:], in1=f_[:]), r=[v_, f_], w=[vf_])
            S.op("dve", lambda e: e.scalar_tensor_tensor(out=ad_[:], in0=f_[:], scalar=-1.0, in1=v_[:], op0=ALU.add, op1=ALU.add), r=[f_, v_], w=[ad_])
            S.op("dve", lambda e: e.tensor_mul(out=score[:], in0=imp[:], in1=vf_[:]), r=[imp, vf_], w=[score])
            S.op("dve", lambda e: e.scalar_tensor_tensor(out=score[:], in0=ad_[:], scalar=1e9, in1=score[:], op0=ALU.mult, op1=ALU.add), r=[ad_, score], w=[score])
            S.op("dve", lambda e: e.max(out=m8[:, 0:8], in_=score[:]), r=[score], w=[m8])
            S.op("dve", lambda e: e.match_replace(out=sc2[:], in_to_replace=m8[:, 0:8], in_values=score[:], imm_value=-3e9), r=[score, m8], w=[sc2])
            S.op("dve", lambda e: e.max(out=m8[:, 8:16], in_=sc2[:]), r=[sc2, m8], w=[m8])
            S.op("dve", lambda e: e.tensor_scalar(out=sc2[:], in0=score[:], scalar1=m8[:, 15:16], scalar2=None, op0=ALU.is_ge, op1=ALU.bypass), r=[score, m8], w=[sc2])
            S.op("dve", lambda e: e.tensor_scalar(out=selb[:], in0=sc2[:], scalar1=-1.0, scalar2=-NEG, op0=ALU.add, op1=ALU.mult), r=[sc2], w=[selb])
            if "dbg_kcmp" in A and qs == A["dbg_qs"]:
                S.dma("sp", A["dbg_imp"], imp[:], r=[imp], is_output=True)
                S.dma("sp", A["dbg_score"], score[:], r=[score], is_output=True)
                S.dma("sp", A["dbg_sel"], sc2[:], r=[sc2], is_output=True)
                S.dma("sp", A["dbg_m8"], m8[:], r=[m8], is_output=True)
                S.dma("sp", A["dbg_ps4"], Ps4[:], r=[Ps4], is_output=True)
            S.op("pe", lambda e: e.transpose(out=ps_t[:, 0:128], in_=selb[:], identity=ident[:]), r=[selb, ident], w=[ps_t])
            S.op("act", lambda e, sub=sub, sT=sT: e.activation(out=sT[:, sub * 128:(sub + 1) * 128], in_=ps_t[:, 0:128], func=AF.Copy), r=[ps_t], w=[sT])
        for _s in range(4):
            _sub(_s)
        S.dma("sp", glb[:], A["gl"][qsl, :].rearrange("(s p) c -> p s c", p=128), w=[glb], allow_slow_non_contiguous=True)
        S.op("act", lambda e: e.activation(out=gg[:], in_=glb[:], func=AF.Exp, scale=-1.0), r=[glb], w=[gg])
        S.op("dve", lambda e: e.tensor_single_scalar(out=gg[:], in_=gg[:], scalar=1.0, op=ALU.add), r=[gg], w=[gg])
        S.op("dve", lambda e: e.reciprocal(out=gg[:], in_=gg[:]), r=[gg], w=[gg])
        def _head(i):
            first_o = True
            for kb in range(4 * qt + 4):
                d = kb - 4 * qt
                itc[0] += 1; it = itc[0]
                ps = ps_s[it % 2]; p_t = pT[it % 3]
                S.op("pe", lambda e, ps=ps, kb=kb, i=i: e.matmul(ps[:], lhsT=ksT[:, kb * 128:(kb + 1) * 128], rhs=qn[i][:, qsl], start=True, stop=False), r=[ksT, qn[i]], w=[ps])
                S.op("pe", lambda e, ps=ps, kb=kb, d=d: e.matmul(ps[:], lhsT=eall[:, kb * 128:(kb + 1) * 128], rhs=sT[:], start=False, stop=(d < 0)), r=[eall, sT], w=[ps])
                if d >= 0:
                    S.op("pe", lambda e, ps=ps, d=d: e.matmul(ps[:], lhsT=ident[:], rhs=mle[:, d, :], start=False, stop=True), r=[ident, mle], w=[ps])
                S.op("act", lambda e, ps=ps, p_t=p_t: e.activation(out=p_t[:], in_=ps[:], func=AF.Exp, scale=0.125), r=[ps], w=[p_t])
                for sub in range(max(d, 0), 4):
                    S.op("pe", lambda e, sub=sub, p_t=p_t, kb=kb, st=first_o: e.matmul(ps_os[:, sub, :], lhsT=p_t[:, sub * 128:(sub + 1) * 128], rhs=vsa[:, kb, :], start=st, stop=(kb == 4 * qt + sub), skip_group_check=True), r=[p_t, vsa], w=[ps_os])
                    first_o = False
            S.op("act", lambda e: e.activation(out=oss[:], in_=ps_os[:], func=AF.Copy), r=[ps_os], w=[oss])
            first_o = True
            for kb in range(max(0, 4 * qt - 4), 4 * qt + 4):
                d = kb - 4 * qt
                itc[0] += 1; it = itc[0]
                ps = ps_s[it % 2]; p_t = pT[it % 3]
                S.op("pe", lambda e, ps=ps, kb=kb, i=i: e.matmul(ps[:], lhsT=kwT[:, kb * 128:(kb + 1) * 128], rhs=qn[i][:, qsl], start=True, stop=False), r=[kwT, qn[i]], w=[ps])
                mk = mle[:, d, :] if d >= 0 else mwin[:, d + 4, :]
                S.op("pe", lambda e, ps=ps, mk=mk: e.matmul(ps[:], lhsT=ident[:], rhs=mk, start=False, stop=True), r=[ident, mle, mwin], w=[ps])
                S.op("act", lambda e, ps=ps, p_t=p_t: e.activation(out=p_t[:], in_=ps[:], func=AF.Exp, scale=0.125), r=[ps], w=[p_t])
                subs = range(d, 4) if d >= 0 else range(0, d + 5)
                for sub in subs:
                    last_kb = 4 * qt + sub
                    S.op("pe", lambda e, sub=sub, p_t=p_t, kb=kb, st=first_o, last_kb=last_kb: e.matmul(ps_ow[:, sub, :], lhsT=p_t[:, sub * 128:(sub + 1) * 128], rhs=vwa[:, kb, :], start=st, stop=(kb == last_kb), skip_group_check=True), r=[p_t, vwa], w=[ps_ow])
                    first_o = False
            S.op("dve", lambda e, i=i: e.tensor_single_scalar(out=ww[:, :, 0], in_=oc_t[:, :, i, 64], scalar=1e-30, op=ALU.max), r=[oc_t], w=[ww])
            S.op("dve", lambda e: e.tensor_copy(out=ww[:, :, 1], in_=oss[:, :, 64]), r=[oss, ww], w=[ww])
            S.op("dve", lambda e: e.tensor_copy(out=ww[:, :, 2], in_=ps_ow[:, :, 64]), r=[ps_ow, ww], w=[ww])
            S.op("dve", lambda e: e.reciprocal(out=ww[:], in_=ww[:]), r=[ww], w=[ww])
            S.op("dve", lambda e, i=i: e.tensor_mul(out=ww[:], in0=ww[:], in1=gg[:, :, i * 3:(i + 1) * 3]), r=[ww, gg], w=[ww])
            if "dbg_kcmp" in A and qt == A["dbg_qs"] // 4 and i == 0:
                owf = S.sb([128, 4, 65], F32, "owf")
                S.op("dve", lambda e: e.tensor_copy(out=owf[:], in_=ps_ow[:]), r=[ps_ow], w=[owf])
                S.dma("sp", A["dbg_ow"], owf[:], r=[owf], is_output=True)
                S.dma("sp", A["dbg_os"], oss[:], r=[oss], is_output=True)
                S.dma("sp", A["dbg_oc"], oc_t[:], r=[oc_t], is_output=True)
                S.dma("sp", A["dbg_ww"], ww[:], r=[ww], is_output=True)
            for sub in range(4):
                S.op("dve", lambda e, sub=sub, i=i: e.tensor_scalar(out=oacc[:, sub, :], in0=oc_t[:, sub, i, 0:64], scalar1=ww[:, sub, 0:1], scalar2=None, op0=ALU.mult, op1=ALU.bypass), r=[oc_t, ww], w=[oacc])
                S.op("dve", lambda e, sub=sub: e.scalar_tensor_tensor(out=oacc[:, sub, :], in0=oss[:, sub, 0:64], scalar=ww[:, sub, 1:2], in1=oacc[:, sub, :], op0=ALU.mult, op1=ALU.add), r=[oss, ww, oacc], w=[oacc])
                S.op("dve", lambda e, sub=sub: e.scalar_tensor_tensor(out=ob[:, sub, :], in0=ps_ow[:, sub, 0:64], scalar=ww[:, sub, 2:3], in1=oacc[:, sub, :], op0=ALU.mult, op1=ALU.add), r=[ps_ow, ww, oacc], w=[ob])
            for sub in range(4):
                S.op("pe", lambda e, sub=sub: e.transpose(out=ps_t[0:64, sub * 128:(sub + 1) * 128], in_=ob[:, sub, :], identity=ident[:]), r=[ob, ident], w=[ps_t])
            o_s = ost[(qt * 2 + i) % 2]
            S.op("act", lambda e, o_s=o_s: e.activation(out=o_s[:], in_=ps_t[0:64, :], func=AF.Copy), r=[ps_t], w=[o_s])
            S.dma("sp", A["oT"][i * 64:(i + 1) * 64, qsl], o_s[:], r=[o_s], is_output=True)
        for _i in range(2):
            _head(_i)
    for _q in range(NQT):
        _qt(_q)


NSA_IN = dict(qnT=lambda T: ([4, 64, T], BF16), kcT=lambda T: ([64, T], BF16), vcT=lambda T: ([64, T], BF16),
              ksT=lambda T: ([64, T], BF16), kwT=lambda T: ([64, T], BF16), vs=lambda T: ([T, 64], BF16), vw=lambda T: ([T, 64], BF16),
              gl=lambda T: ([T, 6], BF16), posc=lambda T: ([1, 512], I32),
              w1k=lambda T: ([2048, 256], F32), w1v=lambda T: ([2048, 256], F32), w2k=lambda T: ([256, 64], F32), w2v=lambda T: ([256, 64], F32),
              posk=lambda T: ([32, 64], F32), posv=lambda T: ([32, 64], F32))


def load_csts_extra(S, nc, arrs):
    out = {}
    for n, arr in arrs.items():
        dt = BF16 if arr.dtype == NPBF else F32
        ap = din(nc, "c_" + n, arr.shape, dt)
        t = S.sb(list(arr.shape), dt, n)
        S.dma("sp", t[:], ap, w=[t])
        out[n] = t
    return out, {"c_" + n: a for n, a in arrs.items()}


def build_nsa(T, dbg_qs=None):
    nc = new_nc()
    A = {}
    if dbg_qs is not None:
        A["dbg_qs"] = dbg_qs
        for n, shp in dict(dbg_kcmp=[64, 512], dbg_vcmp=[128, 4, 65], dbg_imp=[128, 128], dbg_score=[128, 128], dbg_sel=[128, 128], dbg_m8=[128, 16], dbg_ps4=[128, 512],
                           dbg_hid=[2, 128, 2, 512], dbg_b1=[2, 128, 2], dbg_u1=[64, 512], dbg_u2=[64, 512], dbg_cc=[64, 512], dbg_sc=[64, 512], dbg_ow=[128, 4, 65], dbg_os=[128, 4, 65], dbg_oc=[128, 4, 2, 65], dbg_ww=[128, 4, 3]).items():
            A[n] = dout(nc, n, shp, F32)
    for n, f in NSA_IN.items():
        shp, dt = f(T)
        A[n] = din(nc, n, shp, dt)
    A["oT"] = dout(nc, "oT", [128, T], BF16)
    with ExitStack() as st:
        S = Sched(nc, st)
        cst, cmap = load_csts(S, nc, ["ident", "pswap", "ropec", "mle", "mwin"])
        c2, cmap2 = load_csts_extra(S, nc, nsa_consts(T))
        cst.update(c2); cmap.update(cmap2)
        emit_nsa(S, T, A, cst)
        S.finish(); S.emit()
    return nc, cmap


def build_mod():
    nc = new_nc()
    cT = din(nc, "cT", [128, 8, 2], F32); w = din(nc, "w", [2, D, 768], F32); bias = din(nc, "bias", [2, 768], F32)
    out = dout(nc, "modp", [2, 2, 768], F32)
    with ExitStack() as st:
        S = Sched(nc, st)
        ct = S.sb([128, 8, 2], F32, "ct"); cond = S.sb([128, 8, 2], F32, "cond")
        S.dma("sp", ct[:], cT, w=[ct])
        S.op("act", lambda e: e.activation(out=cond[:], in_=ct[:], func=AF.Silu), r=[ct], w=[cond])
        ps = [S.ps([128, 512], F32, "ps") for _ in range(2)]
        for l in range(2):
            W = S.sb([128, 8, 768], F32, "W")
            S.dma("sp", W[:], w[l].rearrange("(k p) c -> p k c", p=128), w=[W])
            bt = S.sb([2, 768], F32, "bt")
            S.dma("sp", bt[:], bias[l:l + 1, :].partition_broadcast(2), w=[bt])
            ot = S.sb([2, 768], F32, "ot")
            for half in range(2):
                p = ps[half]
                for k in range(8):
                    S.op("pe", lambda e, p=p, k=k, half=half, W=W: e.matmul(p[0:2, 0:384], lhsT=cond[:, k, :], rhs=W[:, k, half * 384:(half + 1) * 384], start=(k == 0), stop=(k == 7)), r=[cond, W], w=[p])
                S.op("dve", lambda e, p=p, half=half, ot=ot, bt=bt: e.tensor_add(out=ot[:, half * 384:(half + 1) * 384], in0=p[0:2, 0:384], in1=bt[:, half * 384:(half + 1) * 384]), r=[p, bt], w=[ot])
            S.dma("sp", out[l], ot[:], r=[ot], is_output=True)
        S.finish(); S.emit()
    return nc, {}


def _run(nc, in_maps):
    res = run_bass_kernel_spmd(nc, in_maps, core_ids=list(range(8)))
    return res.results


def kernel(x, c, positions, mod_w, mod_b, norm_mix, norm_ffn, ffn_w_gate, ffn_w_up, ffn_conv_w, ffn_conv_b, ffn_w_down,
           hyb_w_in, nsa_pos_k, nsa_pos_v, nsa_ck_w1, nsa_ck_w2, nsa_cv_w1, nsa_cv_w2, hyb_w_out, diff_w_qkv,
           diff_lq1, diff_lk1, diff_lq2, diff_lk2, diff_subln, diff_w_out, norm_f):
    f32 = lambda a: np.ascontiguousarray(np.asarray(a), dtype=np.float32)
    x = f32(x); c = f32(c); positions = np.ascontiguousarray(np.asarray(positions), dtype=np.int32)
    B, T, _ = x.shape
    TL = T // 4
    ca = np.ascontiguousarray
    nc, cm = build_mod()
    cT = ca(c.T.reshape(8, 128, 2).transpose(1, 0, 2))
    mw = f32(mod_w); mb = f32(mod_b)
    r = _run(nc, [dict(cT=cT, w=ca(mw[:, :, i * 768:(i + 1) * 768]), bias=ca(mb[:, i * 768:(i + 1) * 768])) for i in range(8)])
    mod = np.concatenate([r[i]["modp"] for i in range(8)], axis=-1)
    sh_m, sc_m, g_m, sh_f, sc_f, g_f = [mod[..., k * D:(k + 1) * D] for k in range(6)]
    nmix = f32(norm_mix); nffn = f32(norm_ffn)

    def run_pre(layer, xin, w):
        nc, cm = build_pre(TL, layer)
        maps = []
        for i in range(8):
            b, j = i // 4, i % 4
            maps.append(dict(x=ca(xin[b, j * TL:(j + 1) * TL]), pos=ca(positions[b:b + 1, j * TL:(j + 1) * TL]),
                             mv=ca(np.stack([nmix[layer], sc_m[layer, b], sh_m[layer, b]])), w=w, **cm))
        r = _run(nc, maps)
        FM = [np.concatenate([r[b * 4 + j]["fm"] for j in range(4)], axis=2) for b in range(B)]
        TM = [np.concatenate([r[b * 4 + j]["tm"] for j in range(4)], axis=0) for b in range(B)]
        return FM, TM

    def run_post(layer, OT, xin, wo, final):
        nc, cm = build_post(TL, final)
        maps = []
        for i in range(8):
            b, j = i // 4, i % 4
            oT = np.zeros((D, TL + 2), NPBF); xe = np.zeros((TL + 2, D), np.float32)
            lo = j * TL - 2
            if j > 0:
                oT[:] = OT[b][:, lo:lo + TL + 2]; xe[:] = xin[b, lo:lo + TL + 2]
            else:
                oT[:, 2:] = OT[b][:, 0:TL]; xe[2:] = xin[b, 0:TL]
            m = dict(oT=oT, x=xe, wo=wo, mvec=ca(np.stack([g_m[layer, b], g_f[layer, b]])),
                     mvf=ca(np.stack([nffn[layer], sc_f[layer, b], sh_f[layer, b]])),
                     wg=f32(ffn_w_gate[layer]), wu=f32(ffn_w_up[layer]), wd=f32(ffn_w_down[layer]),
                     cw=f32(ffn_conv_w[layer]), cb=f32(ffn_conv_b[layer])[None, :], flag=np.array([[1.0 if j > 0 else 0.0]], np.float32), **cm)
            if final:
                m["fg"] = f32(norm_f)[None, :]
            maps.append(m)
        r = _run(nc, maps)
        return np.stack([np.concatenate([r[b * 4 + j]["xo"] for j in range(4)], axis=0) for b in range(B)])

    FM, TM = run_pre(0, x, f32(hyb_w_in[0]))
    nc, cm = build_nsa(T)
    maps = []
    NCc = (T - 32) // 16 + 1
    for i in range(8):
        b, hg = i // 4, i % 4
        g = hg // 2
        own = [2 * hg, 2 * hg + 1]
        order = own + [h for h in range(4 * g, 4 * g + 4) if h not in own]
        posc = np.zeros((1, 512), np.int32); posc[0, :NCc] = positions[b, 31::16][:NCc]
        gs = slice(64 * g, 64 * g + 64)
        maps.append(dict(qnT=ca(np.stack([FM[b][h // 2, (h % 2) * 64:(h % 2) * 64 + 64] for h in order])),
                         kcT=ca(FM[b][4, gs]), vcT=ca(FM[b][5, gs]), ksT=ca(FM[b][6, gs]), kwT=ca(FM[b][7, gs]),
                         vs=ca(TM[b][:, 64 * g:64 * g + 64]), vw=ca(TM[b][:, 128 + 64 * g:128 + 64 * g + 64]),
                         gl=ca(TM[b][:, 256 + 3 * own[0]:256 + 3 * own[0] + 6]), posc=posc,
                         w1k=f32(nsa_ck_w1[0]), w1v=f32(nsa_cv_w1[0]), w2k=f32(nsa_ck_w2[0]), w2v=f32(nsa_cv_w2[0]),
                         posk=f32(nsa_pos_k[0]), posv=f32(nsa_pos_v[0]), **cm))
    r_nsa = _run(nc, maps)
    nc, cm = build_sb(T)
    maps = []
    for i in range(8):
        b, hg = i // 4, i % 4
        maps.append(dict(qT=ca(FM[b][8 + hg].reshape(2, 64, T)), kT=ca(FM[b][12 + hg].reshape(2, 64, T)),
                         v=ca(TM[b][:, 280 + 128 * hg:280 + 128 * hg + 128]), **cm))
    r_sb = _run(nc, maps)
    OT = []
    for b in range(B):
        OT.append(np.concatenate([r_nsa[b * 4 + hg]["oT"] for hg in range(4)] + [r_sb[b * 4 + hg]["oT"] for hg in range(4)], axis=0))
    x1 = run_post(0, OT, x, f32(hyb_w_out[0]), False)
    FM, TM = run_pre(1, x1, f32(diff_w_qkv[0]))
    nc, cm = build_diff(T)
    lam = ca(np.stack([f32(diff_lq1[0]), f32(diff_lk1[0]), f32(diff_lq2[0]), f32(diff_lk2[0])]))
    maps = []
    for i in range(8):
        b, hg = i // 4, i % 4
        maps.append(dict(qT=ca(FM[b][2 * hg:2 * hg + 2].reshape(4, 64, T)), kT=ca(FM[b][8 + 2 * hg:8 + 2 * hg + 2].reshape(4, 64, T)),
                         v=ca(TM[b][:, 256 * hg:256 * hg + 256]), lam=lam, subln=f32(diff_subln[0])[None, :], **cm))
    r_d = _run(nc, maps)
    OT = [np.concatenate([r_d[b * 4 + hg]["oT"] for hg in range(4)], axis=0) for b in range(B)]
    out = run_post(1, OT, x1, f32(diff_w_out[0]), True)
    return out.astype(np.float32)
```

```python
import math
from contextlib import ExitStack
import numpy as np
import ml_dtypes
import concourse.bass as bass
import concourse.mybir as mybir
from concourse.bass_utils import run_bass_kernel_spmd

F32 = mybir.dt.float32
BF16 = mybir.dt.bfloat16
I32 = mybir.dt.int32
AF = mybir.ActivationFunctionType
ALU = mybir.AluOpType
AX = mybir.AxisListType
NPBF = ml_dtypes.bfloat16

SAME_ENGINE_SYNC = True
N_DMA_SEMS = 16


class Res:
    __slots__ = ("name", "w", "rs")

    def __init__(self, name):
        self.name = name
        self.w = None
        self.rs = []


class Tile:
    def __init__(self, h, name):
        self.h = h
        self.r = Res(name)
        self._subs = {}
        self.name = name

    def __getitem__(self, k):
        return self.h[k]

    def sub(self, key):
        s = self._subs.get(key)
        if s is None:
            s = Res(f"{self.name}/{key}")
            self._subs[key] = s
        return s


class Sched:
    ENG = ("pe", "act", "dve", "pool", "sp")

    def __init__(self, nc, stack):
        self.nc = nc
        self.stack = stack
        self.ops = {e: [] for e in self.ENG}
        self.cnt = {e: 0 for e in self.ENG}
        self.known = {e: {} for e in self.ENG}
        self.dma_cnt = [0] * N_DMA_SEMS
        self.dma_rr = 0
        self.sems = {}
        for e in ("pe", "act", "dve", "pool"):
            self.sems[e] = stack.enter_context(nc.semaphore("s_" + e))
        for i in range(N_DMA_SEMS):
            self.sems[("d", i)] = stack.enter_context(nc.semaphore(f"s_d{i}"))
        self.out_events = []
        self.n_names = 0

    def sb(self, shape, dtype, name=None):
        self.n_names += 1
        name = f"{name or 't'}_{self.n_names}"
        h = self.stack.enter_context(self.nc.sbuf_tensor(name, list(shape), dtype))
        return Tile(h, name)

    def ps(self, shape, dtype, name=None):
        self.n_names += 1
        name = f"{name or 'p'}_{self.n_names}"
        h = self.stack.enter_context(self.nc.psum_tensor(name, list(shape), dtype))
        return Tile(h, name)

    def _deps(self, eng, r, w):
        deps = {}
        def add(ev):
            if ev is None:
                return
            k, v = ev
            if deps.get(k, 0) < v:
                deps[k] = v
        for x in r:
            add(x.w)
        for x in w:
            add(x.w)
            for ev in x.rs:
                add(ev)
        waits = []
        kn = self.known[eng]
        for k, v in deps.items():
            if k == eng and (eng == "pe" or not SAME_ENGINE_SYNC):
                continue
            if kn.get(k, 0) >= v:
                continue
            kn[k] = v
            waits.append((k, v))
        return waits

    def _commit(self, ev, r, w):
        for x in r:
            x.rs.append(ev)
        for x in w:
            x.w = ev
            x.rs = []

    @staticmethod
    def _res(lst):
        out = []
        for x in lst:
            out.append(x.r if isinstance(x, Tile) else x)
        return out

    def op(self, eng, fn, r=(), w=()):
        r = self._res(r)
        w = self._res(w)
        waits = self._deps(eng, r, w)
        self.cnt[eng] += 1
        ev = (eng, self.cnt[eng])
        self.ops[eng].append((waits, fn, (eng, 1)))
        self._commit(ev, r, w)
        return ev

    def dma(self, eng, out, in_, r=(), w=(), is_output=False, **kw):
        r = self._res(r)
        w = self._res(w)
        waits = self._deps(eng, r, w)
        si = self.dma_rr
        self.dma_rr = (self.dma_rr + 1) % N_DMA_SEMS
        key = ("d", si)
        prev = 16 * self.dma_cnt[si]
        kn = self.known[eng]
        if prev > 0 and kn.get(key, 0) < prev:
            kn[key] = prev
            waits.append((key, prev))
        self.dma_cnt[si] += 1
        ev = (key, 16 * self.dma_cnt[si])
        def fn(e, out=out, in_=in_, kw=kw):
            return e.dma_start(out=out, in_=in_, **kw)
        self.ops[eng].append((waits, fn, (key, 16)))
        self._commit(ev, r, w)
        if is_output:
            self.out_events.append(ev)
        return ev

    def dma_fn(self, eng, fn, r=(), w=(), is_output=False):
        r = self._res(r); w = self._res(w)
        waits = self._deps(eng, r, w)
        si = self.dma_rr
        self.dma_rr = (self.dma_rr + 1) % N_DMA_SEMS
        key = ("d", si)
        prev = 16 * self.dma_cnt[si]
        kn = self.known[eng]
        if prev > 0 and kn.get(key, 0) < prev:
            kn[key] = prev
            waits.append((key, prev))
        self.dma_cnt[si] += 1
        ev = (key, 16 * self.dma_cnt[si])
        self.ops[eng].append((waits, fn, (key, 16)))
        self._commit(ev, r, w)
        if is_output:
            self.out_events.append(ev)
        return ev

    def cc(self, kind, ins, outs, groups, r=(), w=()):
        def fn(e):
            return e.collective_compute(kind, ALU.bypass, replica_groups=groups, ins=list(ins), outs=list(outs))
        return self.dma_fn("pool", fn, r=r, w=w)

    def barrier(self):
        evs = [(e, self.cnt[e]) for e in ("pe", "act", "dve", "pool") if self.cnt[e] > 0]
        evs += [(("d", i), 16 * self.dma_cnt[i]) for i in range(N_DMA_SEMS) if self.dma_cnt[i] > 0]
        for eng in self.ENG:
            kn = self.known[eng]
            waits = []
            for (k, v) in evs:
                if kn.get(k, 0) >= v:
                    continue
                kn[k] = v
                waits.append((k, v))
            if waits:
                self.ops[eng].append((waits, None, None))

    def finish(self):
        final = {}
        for (k, v) in self.out_events:
            if final.get(k, 0) < v:
                final[k] = v
        waits = [(k, v) for k, v in final.items()]
        self.ops["sp"].append((waits, None, None))

    def emit(self):
        nc = self.nc
        sems = self.sems
        ops = self.ops
        def run(engname, e):
            for (waits, fn, inc) in ops[engname]:
                for (k, v) in waits:
                    e.wait_ge(sems[k], v)
                if fn is not None:
                    ins = fn(e)
                    ins.then_inc(sems[inc[0]], inc[1])
        with nc.Block() as block:
            @block.tensor
            def _(e):
                run("pe", e)
            @block.scalar
            def _(e):
                run("act", e)
            @block.vector
            def _(e):
                run("dve", e)
            @block.gpsimd
            def _(e):
                run("pool", e)
            @block.sync
            def _(e):
                run("sp", e)


D = 1024
DFF = 2816
NFC = DFF // 128
EPS = 1e-6
NEG = -30000.0
ROPE_THETA = 500000.0
LAMBDA_INIT = 0.8 - 0.6 * math.exp(-0.3 * 1)
C1 = 6.28125
C2 = 2 * math.pi - 6.28125


def new_nc():
    return bass.Bass("TRN2", target_bir_lowering=False)


def din(nc, name, shape, dt):
    return nc.dram_tensor(name, list(shape), dt, kind="ExternalInput").ap()


def dout(nc, name, shape, dt):
    return nc.dram_tensor(name, list(shape), dt, kind="ExternalOutput").ap()


def host_consts():
    c = {}
    c["ident"] = np.eye(128, dtype=np.float32).astype(NPBF)
    p = np.arange(128)
    sw = np.zeros((128, 128), np.float32)
    for m in range(128):
        r = m % 64
        if r < 8:
            sw[m + 8, m] = 1
        elif r < 16:
            sw[m - 8, m] = 1
    c["pswap"] = sw.astype(NPBF)
    rc = np.zeros((128, 2), np.float32)
    for m in range(128):
        r = m % 64
        if r < 16:
            rc[m, 0] = ROPE_THETA ** (-(2 * (r % 8)) / 16.0)
            rc[m, 1] = -1.0 if r < 8 else 1.0
    c["ropec"] = rc
    n = np.arange(512)[None, :]
    pp = p[:, None]
    mle = np.stack([np.where(n >= 128 * d + pp, 0.0, NEG) for d in range(4)])
    mlt = np.stack([np.where(n > 128 * d + pp, 0.0, NEG) for d in range(4)])
    mwin = np.stack([np.where(n < 128 * d + pp, 0.0, NEG) for d in range(4)])
    c["mle"] = mle.astype(NPBF)
    c["mlt"] = mlt.astype(NPBF)
    c["mwin"] = mwin.astype(NPBF)
    tri = np.where(p[:, None] >= p[None, :], -1.0, 0.0)
    c["negtri"] = tri.astype(NPBF)
    c["negones"] = (-np.ones((128, 128), np.float32)).astype(NPBF)
    return c


def rope_tables(S, pos_ap, ntok, ropec, name):
    Ct = S.sb([128, ntok], F32, name + "C")
    St = S.sb([128, ntok], F32, name + "S")
    with ExitStack() as st:
        old = S.stack
        S.stack = st
        posi = S.sb([128, ntok], I32, "posi")
        ang = S.sb([128, ntok], F32, "ang")
        u = S.sb([128, ntok], F32, "u")
        ki = S.sb([128, ntok], I32, "ki")
        kf = S.sb([128, ntok], F32, "kf")
        S.dma("sp", posi[:], pos_ap.partition_broadcast(128), w=[posi])
        S.op("dve", lambda e: e.tensor_copy(out=ang[:], in_=posi[:]), r=[posi], w=[ang])
        S.op("dve", lambda e: e.tensor_scalar(out=ang[:], in0=ang[:], scalar1=ropec[:, 0:1], scalar2=None, op0=ALU.mult, op1=ALU.bypass), r=[ang, ropec], w=[ang])
        for (off, dst, sgn) in ((0.5 * math.pi, Ct, False), (0.0, St, True)):
            S.op("dve", lambda e, off=off: e.tensor_single_scalar(out=u[:], in_=ang[:], scalar=off, op=ALU.add), r=[ang], w=[u])
            S.op("dve", lambda e: e.tensor_single_scalar(out=ki[:], in_=u[:], scalar=1.0 / (2 * math.pi), op=ALU.mult), r=[u], w=[ki])
            S.op("dve", lambda e: e.tensor_copy(out=kf[:], in_=ki[:]), r=[ki], w=[kf])
            S.op("dve", lambda e: e.scalar_tensor_tensor(out=u[:], in0=kf[:], scalar=-C1, in1=u[:], op0=ALU.mult, op1=ALU.add), r=[kf, u], w=[u])
            S.op("dve", lambda e: e.scalar_tensor_tensor(out=u[:], in0=kf[:], scalar=-C2, in1=u[:], op0=ALU.mult, op1=ALU.add), r=[kf, u], w=[u])
            S.op("dve", lambda e: e.tensor_scalar(out=kf[:], in0=u[:], scalar1=math.pi, scalar2=2 * math.pi, op0=ALU.is_gt, op1=ALU.mult), r=[u], w=[kf])
            S.op("dve", lambda e: e.tensor_sub(out=u[:], in0=u[:], in1=kf[:]), r=[u, kf], w=[u])
            S.op("dve", lambda e: e.tensor_scalar(out=u[:], in0=u[:], scalar1=math.pi, scalar2=-math.pi, op0=ALU.min, op1=ALU.max), r=[u], w=[u])
            S.op("act", lambda e, dst=dst: e.activation(out=dst[:], in_=u[:], func=AF.Sin), r=[u], w=[dst])
            if sgn:
                S.op("dve", lambda e, dst=dst: e.tensor_scalar(out=dst[:], in0=dst[:], scalar1=ropec[:, 1:2], scalar2=None, op0=ALU.mult, op1=ALU.bypass), r=[dst, ropec], w=[dst])
        S.barrier()
        S.stack = old
    return Ct, St


def load_const(S, ap, shape, dt, name):
    t = S.sb(shape, dt, name)
    S.dma("sp", t[:], ap, w=[t])
    return t


def rstd_from_ssq(S, ssq, rstd, n, ntok=128, cols=None):
    sl = (slice(0, ntok), slice(None) if cols is None else cols)
    S.op("dve", lambda e: e.tensor_scalar(out=rstd[sl], in0=ssq[sl], scalar1=1.0 / n, scalar2=EPS, op0=ALU.mult, op1=ALU.add), r=[ssq], w=[rstd])
    S.op("act", lambda e: e.activation(out=rstd[sl], in_=rstd[sl], func=AF.Ln), r=[rstd], w=[rstd])
    S.op("act", lambda e: e.activation(out=rstd[sl], in_=rstd[sl], func=AF.Exp, scale=-0.5), r=[rstd], w=[rstd])


def load_weight_bf16(S, Wb, w_ap, nk, ncols, stage_cols=1024, col0=0, name="wst"):
    stg = [S.sb([128, stage_cols], F32, name) for _ in range(2)]
    i = 0
    for k in range(nk):
        for c0 in range(0, ncols, stage_cols):
            cw = min(stage_cols, ncols - c0)
            s = stg[i % 2]
            i += 1
            S.dma("sp", s[:, 0:cw], w_ap[k * 128:(k + 1) * 128, col0 + c0:col0 + c0 + cw], w=[s])
            S.op("pool", lambda e, s=s, k=k, c0=c0, cw=cw: e.tensor_copy(out=Wb[:, k, c0:c0 + cw], in_=s[:, 0:cw]), r=[s], w=[Wb])


def norm_to_hT(S, x_t, ntok, tok0, hT, a_t, sh_t, ident, scr):
    junk, ssq, rstd, xn, pT = scr
    S.op("act", lambda e: e.activation(out=junk[0:ntok, :], in_=x_t[0:ntok, :], func=AF.Square, accum_out=ssq[0:ntok, :]), r=[x_t], w=[junk, ssq])
    rstd_from_ssq(S, ssq, rstd, D, ntok)
    S.op("act", lambda e: e.activation(out=xn[0:ntok, :], in_=x_t[0:ntok, :], func=AF.Copy, scale=rstd[0:ntok, :]), r=[x_t, rstd], w=[xn])
    for k in range(8):
        S.op("pe", lambda e, k=k: e.transpose(out=pT[:, k * 128:k * 128 + ntok], in_=xn[0:ntok, k * 128:(k + 1) * 128], identity=ident[0:ntok, 0:ntok]), r=[xn, ident], w=[pT])
    for k in range(8):
        eng = "dve" if k % 2 == 0 else "pool"
        eng = "dve"
        S.op(eng, lambda e, k=k: e.tensor_scalar(out=hT[:, k, tok0:tok0 + ntok], in0=pT[:, k * 128:k * 128 + ntok], scalar1=a_t[:, k:k + 1], scalar2=sh_t[:, k:k + 1], op0=ALU.mult, op1=ALU.add), r=[pT, a_t, sh_t], w=[hT.sub(tok0)])


def norm_scratch(S):
    return [(S.sb([128, D], BF16, "junk"), S.sb([128, 1], F32, "ssq"), S.sb([128, 1], F32, "rstd"),
             S.sb([128, D], BF16, "xn"), S.ps([128, D], BF16, "pT")) for _ in range(2)]


def mod_vectors(S, mv_ap):
    mv = S.sb([128, 3, 8], F32, "mv")
    S.dma("sp", mv[:], mv_ap.rearrange("r (k p) -> p r k", p=128), w=[mv], allow_slow_non_contiguous=True)
    a = S.sb([128, 8], F32, "a")
    S.op("dve", lambda e: e.tensor_single_scalar(out=a[:], in_=mv[:, 1, :], scalar=1.0, op=ALU.add), r=[mv], w=[a])
    S.op("dve", lambda e: e.tensor_mul(out=a[:], in0=a[:], in1=mv[:, 0, :]), r=[a, mv], w=[a])
    sh = S.sb([128, 8], F32, "sh")
    S.op("dve", lambda e: e.tensor_copy(out=sh[:], in_=mv[:, 2, :]), r=[mv], w=[sh])
    return a, sh


def emit_pre(S, T_loc, colspec, NC, x_ap, pos_ap, mv_ap, w_ap, fm_ap, tm_ap, cst, x_res=None):
    NT = T_loc // 128
    TG = min(512, T_loc)
    NTG = T_loc // TG
    ident, pswap, ropec = cst["ident"], cst["pswap"], cst["ropec"]
    a, sh = mod_vectors(S, mv_ap)
    hT = S.sb([128, 8, T_loc], BF16, "hT")
    Wb = S.sb([128, 8, NC], BF16, "Wb")
    need_rope = any(c.get("rope") for c in colspec)
    if need_rope:
        Ct, St = rope_tables(S, pos_ap, T_loc, ropec, "rp")
    load_weight_bf16(S, Wb, w_ap, 8, NC)
    scr = norm_scratch(S)
    xt = [S.sb([128, D], F32, "xt") for _ in range(2)]
    for tt in range(NT):
        x_t = xt[tt % 2]
        S.dma("sp", x_t[:], x_ap[tt * 128:(tt + 1) * 128, :], r=[x_res] if x_res else [], w=[x_t])
        norm_to_hT(S, x_t, 128, tt * 128, hT, a, sh, ident, scr[tt % 2])
    hT_all = [hT.sub(tt * 128) for tt in range(NT)]
    psA = [S.ps([128, 512], F32, "psA") for _ in range(2)]
    psB = S.ps([128, 512], F32, "psB")
    xb = [S.sb([128, 512], BF16, "xb") for _ in range(2)]
    t1 = [S.sb([128, 512], F32, "t1") for _ in range(2)]
    t2 = [S.sb([128, 512], F32, "t2") for _ in range(2)]
    ob = [S.sb([128, 512], BF16, "ob") for _ in range(3)]
    it = 0
    fmi = 0
    tmoff = 0
    for c in colspec:
        if c["kind"] == "fm":
            c0 = c["col"]
            for tg in range(NTG):
                it += 1
                ps = psA[it % 2]
                tsl = slice(tg * TG, (tg + 1) * TG)
                for k in range(8):
                    S.op("pe", lambda e, ps=ps, k=k, c0=c0, tsl=tsl: e.matmul(ps[:, 0:TG], lhsT=Wb[:, k, c0:c0 + 128], rhs=hT[:, k, tsl], start=(k == 0), stop=(k == 7)),
                         r=[Wb] + hT_all[tg * (TG // 128):(tg + 1) * (TG // 128)], w=[ps])
                o = ob[it % 3]
                if not c.get("rope"):
                    S.op("act", lambda e, ps=ps, o=o, sc=c.get("scale", 1.0): e.activation(out=o[:, 0:TG], in_=ps[:, 0:TG], func=AF.Copy, scale=sc), r=[ps], w=[o])
                else:
                    b = xb[it % 2]; u1 = t1[it % 2]; u2 = t2[it % 2]
                    S.op("act", lambda e, ps=ps, b=b: e.activation(out=b[:, 0:TG], in_=ps[:, 0:TG], func=AF.Copy), r=[ps], w=[b])
                    S.op("pe", lambda e, b=b: e.matmul(psB[:, 0:TG], lhsT=pswap[:], rhs=b[:, 0:TG], start=True, stop=True), r=[b, pswap], w=[psB])
                    S.op("dve", lambda e, b=b, u1=u1, tsl=tsl: e.tensor_mul(out=u1[:, 0:TG], in0=b[:, 0:TG], in1=Ct[:, tsl]), r=[b, Ct], w=[u1])
                    S.op("dve", lambda e, u2=u2, tsl=tsl: e.tensor_mul(out=u2[:, 0:TG], in0=psB[:, 0:TG], in1=St[:, tsl]), r=[psB, St], w=[u2])
                    S.op("pool", lambda e, o=o, u1=u1, u2=u2: e.tensor_add(out=o[:, 0:TG], in0=u1[:, 0:TG], in1=u2[:, 0:TG]), r=[u1, u2], w=[o])
                S.dma("sp", fm_ap[fmi, :, tsl], o[:, 0:TG], r=[o], is_output=True)
            fmi += 1
        else:
            c0, n = c["col"], c["n"]
            for tt in range(NT):
                for cc in range(0, n, 512):
                    cw = min(512, n - cc)
                    it += 1
                    ps = psA[it % 2]
                    for k in range(8):
                        S.op("pe", lambda e, ps=ps, k=k, tt=tt, cc=cc, cw=cw, c0=c0: e.matmul(ps[:, 0:cw], lhsT=hT[:, k, tt * 128:(tt + 1) * 128], rhs=Wb[:, k, c0 + cc:c0 + cc + cw], start=(k == 0), stop=(k == 7)),
                             r=[Wb, hT_all[tt]], w=[ps])
                    o = ob[it % 3]
                    S.op("act", lambda e, ps=ps, o=o, cw=cw: e.activation(out=o[:, 0:cw], in_=ps[:, 0:cw], func=AF.Copy), r=[ps], w=[o])
                    S.dma("sp", tm_ap[tt * 128:(tt + 1) * 128, tmoff + cc:tmoff + cc + cw], o[:, 0:cw], r=[o], is_output=True)
            tmoff += n


def colspec_l0():
    cs = []
    for i in range(4):
        cs.append(dict(kind="fm", col=128 * i, rope=True))
    cs.append(dict(kind="fm", col=512))
    cs.append(dict(kind="fm", col=640))
    cs.append(dict(kind="fm", col=768, rope=True))
    cs.append(dict(kind="fm", col=1024, rope=True))
    for i in range(4):
        cs.append(dict(kind="fm", col=1304 + 128 * i, scale=0.125))
    for i in range(4):
        cs.append(dict(kind="fm", col=1816 + 128 * i))
    cs.append(dict(kind="tm", col=896, n=128))
    cs.append(dict(kind="tm", col=1152, n=128))
    cs.append(dict(kind="tm", col=1280, n=24))
    cs.append(dict(kind="tm", col=2328, n=512))
    return cs, 16, 792


def colspec_l1():
    cs = []
    for i in range(8):
        cs.append(dict(kind="fm", col=128 * i, rope=True))
    for i in range(8):
        cs.append(dict(kind="fm", col=1024 + 128 * i, rope=True))
    cs.append(dict(kind="tm", col=2048, n=1024))
    return cs, 16, 1024


def load_csts(S, nc, names):
    hc = host_consts()
    out = {}
    for n in names:
        arr = hc[n]
        dt = BF16 if arr.dtype == NPBF else F32
        ap = din(nc, "c_" + n, arr.shape, dt)
        if arr.ndim == 3:
            t = S.sb([arr.shape[1], arr.shape[0], arr.shape[2]], dt, n)
            S.dma("sp", t[:], ap.rearrange("d p n -> p d n"), w=[t])
        else:
            t = S.sb(list(arr.shape), dt, n)
            S.dma("sp", t[:], ap, w=[t])
        out[n] = t
    return out, {"c_" + n: hc[n] for n in names}


def build_pre(T_loc, layer):
    cs, nfm, ntm = colspec_l0() if layer == 0 else colspec_l1()
    NC = 2840 if layer == 0 else 3072
    nc = new_nc()
    x = din(nc, "x", [T_loc, D], F32)
    pos = din(nc, "pos", [1, T_loc], I32)
    mv = din(nc, "mv", [3, D], F32)
    w = din(nc, "w", [D, NC], F32)
    fm = dout(nc, "fm", [nfm, 128, T_loc], BF16)
    tm = dout(nc, "tm", [T_loc, ntm], BF16)
    with ExitStack() as st:
        S = Sched(nc, st)
        cst, cmap = load_csts(S, nc, ["ident", "pswap", "ropec"])
        emit_pre(S, T_loc, cs, NC, x, pos, mv, w, fm, tm, cst)
        S.finish(); S.emit()
    return nc, cmap


def emit_post(S, T_loc, oT_ap, x_ap, wo_ap, mvec_ap, mvf_ap, wg_ap, wu_ap, wd_ap, cw_ap, cb_ap, flag_ap, xo_ap, cst, final_g_ap=None):
    TE = T_loc + 2
    NT = T_loc // 128
    ident = cst["ident"]
    tiles = [(0, 2)] + [(2 + 128 * i, 128) for i in range(NT)]
    a_f, sh_f = mod_vectors(S, mvf_ap)
    gm_b = S.sb([128, D], F32, "gm_b"); gf_b = S.sb([128, D], F32, "gf_b")
    S.dma("sp", gm_b[:], mvec_ap[0:1, :].partition_broadcast(128), w=[gm_b])
    S.dma("sp", gf_b[:], mvec_ap[1:2, :].partition_broadcast(128), w=[gf_b])
    if final_g_ap is not None:
        nf_b = S.sb([128, D], F32, "nf_b")
        S.dma("sp", nf_b[:], final_g_ap.partition_broadcast(128), w=[nf_b])
    flag = S.sb([128, 1], F32, "flag")
    S.dma("sp", flag[:], flag_ap.partition_broadcast(128), w=[flag])
    cw = S.sb([128, 3, NFC], F32, "cw"); cb = S.sb([128, NFC], F32, "cb")
    S.dma("sp", cw[:], cw_ap.rearrange("r (c p) -> p r c", p=128), w=[cw], allow_slow_non_contiguous=True)
    S.dma("sp", cb[:], cb_ap.rearrange("r (c p) -> p (r c)", p=128), w=[cb], allow_slow_non_contiguous=True)
    hT = S.sb([128, 8, TE], BF16, "hT")
    psA = [S.ps([128, 512], F32, "psA") for _ in range(2)]
    psB = [S.ps([128, 512], F32, "psB") for _ in range(2)]
    with ExitStack() as st:
        old = S.stack; S.stack = st
        oT = S.sb([128, 8, TE], BF16, "oT")
        S.dma("sp", oT[:], oT_ap.rearrange("(k p) t -> p k t", p=128), w=[oT])
        Wo = S.sb([128, 8, D], BF16, "Wo")
        load_weight_bf16(S, Wo, wo_ap, 8, D)
        scr = norm_scratch(S)
        xt = [S.sb([128, D], F32, "xt") for _ in range(2)]
        tmp = [S.sb([128, D], F32, "tmp") for _ in range(2)]
        for ti, (r0, n) in enumerate(tiles):
            x_t = xt[ti % 2]; t_t = tmp[ti % 2]
            S.dma("sp", x_t[0:n, :], x_ap[r0:r0 + n, :], w=[x_t])
            for half in range(2):
                ps = psA[half]
                for k in range(8):
                    S.op("pe", lambda e, ps=ps, k=k, r0=r0, n=n, half=half: e.matmul(ps[0:n, :], lhsT=oT[:, k, r0:r0 + n], rhs=Wo[:, k, half * 512:(half + 1) * 512], start=(k == 0), stop=(k == 7)), r=[oT, Wo], w=[ps])
                S.op("dve", lambda e, ps=ps, n=n, half=half, t_t=t_t: e.tensor_mul(out=t_t[0:n, half * 512:(half + 1) * 512], in0=ps[0:n, :], in1=gm_b[0:n, half * 512:(half + 1) * 512]), r=[ps, gm_b], w=[t_t])
            S.op("pool", lambda e, n=n, x_t=x_t, t_t=t_t: e.tensor_add(out=x_t[0:n, :], in0=x_t[0:n, :], in1=t_t[0:n, :]), r=[t_t, x_t], w=[x_t])
            if ti > 0:
                S.dma("sp", xo_ap[r0 - 2:r0 - 2 + n, :], x_t[0:n, :], r=[x_t], w=[S_xo(S)], is_output=True)
            norm_to_hT(S, x_t, n, r0, hT, a_f, sh_f, ident, scr[ti % 2])
        S.barrier()
        S.stack = old
    hT_all = [hT.sub(r0) for (r0, n) in tiles]
    actT = S.sb([128, NFC, T_loc], BF16, "actT")
    Wd = S.sb([128, NFC, D], BF16, "Wd")
    with ExitStack() as st:
        old = S.stack; S.stack = st
        wst = [S.sb([128, 8, 256], F32, "wst") for _ in range(1)]
        Wgu = [S.sb([128, 8, 256], BF16, "Wgu") for _ in range(2)]
        gx = [S.sb([128, 514], F32, "gx") for _ in range(2)]
        tc_ = [S.sb([128, 512], F32, "tc") for _ in range(2)]
        sg = [S.sb([128, 512], F32, "sg") for _ in range(2)]
        wdst = [S.sb([128, 512], F32, "wdst") for _ in range(1)]
        TG = min(512, T_loc)
        groups = [(0, 2)] + [(2 + TG * i, TG) for i in range(T_loc // TG)]
        it = 0
        for fc in range(NFC):
            ws = wst[0]; wb = Wgu[fc % 2]
            S.dma("sp", ws[:, :, 0:128], wg_ap[:, fc * 128:(fc + 1) * 128].rearrange("(k p) c -> p k c", p=128), w=[ws])
            S.dma("sp", ws[:, :, 128:256], wu_ap[:, fc * 128:(fc + 1) * 128].rearrange("(k p) c -> p k c", p=128), w=[ws])
            S.op("pool", lambda e, ws=ws, wb=wb: e.tensor_copy(out=wb[:], in_=ws[:]), r=[ws], w=[wb])
            wd_s = wdst[0]
            for hh in range(2):
                S.dma("sp", wd_s[:], wd_ap[fc * 128:(fc + 1) * 128, hh * 512:(hh + 1) * 512], w=[wd_s])
                S.op("pool", lambda e, wd_s=wd_s, fc=fc, hh=hh: e.tensor_copy(out=Wd[:, fc, hh * 512:(hh + 1) * 512], in_=wd_s[:]), r=[wd_s], w=[Wd.sub((fc, hh))])
            for gi, (r0, n) in enumerate(groups):
                it += 1
                pg = psA[it % 2]; pu = psB[it % 2]
                g = gx[it % 2]; gprev = gx[(it - 1) % 2]
                hdeps = [hT_all[i] for i, (tr0, tn) in enumerate(tiles) if tr0 >= r0 and tr0 < r0 + n]
                for k in range(8):
                    S.op("pe", lambda e, pg=pg, k=k, r0=r0, n=n, wb=wb: e.matmul(pg[:, 0:n], lhsT=wb[:, k, 0:128], rhs=hT[:, k, r0:r0 + n], start=(k == 0), stop=(k == 7)), r=[wb] + hdeps, w=[pg])
                if gi == 0:
                    gnext = gx[(it + 1) % 2]
                    S.op("dve", lambda e, pg=pg, gnext=gnext: e.tensor_scalar(out=gnext[:, 0:2], in0=pg[:, 0:2], scalar1=flag[:, 0:1], scalar2=None, op0=ALU.mult, op1=ALU.bypass), r=[pg, flag], w=[gnext])
                    continue
                for k in range(8):
                    S.op("pe", lambda e, pu=pu, k=k, r0=r0, n=n, wb=wb: e.matmul(pu[:, 0:n], lhsT=wb[:, k, 128:256], rhs=hT[:, k, r0:r0 + n], start=(k == 0), stop=(k == 7)), r=[wb] + hdeps, w=[pu])
                S.op("act", lambda e, pg=pg, g=g, n=n: e.activation(out=g[:, 2:2 + n], in_=pg[:, 0:n], func=AF.Copy), r=[pg], w=[g])
                if gi < len(groups) - 1:
                    gnext = gx[(it + 1) % 2]
                    S.op("pool", lambda e, g=g, gnext=gnext, n=n: e.tensor_copy(out=gnext[:, 0:2], in_=g[:, n:n + 2]), r=[g], w=[gnext])
                t = tc_[it % 2]; s = sg[it % 2]
                S.op("dve", lambda e, g=g, t=t, n=n, fc=fc: e.tensor_scalar(out=t[:, 0:n], in0=g[:, 2:2 + n], scalar1=cw[:, 2, fc:fc + 1], scalar2=cb[:, fc:fc + 1], op0=ALU.mult, op1=ALU.add), r=[g, cw, cb], w=[t])
                S.op("dve", lambda e, g=g, t=t, n=n, fc=fc: e.scalar_tensor_tensor(out=t[:, 0:n], in0=g[:, 1:1 + n], scalar=cw[:, 1, fc:fc + 1], in1=t[:, 0:n], op0=ALU.mult, op1=ALU.add), r=[g, cw, t], w=[t])
                S.op("dve", lambda e, g=g, t=t, n=n, fc=fc: e.scalar_tensor_tensor(out=t[:, 0:n], in0=g[:, 0:n], scalar=cw[:, 0, fc:fc + 1], in1=t[:, 0:n], op0=ALU.mult, op1=ALU.add), r=[g, cw, t], w=[t])
                S.op("act", lambda e, t=t, s=s, n=n: e.activation(out=s[:, 0:n], in_=t[:, 0:n], func=AF.Silu), r=[t], w=[s])
                S.op("dve", lambda e, s=s, pu=pu, n=n, fc=fc, r0=r0: e.tensor_mul(out=actT[:, fc, r0 - 2:r0 - 2 + n], in0=pu[:, 0:n], in1=s[:, 0:n]), r=[pu, s], w=[actT.sub((fc, r0))])
        S.barrier()
        S.stack = old
    xm = [S.sb([128, D], F32, "xm") for _ in range(2)]
    tmp = [S.sb([128, D], F32, "tmp2") for _ in range(2)]
    junk = S.sb([128, D], BF16, "junk2"); ssq = S.sb([128, 1], F32, "ssq2"); rstd = S.sb([128, 1], F32, "rstd2")
    for tt in range(NT):
        x_t = xm[tt % 2]; t_t = tmp[tt % 2]
        S.dma("sp", x_t[:], xo_ap[tt * 128:(tt + 1) * 128, :], r=[S_xo(S)], w=[x_t])
        for half in range(2):
            ps = psA[half]
            for fc in range(NFC):
                S.op("pe", lambda e, ps=ps, fc=fc, tt=tt, half=half: e.matmul(ps[:], lhsT=actT[:, fc, tt * 128:(tt + 1) * 128], rhs=Wd[:, fc, half * 512:(half + 1) * 512], start=(fc == 0), stop=(fc == NFC - 1)), r=[actT, Wd] + [Wd.sub((fc, half))], w=[ps])
            S.op("dve", lambda e, ps=ps, half=half, t_t=t_t: e.tensor_mul(out=t_t[:, half * 512:(half + 1) * 512], in0=ps[:], in1=gf_b[:, half * 512:(half + 1) * 512]), r=[ps, gf_b], w=[t_t])
        S.op("pool", lambda e, x_t=x_t, t_t=t_t: e.tensor_add(out=t_t[:], in0=x_t[:], in1=t_t[:]), r=[t_t, x_t], w=[t_t])
        if final_g_ap is not None:
            S.op("act", lambda e, t_t=t_t: e.activation(out=junk[:], in_=t_t[:], func=AF.Square, accum_out=ssq[:]), r=[t_t], w=[junk, ssq])
            rstd_from_ssq(S, ssq, rstd, D)
            S.op("dve", lambda e, t_t=t_t: e.scalar_tensor_tensor(out=t_t[:], in0=t_t[:], scalar=rstd[:, 0:1], in1=nf_b[:], op0=ALU.mult, op1=ALU.mult), r=[t_t, rstd, nf_b], w=[t_t])
        S.dma("sp", xo_ap[tt * 128:(tt + 1) * 128, :], t_t[:], r=[t_t], w=[S_xo(S)], is_output=True)


def S_xo(S):
    if not hasattr(S, "_xo"):
        S._xo = Res("xo_dram")
    return S._xo


def build_post(T_loc, final):
    nc = new_nc()
    oT = din(nc, "oT", [D, T_loc + 2], BF16); x = din(nc, "x", [T_loc + 2, D], F32)
    wo = din(nc, "wo", [D, D], F32); mvec = din(nc, "mvec", [2, D], F32); mvf = din(nc, "mvf", [3, D], F32)
    wg = din(nc, "wg", [D, DFF], F32); wu = din(nc, "wu", [D, DFF], F32); wd = din(nc, "wd", [DFF, D], F32)
    cw = din(nc, "cw", [3, DFF], F32); cb = din(nc, "cb", [1, DFF], F32); flag = din(nc, "flag", [1, 1], F32)
    fg = din(nc, "fg", [1, D], F32) if final else None
    xo = dout(nc, "xo", [T_loc, D], F32)
    with ExitStack() as st:
        S = Sched(nc, st)
        cst, cmap = load_csts(S, nc, ["ident"])
        emit_post(S, T_loc, oT, x, wo, mvec, mvf, wg, wu, wd, cw, cb, flag, xo, cst, final_g_ap=fg)
        S.finish(); S.emit()
    return nc, cmap


def emit_diff(S, T, qT_ap, kT_ap, v_ap, lam_ap, subln_ap, oT_ap, cst):
    NKB = T // 128
    NQT = T // 512
    ident, mle = cst["ident"], cst["mle"]
    qT = [S.sb([128, T], BF16, "qT") for _ in range(4)]
    kT = [S.sb([128, T], BF16, "kT") for _ in range(4)]
    for i in range(4):
        S.op("pool", lambda e, i=i: e.memset(qT[i][64:128, :], 0.0), w=[qT[i]])
        S.op("pool", lambda e, i=i: e.memset(kT[i][64:128, :], 0.0), w=[kT[i]])
        S.dma("sp", qT[i][0:64, :], qT_ap[i], w=[qT[i]])
        S.dma("sp", kT[i][0:64, :], kT_ap[i], w=[kT[i]])
    Va = S.sb([128, NKB, 2, 129], BF16, "Va")
    S.op("pool", lambda e: e.memset(Va[:], 1.0), w=[Va])
    for h in range(2):
        S.dma("sp", Va[:, :, h, 0:128], v_ap[:, h * 128:(h + 1) * 128].rearrange("(kb p) d -> p kb d", p=128), w=[Va])
    lv = S.sb([128, 4, 64], F32, "lv")
    S.dma("sp", lv[:], lam_ap.rearrange("a d -> (a d)").partition_broadcast(128).rearrange("p (a d) -> p a d", a=4), w=[lv])
    pr = S.sb([128, 2, 64], F32, "pr")
    S.op("dve", lambda e: e.tensor_mul(out=pr[:, 0, :], in0=lv[:, 0, :], in1=lv[:, 1, :]), r=[lv], w=[pr])
    S.op("dve", lambda e: e.tensor_mul(out=pr[:, 1, :], in0=lv[:, 2, :], in1=lv[:, 3, :]), r=[lv, pr], w=[pr])
    sm = S.sb([128, 2], F32, "sm")
    S.op("dve", lambda e: e.tensor_reduce(out=sm[:], in_=pr[:], axis=AX.X, op=ALU.add), r=[pr], w=[sm])
    S.op("act", lambda e: e.activation(out=sm[:], in_=sm[:], func=AF.Exp), r=[sm], w=[sm])
    nlam = S.sb([128, 1], F32, "nlam")
    S.op("dve", lambda e: e.tensor_sub(out=nlam[:], in0=sm[:, 1:2], in1=sm[:, 0:1]), r=[sm], w=[nlam])
    S.op("dve", lambda e: e.tensor_single_scalar(out=nlam[:], in_=nlam[:], scalar=-LAMBDA_INIT, op=ALU.add), r=[nlam], w=[nlam])
    gsub = S.sb([128, 128], F32, "gsub")
    S.dma("sp", gsub[:], subln_ap.partition_broadcast(128), w=[gsub])
    S.op("dve", lambda e: e.tensor_single_scalar(out=gsub[:], in_=gsub[:], scalar=1.0 - LAMBDA_INIT, op=ALU.mult), r=[gsub], w=[gsub])

    ps_s = [S.ps([128, 512], F32, "ps_s") for _ in range(2)]
    ps_o = [S.ps([128, 2, 129], F32, "ps_o") for _ in range(4)]
    ps_t = S.ps([128, 512], BF16, "ps_t")
    pT = [S.sb([128, 512], BF16, "pT") for _ in range(3)]
    osb = [S.sb([128, 4, 128], F32, "osb") for _ in range(2)]
    rl = S.sb([128, 8], F32, "rl")
    od = S.sb([128, 4, 128], F32, "od")
    junk = S.sb([128, 128], BF16, "junk")
    ssq = S.sb([128, 4], F32, "ssq")
    rstd = S.sb([128, 4], F32, "rstd")
    onb = S.sb([128, 4, 128], BF16, "onb")
    ost = [S.sb([128, 512], BF16, "ost") for _ in range(2)]
    it = 0
    for h in range(2):
        for qt in range(NQT):
            qsl = slice(qt * 512, (qt + 1) * 512)
            for j in range(2):
                q_t, k_t = qT[h * 2 + j], kT[h * 2 + j]
                nkb = 4 * qt + 4
                started = [False, False]
                for kb in range(nkb):
                    d = kb - 4 * qt
                    it += 1
                    ps = ps_s[it % 2]
                    p_t = pT[it % 3]
                    S.op("pe", lambda e, ps=ps, k_t=k_t, q_t=q_t, kb=kb, qsl=qsl, d=d: e.matmul(ps[:], lhsT=k_t[:, kb * 128:(kb + 1) * 128], rhs=q_t[:, qsl], start=True, stop=(d < 0)), r=[k_t, q_t], w=[ps])
                    if d >= 0:
                        S.op("pe", lambda e, ps=ps, d=d: e.matmul(ps[:], lhsT=ident[:], rhs=mle[:, d, :], start=False, stop=True), r=[ident, mle], w=[ps])
                    S.op("act", lambda e, ps=ps, p_t=p_t: e.activation(out=p_t[:], in_=ps[:], func=AF.Exp, scale=0.125), r=[ps], w=[p_t])
                    for sub in range(max(d, 0), 4):
                        bank = ps_o[j * 2 + sub // 2]
                        st = not started[sub // 2]
                        started[sub // 2] = True
                        S.op("pe", lambda e, bank=bank, sub=sub, p_t=p_t, kb=kb, st=st, h=h: e.matmul(bank[:, sub % 2, :], lhsT=p_t[:, sub * 128:(sub + 1) * 128], rhs=Va[:, kb, h, :], start=st, stop=(kb == 4 * qt + sub), skip_group_check=True), r=[p_t, Va], w=[bank])
                for sub in range(4):
                    bank = ps_o[j * 2 + sub // 2]
                    c = j * 4 + sub
                    S.op("dve", lambda e, bank=bank, sub=sub, c=c: e.reciprocal(out=rl[:, c:c + 1], in_=bank[:, sub % 2, 128:129]), r=[bank], w=[rl])
                    S.op("dve", lambda e, bank=bank, sub=sub, c=c, j=j: e.tensor_scalar(out=osb[j][:, sub, :], in0=bank[:, sub % 2, 0:128], scalar1=rl[:, c:c + 1], scalar2=None, op0=ALU.mult, op1=ALU.bypass), r=[bank, rl], w=[osb[j]])
            S.op("dve", lambda e: e.scalar_tensor_tensor(out=od[:], in0=osb[1][:], scalar=nlam[:, 0:1], in1=osb[0][:], op0=ALU.mult, op1=ALU.add), r=[osb[0], osb[1], nlam], w=[od])
            for sub in range(4):
                S.op("act", lambda e, sub=sub: e.activation(out=junk[:], in_=od[:, sub, :], func=AF.Square, accum_out=ssq[:, sub:sub + 1]), r=[od], w=[junk, ssq])
            rstd_from_ssq(S, ssq, rstd, 128)
            for sub in range(4):
                S.op("dve", lambda e, sub=sub: e.scalar_tensor_tensor(out=onb[:, sub, :], in0=od[:, sub, :], scalar=rstd[:, sub:sub + 1], in1=gsub[:], op0=ALU.mult, op1=ALU.mult), r=[od, rstd, gsub], w=[onb])
            for sub in range(4):
                S.op("pe", lambda e, sub=sub: e.transpose(out=ps_t[:, sub * 128:(sub + 1) * 128], in_=onb[:, sub, :], identity=ident[:]), r=[onb, ident], w=[ps_t])
            o_s = ost[(h * NQT + qt) % 2]
            S.op("act", lambda e, o_s=o_s: e.activation(out=o_s[:], in_=ps_t[:], func=AF.Copy), r=[ps_t], w=[o_s])
            S.dma("sp", oT_ap[h * 128:(h + 1) * 128, qsl], o_s[:], r=[o_s], is_output=True)


def build_diff(T):
    nc = new_nc()
    qT = din(nc, "qT", [4, 64, T], BF16); kT = din(nc, "kT", [4, 64, T], BF16)
    v = din(nc, "v", [T, 256], BF16); lam = din(nc, "lam", [4, 64], F32); subln = din(nc, "subln", [1, 128], F32)
    oT = dout(nc, "oT", [256, T], BF16)
    with ExitStack() as st:
        S = Sched(nc, st)
        cst, cmap = load_csts(S, nc, ["ident", "mle"])
        emit_diff(S, T, qT, kT, v, lam, subln, oT, cst)
        S.finish(); S.emit()
    return nc, cmap


def emit_sb(S, T, qT_ap, kT_ap, v_ap, oT_ap, cst, banks=None):
    NKB = T // 128
    NQT = T // 512
    ident, mlt, negtri, negones = cst["ident"], cst["mlt"], cst["negtri"], cst["negones"]
    qT = [S.sb([128, T], BF16, "sqT") for _ in range(2)]
    kT = [S.sb([128, T], BF16, "skT") for _ in range(2)]
    for i in range(2):
        S.op("pool", lambda e, i=i: e.memset(qT[i][64:128, :], 0.0), w=[qT[i]])
        S.op("pool", lambda e, i=i: e.memset(kT[i][64:128, :], 0.0), w=[kT[i]])
        S.dma("sp", qT[i][0:64, :], qT_ap[i], w=[qT[i]])
        S.dma("sp", kT[i][0:64, :], kT_ap[i], w=[kT[i]])
    V = S.sb([128, NKB, 128], BF16, "sV")
    S.dma("sp", V[:], v_ap.rearrange("(kb p) d -> p kb d", p=128), w=[V])
    ps_z = [S.ps([128, 512], F32, "ps_z") for _ in range(2)]
    ps_c = [S.ps([128, 512], F32, "ps_c") for _ in range(2)]
    ps_o = [S.ps([128, 4, 64], F32, "ps_o") for _ in range(2)]
    ps_t = S.ps([128, 512], BF16, "ps_t")
    E = [S.sb([128, 512], F32, "E") for _ in range(2)]
    sp = [S.sb([128, 512], BF16, "sp") for _ in range(2)]
    Racc = [S.sb([128, 512], BF16, "Racc") for _ in range(2)]
    aT = [S.sb([128, 512], BF16, "aT") for _ in range(2)]
    ob = S.sb([128, 4, 64], BF16, "ob")
    ost = [S.sb([64, 512], BF16, "ost") for _ in range(2)]
    it = 0
    for j in range(2):
        for qt in range(NQT):
            qsl = slice(qt * 512, (qt + 1) * 512)
            po = ps_o[(j * NQT + qt) % 2]
            first_o = True
            ri = 0
            for kb in range(4 * qt + 3, -1, -1):
                d = kb - 4 * qt
                it += 1
                pz = ps_z[it % 2]; pc = ps_c[it % 2]; e_t = E[it % 2]; s_t = sp[it % 2]; a_t = aT[it % 2]
                first = (kb == 4 * qt + 3)
                def zmm(e, ps, last, kb=kb, qsl=qsl, d=d, j=j):
                    pass
                S.op("pe", lambda e, pz=pz, kb=kb, qsl=qsl, d=d, j=j: e.matmul(pz[:], lhsT=kT[j][:, kb * 128:(kb + 1) * 128], rhs=qT[j][:, qsl], start=True, stop=(d < 0)), r=[kT[j], qT[j]], w=[pz])
                if d >= 0:
                    S.op("pe", lambda e, pz=pz, d=d: e.matmul(pz[:], lhsT=ident[:], rhs=mlt[:, d, :], start=False, stop=True), r=[ident, mlt], w=[pz])
                S.op("act", lambda e, pz=pz, e_t=e_t: e.activation(out=e_t[:], in_=pz[:], func=AF.Exp), r=[pz], w=[e_t])
                S.op("act", lambda e, e_t=e_t, s_t=s_t: e.activation(out=s_t[:], in_=e_t[:], func=AF.Ln, bias=1.0), r=[e_t], w=[s_t])
                S.op("pe", lambda e, pc=pc, kb=kb, qsl=qsl, j=j: e.matmul(pc[:], lhsT=kT[j][:, kb * 128:(kb + 1) * 128], rhs=qT[j][:, qsl], start=True, stop=False), r=[kT[j], qT[j]], w=[pc])
                if d >= 0:
                    S.op("pe", lambda e, pc=pc, d=d: e.matmul(pc[:], lhsT=ident[:], rhs=mlt[:, d, :], start=False, stop=False), r=[ident, mlt], w=[pc])
                S.op("pe", lambda e, pc=pc, s_t=s_t, first=first: e.matmul(pc[:], lhsT=negtri[:], rhs=s_t[:], start=False, stop=first), r=[negtri, s_t], w=[pc])
                if not first:
                    rc = Racc[ri % 2]
                    S.op("pe", lambda e, pc=pc, rc=rc: e.matmul(pc[:], lhsT=negones[:], rhs=rc[:], start=False, stop=True), r=[negones, rc], w=[pc])
                    if kb > 0:
                        rn = Racc[(ri + 1) % 2]
                        S.op("pool", lambda e, rc=rc, rn=rn, s_t=s_t: e.tensor_add(out=rn[:], in0=rc[:], in1=s_t[:]), r=[rc, s_t], w=[rn])
                        ri += 1
                else:
                    rn = Racc[ri % 2]
                    S.op("pool", lambda e, rn=rn, s_t=s_t: e.tensor_copy(out=rn[:], in_=s_t[:]), r=[s_t], w=[rn])
                S.op("act", lambda e, pc=pc, a_t=a_t: e.activation(out=a_t[:], in_=pc[:], func=AF.Exp), r=[pc], w=[a_t])
                for sub in range(max(d, 0), 4):
                    S.op("pe", lambda e, po=po, sub=sub, a_t=a_t, kb=kb, st=first_o, j=j: e.matmul(po[:, sub, :], lhsT=a_t[:, sub * 128:(sub + 1) * 128], rhs=V[:, kb, j * 64:(j + 1) * 64], start=st, stop=(kb == 0), skip_group_check=True), r=[a_t, V], w=[po])
                    first_o = False
            S.op("act", lambda e, po=po: e.activation(out=ob[:], in_=po[:], func=AF.Copy), r=[po], w=[ob])
            for sub in range(4):
                S.op("pe", lambda e, sub=sub: e.transpose(out=ps_t[0:64, sub * 128:(sub + 1) * 128], in_=ob[:, sub, :], identity=ident[:]), r=[ob, ident], w=[ps_t])
            o_s = ost[(j * NQT + qt) % 2]
            S.op("dve", lambda e, o_s=o_s: e.tensor_copy(out=o_s[:], in_=ps_t[0:64, :]), r=[ps_t], w=[o_s])
            S.dma("sp", oT_ap[j * 64:(j + 1) * 64, qsl], o_s[:], r=[o_s], is_output=True)


def build_sb(T):
    nc = new_nc()
    qT = din(nc, "qT", [2, 64, T], BF16); kT = din(nc, "kT", [2, 64, T], BF16)
    v = din(nc, "v", [T, 128], BF16)
    oT = dout(nc, "oT", [128, T], BF16)
    with ExitStack() as st:
        S = Sched(nc, st)
        cst, cmap = load_csts(S, nc, ["ident", "mlt", "negtri", "negones"])
        emit_sb(S, T, qT, kT, v, oT, cst)
        S.finish(); S.emit()
    return nc, cmap


def nsa_consts(T):
    c = {}
    p = np.arange(128)[:, None]
    cc = np.arange(512)[None, :]
    c["mbase"] = (16.0 * cc + 31.0 - p).astype(np.float32)
    n = np.arange(128)[None, :]
    c["dmat"] = (n - (p >= 64)).astype(np.float32)
    c["col0"] = np.broadcast_to((n == 0), (128, 128)).astype(np.float32)
    key = np.arange(T)[None, :]
    c["eall"] = ((key // 64) == p).astype(np.float32).astype(NPBF)
    return c


def emit_nsa(S, T, A, cst):
    NKB = T // 128
    NQT = T // 512
    NCc = (T - 32) // 16 + 1
    NCB = (NCc + 127) // 128
    ident, pswap, ropec, mle, mwin = cst["ident"], cst["pswap"], cst["ropec"], cst["mle"], cst["mwin"]
    mbase, dmat, col0, eall = cst["mbase"], cst["dmat"], cst["col0"], cst["eall"]
    ps_s = [S.ps([128, 512], F32, "ps_s") for _ in range(2)]
    ps_os = S.ps([128, 4, 65], F32, "ps_os")
    ps_ow = S.ps([128, 4, 65], F32, "ps_ow")
    ps_oc = S.ps([128, 2, 65], F32, "ps_oc")
    ps_t = S.ps([128, 512], BF16, "ps_t")
    ps_m = S.ps([128, 512], F32, "ps_m")
    kcmpT = S.sb([128, 512], BF16, "kcmpT")
    vcmp = S.sb([128, 4, 65], BF16, "vcmp")
    S.op("pool", lambda e: e.memset(kcmpT[:], 0.0), w=[kcmpT])
    S.op("pool", lambda e: e.memset(vcmp[:], 1.0), w=[vcmp])
    with ExitStack() as st:
        old = S.stack; S.stack = st
        Cc, Sc = rope_tables(S, A["posc"], 512, ropec, "rc")
        def _cmp(which):
            xT = S.sb([64, T], BF16, "cxT")
            S.dma("sp", xT[:], A["kcT"] if which == 0 else A["vcT"], w=[xT])
            W1 = S.sb([64, 32, 256], BF16, "W1")
            w1st = [S.sb([64, 4, 256], F32, "w1st") for _ in range(2)]
            w1v = (A["w1k"] if which == 0 else A["w1v"]).rearrange("(l d) h -> d l h", d=64)
            for li in range(8):
                s = w1st[li % 2]
                S.dma("sp", s[:], w1v[:, li * 4:(li + 1) * 4, :], w=[s])
                S.op("pool", lambda e, s=s, li=li: e.tensor_copy(out=W1[:, li * 4:(li + 1) * 4, :], in_=s[:]), r=[s], w=[W1])
            posf = S.sb([64, 32], F32, "posf"); posb = S.sb([64, 32], BF16, "posb")
            S.dma("sp", posf[:], (A["posk"] if which == 0 else A["posv"]).rearrange("l d -> d l"), w=[posf], allow_slow_non_contiguous=True)
            S.op("dve", lambda e: e.tensor_copy(out=posb[:], in_=posf[:]), r=[posf], w=[posb])
            w2f = S.sb([128, 2, 64], F32, "w2f"); W2 = S.sb([128, 2, 64], BF16, "W2")
            S.dma("sp", w2f[:], (A["w2k"] if which == 0 else A["w2v"]).rearrange("(c p) d -> p c d", p=128), w=[w2f])
            S.op("dve", lambda e: e.tensor_copy(out=W2[:], in_=w2f[:]), r=[w2f], w=[W2])
            hidT = S.sb([128, 2, 512], BF16, "hidT")
            S.op("pool", lambda e: e.memset(hidT[:], 0.0), w=[hidT])
            b1 = S.sb([128, 2], F32, "b1")
            for hc in range(2):
                for l in range(32):
                    S.op("pe", lambda e, l=l, hc=hc: e.matmul(ps_m[:, 0:1], lhsT=W1[:, l, hc * 128:(hc + 1) * 128], rhs=posb[:, l:l + 1], start=(l == 0), stop=(l == 31)), r=[W1, posb], w=[ps_m])
                S.op("dve", lambda e, hc=hc: e.tensor_copy(out=b1[:, hc:hc + 1], in_=ps_m[:, 0:1]), r=[ps_m], w=[b1])
                ps = ps_s[hc]
                for l in range(32):
                    S.op("pe", lambda e, l=l, hc=hc, ps=ps: e.matmul(ps[:, 0:NCc], lhsT=W1[:, l, hc * 128:(hc + 1) * 128], rhs=xT[:, l:l + 16 * (NCc - 1) + 1:16], start=(l == 0), stop=(l == 31)), r=[W1, xT], w=[ps])
                S.op("act", lambda e, hc=hc, ps=ps: e.activation(out=hidT[:, hc, 0:NCc], in_=ps[:, 0:NCc], func=AF.Silu, bias=b1[:, hc:hc + 1]), r=[ps, b1], w=[hidT])
            if "dbg_kcmp" in A:
                hf = S.sb([128, 2, 512], F32, "hf")
                S.op("dve", lambda e: e.tensor_copy(out=hf[:], in_=hidT[:]), r=[hidT], w=[hf])
                S.dma("sp", A["dbg_hid"][which], hf[:], r=[hf], is_output=True)
                S.dma("sp", A["dbg_b1"][which], b1[:], r=[b1], is_output=True)
            if which == 0:
                ktok = S.sb([128, 4, 64], BF16, "ktok")
                S.op("pool", lambda e: e.memset(ktok[:], 0.0), w=[ktok])
                for cb in range(NCB):
                    nb = min(128, NCc - cb * 128)
                    for hc in range(2):
                        S.op("pe", lambda e, hc=hc, cb=cb, nb=nb: e.matmul(ps_m[0:nb, 0:64], lhsT=hidT[:, hc, cb * 128:cb * 128 + nb], rhs=W2[:, hc, :], start=(hc == 0), stop=(hc == 1)), r=[W2, hidT], w=[ps_m])
                    S.op("act", lambda e, cb=cb, nb=nb: e.activation(out=ktok[0:nb, cb, :], in_=ps_m[0:nb, 0:64], func=AF.Copy), r=[ps_m], w=[ktok])
                for cb in range(4):
                    S.op("pe", lambda e, cb=cb: e.transpose(out=ps_t[0:64, cb * 128:(cb + 1) * 128], in_=ktok[:, cb, :], identity=ident[:]), r=[ktok, ident], w=[ps_t])
                kb_ = S.sb([64, 512], BF16, "kb_"); u1 = S.sb([64, 512], F32, "u1"); u2 = S.sb([64, 512], F32, "u2")
                S.op("act", lambda e: e.activation(out=kb_[:], in_=ps_t[0:64, :], func=AF.Copy), r=[ps_t], w=[kb_])
                S.op("pe", lambda e: e.matmul(ps_s[0][:, :], lhsT=pswap[0:64, :], rhs=kb_[:], start=True, stop=True), r=[pswap, kb_], w=[ps_s[0]])
                S.op("dve", lambda e: e.tensor_mul(out=u1[:], in0=kb_[:], in1=Cc[0:64, :]), r=[kb_, Cc], w=[u1])
                S.op("dve", lambda e: e.tensor_mul(out=u2[:], in0=ps_s[0][0:64, :], in1=Sc[0:64, :]), r=[ps_s[0], Sc], w=[u2])
                S.op("pool", lambda e: e.tensor_add(out=kcmpT[0:64, 0:NCc], in0=u1[:, 0:NCc], in1=u2[:, 0:NCc]), r=[u1, u2], w=[kcmpT])
                if "dbg_kcmp" in A:
                    S.dma("sp", A["dbg_u1"], u1[:], r=[u1], is_output=True)
                    S.dma("sp", A["dbg_u2"], u2[:], r=[u2], is_output=True)
                    S.dma("sp", A["dbg_cc"], Cc[0:64, :], r=[Cc], is_output=True)
                    S.dma("sp", A["dbg_sc"], Sc[0:64, :], r=[Sc], is_output=True)
            else:
                for cb in range(NCB):
                    nb = min(128, NCc - cb * 128)
                    for hc in range(2):
                        S.op("pe", lambda e, hc=hc, cb=cb, nb=nb: e.matmul(ps_m[0:nb, 0:64], lhsT=hidT[:, hc, cb * 128:cb * 128 + nb], rhs=W2[:, hc, :], start=(hc == 0), stop=(hc == 1)), r=[W2, hidT], w=[ps_m])
                    S.op("act", lambda e, cb=cb, nb=nb: e.activation(out=vcmp[0:nb, cb, 0:64], in_=ps_m[0:nb, 0:64], func=AF.Copy), r=[ps_m], w=[vcmp])
            S.barrier()
        for _w in (0, 1):
            _cmp(_w)
        S.stack = old
    if "dbg_kcmp" in A:
        kcf = S.sb([64, 512], F32, "kcf"); vcf = S.sb([128, 4, 65], F32, "vcf")
        S.op("dve", lambda e: e.tensor_copy(out=kcf[:], in_=kcmpT[0:64, :]), r=[kcmpT], w=[kcf])
        S.op("dve", lambda e: e.tensor_copy(out=vcf[:], in_=vcmp[:]), r=[vcmp], w=[vcf])
        S.dma("sp", A["dbg_kcmp"], kcf[:], r=[kcf], is_output=True)
        S.dma("sp", A["dbg_vcmp"], vcf[:], r=[vcf], is_output=True)
    qn = [S.sb([128, T], BF16, "qn") for _ in range(4)]
    for i in range(4):
        S.op("pool", lambda e, i=i: e.memset(qn[i][64:128, :], 0.0), w=[qn[i]])
        S.dma("sp", qn[i][0:64, :], A["qnT"][i], w=[qn[i]])
    ksT = S.sb([128, T], BF16, "ksT"); kwT = S.sb([128, T], BF16, "kwT")
    S.op("pool", lambda e: e.memset(ksT[64:128, :], 0.0), w=[ksT]); S.op("pool", lambda e: e.memset(kwT[64:128, :], 0.0), w=[kwT])
    S.dma("sp", ksT[0:64, :], A["ksT"], w=[ksT]); S.dma("sp", kwT[0:64, :], A["kwT"], w=[kwT])
    vsa = S.sb([128, NKB, 65], BF16, "vsa"); vwa = S.sb([128, NKB, 65], BF16, "vwa")
    S.op("pool", lambda e: e.memset(vsa[:], 1.0), w=[vsa]); S.op("pool", lambda e: e.memset(vwa[:], 1.0), w=[vwa])
    S.dma("sp", vsa[:, :, 0:64], A["vs"].rearrange("(kb p) d -> p kb d", p=128), w=[vsa])
    S.dma("sp", vwa[:, :, 0:64], A["vw"].rearrange("(kb p) d -> p kb d", p=128), w=[vwa])
    madd = S.sb([128, 512], F32, "madd")
    sm = [S.sb([128, 512], F32, "sm") for _ in range(2)]
    P = [S.sb([128, 512], F32, "P") for _ in range(2)]
    Pb = [S.sb([128, 512], BF16, "Pb") for _ in range(2)]
    PT = [S.sb([128, 512], BF16, "PT") for _ in range(2)]
    lc = S.sb([128, 4], F32, "lc"); rlc = S.sb([128, 4], F32, "rlc")
    Ps4 = S.sb([128, 512], F32, "Ps4")
    imp = S.sb([128, 128], F32, "imp")
    v_ = S.sb([128, 128], F32, "v_"); f_ = S.sb([128, 128], F32, "f_"); vf_ = S.sb([128, 128], F32, "vf_"); ad_ = S.sb([128, 128], F32, "ad_")
    score = S.sb([128, 128], F32, "score"); sc2 = S.sb([128, 128], F32, "sc2"); m8 = S.sb([128, 16], F32, "m8")
    selb = S.sb([128, 128], BF16, "selb")
    selT = [S.sb([128, 512], BF16, "selT") for _ in range(2)]
    ocs = [S.sb([128, 4, 2, 65], F32, "ocs") for _ in range(2)]
    pT = [S.sb([128, 512], BF16, "pT") for _ in range(3)]
    oss = S.sb([128, 4, 65], F32, "oss")
    glb = S.sb([128, 4, 6], BF16, "glb"); gg = S.sb([128, 4, 6], F32, "gg")
    ww = S.sb([128, 4, 3], F32, "ww")
    oacc = S.sb([128, 4, 64], F32, "oacc"); ob = S.sb([128, 4, 64], BF16, "ob")
    ost = [S.sb([64, 512], BF16, "ost") for _ in range(2)]
    itc = [0]
    def _qt(qt):
        qsl = slice(qt * 512, (qt + 1) * 512)
        oc_t = ocs[qt % 2]; sT = selT[qt % 2]
        def _sub(sub):
            qs = qt * 4 + sub
            q1 = slice(qs * 128, (qs + 1) * 128)
            S.op("dve", lambda e, qs=qs: e.tensor_scalar(out=madd[:], in0=mbase[:], scalar1=float(128 * qs), scalar2=NEG, op0=ALU.is_gt, op1=ALU.mult), r=[mbase], w=[madd])
            for i in range(4):
                itc[0] += 1; it = itc[0]
                ps = ps_s[it % 2]; s_ = sm[it % 2]; p_ = P[it % 2]
                S.op("pe", lambda e, ps=ps, i=i, q1=q1: e.matmul(ps[:], lhsT=qn[i][:, q1], rhs=kcmpT[:], start=True, stop=True), r=[qn[i], kcmpT], w=[ps])
                S.op("dve", lambda e, ps=ps, s_=s_: e.scalar_tensor_tensor(out=s_[:], in0=ps[:], scalar=0.125, in1=madd[:], op0=ALU.mult, op1=ALU.add), r=[ps, madd], w=[s_])
                S.op("act", lambda e, s_=s_, p_=p_, i=i: e.activation(out=p_[:], in_=s_[:], func=AF.Exp, accum_out=lc[:, i:i + 1]), r=[s_], w=[p_, lc])
                S.op("dve", lambda e, i=i: e.tensor_single_scalar(out=rlc[:, i:i + 1], in_=lc[:, i:i + 1], scalar=1e-30, op=ALU.max), r=[lc], w=[rlc])
                S.op("dve", lambda e, i=i: e.reciprocal(out=rlc[:, i:i + 1], in_=rlc[:, i:i + 1]), r=[rlc], w=[rlc])
                if i == 0:
                    S.op("dve", lambda e, p_=p_, i=i: e.tensor_scalar(out=Ps4[:], in0=p_[:], scalar1=rlc[:, i:i + 1], scalar2=None, op0=ALU.mult, op1=ALU.bypass), r=[p_, rlc], w=[Ps4])
                else:
                    S.op("dve", lambda e, p_=p_, i=i: e.scalar_tensor_tensor(out=Ps4[:], in0=p_[:], scalar=rlc[:, i:i + 1], in1=Ps4[:], op0=ALU.mult, op1=ALU.add), r=[p_, rlc, Ps4], w=[Ps4])
                if i < 2:
                    pb = Pb[i]; pt = PT[i]
                    S.op("pool", lambda e, p_=p_, pb=pb: e.tensor_copy(out=pb[:], in_=p_[:]), r=[p_], w=[pb])
                    for cb in range(NCB):
                        S.op("pe", lambda e, pb=pb, cb=cb: e.transpose(out=ps_t[:, cb * 128:(cb + 1) * 128], in_=pb[:, cb * 128:(cb + 1) * 128], identity=ident[:]), r=[pb, ident], w=[ps_t])
                    S.op("act", lambda e, pt=pt: e.activation(out=pt[:, 0:NCB * 128], in_=ps_t[:, 0:NCB * 128], func=AF.Copy), r=[ps_t], w=[pt])
                    for cb in range(NCB):
                        S.op("pe", lambda e, pt=pt, cb=cb, i=i: e.matmul(ps_oc[:, i, :], lhsT=pt[:, cb * 128:(cb + 1) * 128], rhs=vcmp[:, cb, :], start=(cb == 0), stop=(cb == NCB - 1)), r=[pt, vcmp], w=[ps_oc])
                    S.op("act", lambda e, i=i, sub=sub, oc_t=oc_t: e.activation(out=oc_t[:, sub, i, :], in_=ps_oc[:, i, :], func=AF.Copy), r=[ps_oc], w=[oc_t])
            S.op("dve", lambda e: e.tensor_reduce(out=imp[:], in_=Ps4[:].rearrange("p (n f) -> p n f", f=4), axis=AX.X, op=ALU.add), r=[Ps4], w=[imp])
            S.op("dve", lambda e: e.tensor_add(out=imp[:, 1:128], in0=imp[:, 1:128], in1=Ps4[:, 3:508:4]), r=[imp, Ps4], w=[imp])
            S.op("dve", lambda e, qs=qs: e.tensor_single_scalar(out=v_[:], in_=dmat[:], scalar=float(2 * qs), op=ALU.is_le), r=[dmat], w=[v_])
            S.op("dve", lambda e, qs=qs: e.scalar_tensor_tensor(out=f_[:], in0=dmat[:], scalar=float(2 * qs - 1), in1=v_[:], op0=ALU.is_ge, op1=ALU.mult), r=[dmat, v_], w=[f_])
            S.op("dve", lambda e: e.tensor_max(out=f_[:], in0=f_[:], in1=col0[:]), r=[f_, col0], w=[f_])
            S.op("dve", lambda e: e.tensor_sub(out=vf_[:], in0=v_[:], in1=f_[:]), r=[v_, f_], w=[vf_])
            S.op("dve", lambda e: e.scalar_tensor_tensor(out=ad_[:], in0=f_[:], scalar=-1.0, in1=v_[:], op0=ALU.add, op1=ALU.add), r=[f_, v_], w=[ad_])
            S.op("dve", lambda e: e.tensor_mul(out=score[:], in0=imp[:], in1=vf_[:]), r=[imp, vf_], w=[score])
            S.op("dve", lambda e: e.scalar_tensor_tensor(out=score[:], in0=ad_[:], scalar=1e9, in1=score[:], op0=ALU.mult, op1=ALU.add), r=[ad_, score], w=[score])
            S.op("dve", lambda e: e.max(out=m8[:, 0:8], in_=score[:]), r=[score], w=[m8])
            S.op("dve", lambda e: e.match_replace(out=sc2[:], in_to_replace=m8[:, 0:8], in_values=score[:], imm_value=-3e9), r=[score, m8], w=[sc2])
            S.op("dve", lambda e: e.max(out=m8[:, 8:16], in_=sc2[:]), r=[sc2, m8], w=[m8])
            S.op("dve", lambda e: e.tensor_scalar(out=sc2[:], in0=score[:], scalar1=m8[:, 15:16], scalar2=None, op0=ALU.is_ge, op1=ALU.bypass), r=[score, m8], w=[sc2])
            S.op("dve", lambda e: e.tensor_scalar(out=selb[:], in0=sc2[:], scalar1=-1.0, scalar2=-NEG, op0=ALU.add, op1=ALU.mult), r=[sc2], w=[selb])
            if "dbg_kcmp" in A and qs == A["dbg_qs"]:
                S.dma("sp", A["dbg_imp"], imp[:], r=[imp], is_output=True)
                S.dma("sp", A["dbg_score"], score[:], r=[score], is_output=True)
                S.dma("sp", A["dbg_sel"], sc2[:], r=[sc2], is_output=True)
                S.dma("sp", A["dbg_m8"], m8[:], r=[m8], is_output=True)
                S.dma("sp", A["dbg_ps4"], Ps4[:], r=[Ps4], is_output=True)
            S.op("pe", lambda e: e.transpose(out=ps_t[:, 0:128], in_=selb[:], identity=ident[:]), r=[selb, ident], w=[ps_t])
            S.op("act", lambda e, sub=sub, sT=sT: e.activation(out=sT[:, sub * 128:(sub + 1) * 128], in_=ps_t[:, 0:128], func=AF.Copy), r=[ps_t], w=[sT])
        for _s in range(4):
            _sub(_s)
        S.dma("sp", glb[:], A["gl"][qsl, :].rearrange("(s p) c -> p s c", p=128), w=[glb], allow_slow_non_contiguous=True)
        S.op("act", lambda e: e.activation(out=gg[:], in_=glb[:], func=AF.Exp, scale=-1.0), r=[glb], w=[gg])
        S.op("dve", lambda e: e.tensor_single_scalar(out=gg[:], in_=gg[:], scalar=1.0, op=ALU.add), r=[gg], w=[gg])
        S.op("dve", lambda e: e.reciprocal(out=gg[:], in_=gg[:]), r=[gg], w=[gg])
        def _head(i):
            first_o = True
            for kb in range(4 * qt + 4):
                d = kb - 4 * qt
                itc[0] += 1; it = itc[0]
                ps = ps_s[it % 2]; p_t = pT[it % 3]
                S.op("pe", lambda e, ps=ps, kb=kb, i=i: e.matmul(ps[:], lhsT=ksT[:, kb * 128:(kb + 1) * 128], rhs=qn[i][:, qsl], start=True, stop=False), r=[ksT, qn[i]], w=[ps])
                S.op("pe", lambda e, ps=ps, kb=kb, d=d: e.matmul(ps[:], lhsT=eall[:, kb * 128:(kb + 1) * 128], rhs=sT[:], start=False, stop=(d < 0)), r=[eall, sT], w=[ps])
                if d >= 0:
                    S.op("pe", lambda e, ps=ps, d=d: e.matmul(ps[:], lhsT=ident[:], rhs=mle[:, d, :], start=False, stop=True), r=[ident, mle], w=[ps])
                S.op("act", lambda e, ps=ps, p_t=p_t: e.activation(out=p_t[:], in_=ps[:], func=AF.Exp, scale=0.125), r=[ps], w=[p_t])
                for sub in range(max(d, 0), 4):
                    S.op("pe", lambda e, sub=sub, p_t=p_t, kb=kb, st=first_o: e.matmul(ps_os[:, sub, :], lhsT=p_t[:, sub * 128:(sub + 1) * 128], rhs=vsa[:, kb, :], start=st, stop=(kb == 4 * qt + sub), skip_group_check=True), r=[p_t, vsa], w=[ps_os])
                    first_o = False
            S.op("act", lambda e: e.activation(out=oss[:], in_=ps_os[:], func=AF.Copy), r=[ps_os], w=[oss])
            first_o = True
            for kb in range(max(0, 4 * qt - 4), 4 * qt + 4):
                d = kb - 4 * qt
                itc[0] += 1; it = itc[0]
                ps = ps_s[it % 2]; p_t = pT[it % 3]
                S.op("pe", lambda e, ps=ps, kb=kb, i=i: e.matmul(ps[:], lhsT=kwT[:, kb * 128:(kb + 1) * 128], rhs=qn[i][:, qsl], start=True, stop=False), r=[kwT, qn[i]], w=[ps])
                mk = mle[:, d, :] if d >= 0 else mwin[:, d + 4, :]
                S.op("pe", lambda e, ps=ps, mk=mk: e.matmul(ps[:], lhsT=ident[:], rhs=mk, start=False, stop=True), r=[ident, mle, mwin], w=[ps])
                S.op("act", lambda e, ps=ps, p_t=p_t: e.activation(out=p_t[:], in_=ps[:], func=AF.Exp, scale=0.125), r=[ps], w=[p_t])
                subs = range(d, 4) if d >= 0 else range(0, d + 5)
                for sub in subs:
                    last_kb = 4 * qt + sub
                    S.op("pe", lambda e, sub=sub, p_t=p_t, kb=kb, st=first_o, last_kb=last_kb: e.matmul(ps_ow[:, sub, :], lhsT=p_t[:, sub * 128:(sub + 1) * 128], rhs=vwa[:, kb, :], start=st, stop=(kb == last_kb), skip_group_check=True), r=[p_t, vwa], w=[ps_ow])
                    first_o = False
            S.op("dve", lambda e, i=i: e.tensor_single_scalar(out=ww[:, :, 0], in_=oc_t[:, :, i, 64], scalar=1e-30, op=ALU.max), r=[oc_t], w=[ww])
            S.op("dve", lambda e: e.tensor_copy(out=ww[:, :, 1], in_=oss[:, :, 64]), r=[oss, ww], w=[ww])
            S.op("dve", lambda e: e.tensor_copy(out=ww[:, :, 2], in_=ps_ow[:, :, 64]), r=[ps_ow, ww], w=[ww])
            S.op("dve", lambda e: e.reciprocal(out=ww[:], in_=ww[:]), r=[ww], w=[ww])
            S.op("dve", lambda e, i=i: e.tensor_mul(out=ww[:], in0=ww[:], in1=gg[:, :, i * 3:(i + 1) * 3]), r=[ww, gg], w=[ww])
            if "dbg_kcmp" in A and qt == A["dbg_qs"] // 4 and i == 0:
                owf = S.sb([128, 4, 65], F32, "owf")
                S.op("dve", lambda e: e.tensor_copy(out=owf[:], in_=ps_ow[:]), r=[ps_ow], w=[owf])
                S.dma("sp", A["dbg_ow"], owf[:], r=[owf], is_output=True)
                S.dma("sp", A["dbg_os"], oss[:], r=[oss], is_output=True)
                S.dma("sp", A["dbg_oc"], oc_t[:], r=[oc_t], is_output=True)
                S.dma("sp", A["dbg_ww"], ww[:], r=[ww], is_output=True)
            for sub in range(4):
                S.op("dve", lambda e, sub=sub, i=i: e.tensor_scalar(out=oacc[:, sub, :], in0=oc_t[:, sub, i, 0:64], scalar1=ww[:, sub, 0:1], scalar2=None, op0=ALU.mult, op1=ALU.bypass), r=[oc_t, ww], w=[oacc])
                S.op("dve", lambda e, sub=sub: e.scalar_tensor_tensor(out=oacc[:, sub, :], in0=oss[:, sub, 0:64], scalar=ww[:, sub, 1:2], in1=oacc[:, sub, :], op0=ALU.mult, op1=ALU.add), r=[oss, ww, oacc], w=[oacc])
                S.op("dve", lambda e, sub=sub: e.scalar_tensor_tensor(out=ob[:, sub, :], in0=ps_ow[:, sub, 0:64], scalar=ww[:, sub, 2:3], in1=oacc[:, sub, :], op0=ALU.mult, op1=ALU.add), r=[ps_ow, ww, oacc], w=[ob])
            for sub in range(4):
                S.op("pe", lambda e, sub=sub: e.transpose(out=ps_t[0:64, sub * 128:(sub + 1) * 128], in_=ob[:, sub, :], identity=ident[:]), r=[ob, ident], w=[ps_t])
            o_s = ost[(qt * 2 + i) % 2]
            S.op("act", lambda e, o_s=o_s: e.activation(out=o_s[:], in_=ps_t[0:64, :], func=AF.Copy), r=[ps_t], w=[o_s])
            S.dma("sp", A["oT"][i * 64:(i + 1) * 64, qsl], o_s[:], r=[o_s], is_output=True)
        for _i in range(2):
            _head(_i)
    for _q in range(NQT):
        _qt(_q)


NSA_IN = dict(qnT=lambda T: ([4, 64, T], BF16), kcT=lambda T: ([64, T], BF16), vcT=lambda T: ([64, T], BF16),
              ksT=lambda T: ([64, T], BF16), kwT=lambda T: ([64, T], BF16), vs=lambda T: ([T, 64], BF16), vw=lambda T: ([T, 64], BF16),
              gl=lambda T: ([T, 6], BF16), posc=lambda T: ([1, 512], I32),
              w1k=lambda T: ([2048, 256], F32), w1v=lambda T: ([2048, 256], F32), w2k=lambda T: ([256, 64], F32), w2v=lambda T: ([256, 64], F32),
              posk=lambda T: ([32, 64], F32), posv=lambda T: ([32, 64], F32))


def load_csts_extra(S, nc, arrs):
    out = {}
    for n, arr in arrs.items():
        dt = BF16 if arr.dtype == NPBF else F32
        ap = din(nc, "c_" + n, arr.shape, dt)
        t = S.sb(list(arr.shape), dt, n)
        S.dma("sp", t[:], ap, w=[t])
        out[n] = t
    return out, {"c_" + n: a for n, a in arrs.items()}


def build_nsa(T, dbg_qs=None):
    nc = new_nc()
    A = {}
    if dbg_qs is not None:
        A["dbg_qs"] = dbg_qs
        for n, shp in dict(dbg_kcmp=[64, 512], dbg_vcmp=[128, 4, 65], dbg_imp=[128, 128], dbg_score=[128, 128], dbg_sel=[128, 128], dbg_m8=[128, 16], dbg_ps4=[128, 512],
                           dbg_hid=[2, 128, 2, 512], dbg_b1=[2, 128, 2], dbg_u1=[64, 512], dbg_u2=[64, 512], dbg_cc=[64, 512], dbg_sc=[64, 512], dbg_ow=[128, 4, 65], dbg_os=[128, 4, 65], dbg_oc=[128, 4, 2, 65], dbg_ww=[128, 4, 3]).items():
            A[n] = dout(nc, n, shp, F32)
    for n, f in NSA_IN.items():
        shp, dt = f(T)
        A[n] = din(nc, n, shp, dt)
    A["oT"] = dout(nc, "oT", [128, T], BF16)
    with ExitStack() as st:
        S = Sched(nc, st)
        cst, cmap = load_csts(S, nc, ["ident", "pswap", "ropec", "mle", "mwin"])
        c2, cmap2 = load_csts_extra(S, nc, nsa_consts(T))
        cst.update(c2); cmap.update(cmap2)
        emit_nsa(S, T, A, cst)
        S.finish(); S.emit()
    return nc, cmap


def build_mod():
    nc = new_nc()
    cT = din(nc, "cT", [128, 8, 2], F32); w = din(nc, "w", [2, D, 768], F32); bias = din(nc, "bias", [2, 768], F32)
    out = dout(nc, "modp", [2, 2, 768], F32)
    with ExitStack() as st:
        S = Sched(nc, st)
        ct = S.sb([128, 8, 2], F32, "ct"); cond = S.sb([128, 8, 2], F32, "cond")
        S.dma("sp", ct[:], cT, w=[ct])
        S.op("act", lambda e: e.activation(out=cond[:], in_=ct[:], func=AF.Silu), r=[ct], w=[cond])
        ps = [S.ps([128, 512], F32, "ps") for _ in range(2)]
        for l in range(2):
            W = S.sb([128, 8, 768], F32, "W")
            S.dma("sp", W[:], w[l].rearrange("(k p) c -> p k c", p=128), w=[W])
            bt = S.sb([2, 768], F32, "bt")
            S.dma("sp", bt[:], bias[l:l + 1, :].partition_broadcast(2), w=[bt])
            ot = S.sb([2, 768], F32, "ot")
            for half in range(2):
                p = ps[half]
                for k in range(8):
                    S.op("pe", lambda e, p=p, k=k, half=half, W=W: e.matmul(p[0:2, 0:384], lhsT=cond[:, k, :], rhs=W[:, k, half * 384:(half + 1) * 384], start=(k == 0), stop=(k == 7)), r=[cond, W], w=[p])
                S.op("dve", lambda e, p=p, half=half, ot=ot, bt=bt: e.tensor_add(out=ot[:, half * 384:(half + 1) * 384], in0=p[0:2, 0:384], in1=bt[:, half * 384:(half + 1) * 384]), r=[p, bt], w=[ot])
            S.dma("sp", out[l], ot[:], r=[ot], is_output=True)
        S.finish(); S.emit()
    return nc, {}


def _run(nc, in_maps):
    res = run_bass_kernel_spmd(nc, in_maps, core_ids=list(range(8)))
    return res.results


def kernel(x, c, positions, mod_w, mod_b, norm_mix, norm_ffn, ffn_w_gate, ffn_w_up, ffn_conv_w, ffn_conv_b, ffn_w_down,
           hyb_w_in, nsa_pos_k, nsa_pos_v, nsa_ck_w1, nsa_ck_w2, nsa_cv_w1, nsa_cv_w2, hyb_w_out, diff_w_qkv,
           diff_lq1, diff_lk1, diff_lq2, diff_lk2, diff_subln, diff_w_out, norm_f):
    f32 = lambda a: np.ascontiguousarray(np.asarray(a), dtype=np.float32)
    x = f32(x); c = f32(c); positions = np.ascontiguousarray(np.asarray(positions), dtype=np.int32)
    B, T, _ = x.shape
    TL = T // 4
    ca = np.ascontiguousarray
    nc, cm = build_mod()
    cT = ca(c.T.reshape(8, 128, 2).transpose(1, 0, 2))
    mw = f32(mod_w); mb = f32(mod_b)
    r = _run(nc, [dict(cT=cT, w=ca(mw[:, :, i * 768:(i + 1) * 768]), bias=ca(mb[:, i * 768:(i + 1) * 768])) for i in range(8)])
    mod = np.concatenate([r[i]["modp"] for i in range(8)], axis=-1)
    sh_m, sc_m, g_m, sh_f, sc_f, g_f = [mod[..., k * D:(k + 1) * D] for k in range(6)]
    nmix = f32(norm_mix); nffn = f32(norm_ffn)

    def run_pre(layer, xin, w):
        nc, cm = build_pre(TL, layer)
        maps = []
        for i in range(8):
            b, j = i // 4, i % 4
            maps.append(dict(x=ca(xin[b, j * TL:(j + 1) * TL]), pos=ca(positions[b:b + 1, j * TL:(j + 1) * TL]),
                             mv=ca(np.stack([nmix[layer], sc_m[layer, b], sh_m[layer, b]])), w=w, **cm))
        r = _run(nc, maps)
        FM = [np.concatenate([r[b * 4 + j]["fm"] for j in range(4)], axis=2) for b in range(B)]
        TM = [np.concatenate([r[b * 4 + j]["tm"] for j in range(4)], axis=0) for b in range(B)]
        return FM, TM

    def run_post(layer, OT, xin, wo, final):
        nc, cm = build_post(TL, final)
        maps = []
        for i in range(8):
            b, j = i // 4, i % 4
            oT = np.zeros((D, TL + 2), NPBF); xe = np.zeros((TL + 2, D), np.float32)
            lo = j * TL - 2
            if j > 0:
                oT[:] = OT[b][:, lo:lo + TL + 2]; xe[:] = xin[b, lo:lo + TL + 2]
            else:
                oT[:, 2:] = OT[b][:, 0:TL]; xe[2:] = xin[b, 0:TL]
            m = dict(oT=oT, x=xe, wo=wo, mvec=ca(np.stack([g_m[layer, b], g_f[layer, b]])),
                     mvf=ca(np.stack([nffn[layer], sc_f[layer, b], sh_f[layer, b]])),
                     wg=f32(ffn_w_gate[layer]), wu=f32(ffn_w_up[layer]), wd=f32(ffn_w_down[layer]),
                     cw=f32(ffn_conv_w[layer]), cb=f32(ffn_conv_b[layer])[None, :], flag=np.array([[1.0 if j > 0 else 0.0]], np.float32), **cm)
            if final:
                m["fg"] = f32(norm_f)[None, :]
            maps.append(m)
        r = _run(nc, maps)
        return np.stack([np.concatenate([r[b * 4 + j]["xo"] for j in range(4)], axis=0) for b in range(B)])

    FM, TM = run_pre(0, x, f32(hyb_w_in[0]))
    nc, cm = build_nsa(T)
    maps = []
    NCc = (T - 32) // 16 + 1
    for i in range(8):
        b, hg = i // 4, i % 4
        g = hg // 2
        own = [2 * hg, 2 * hg + 1]
        order = own + [h for h in range(4 * g, 4 * g + 4) if h not in own]
        posc = np.zeros((1, 512), np.int32); posc[0, :NCc] = positions[b, 31::16][:NCc]
        gs = slice(64 * g, 64 * g + 64)
        maps.append(dict(qnT=ca(np.stack([FM[b][h // 2, (h % 2) * 64:(h % 2) * 64 + 64] for h in order])),
                         kcT=ca(FM[b][4, gs]), vcT=ca(FM[b][5, gs]), ksT=ca(FM[b][6, gs]), kwT=ca(FM[b][7, gs]),
                         vs=ca(TM[b][:, 64 * g:64 * g + 64]), vw=ca(TM[b][:, 128 + 64 * g:128 + 64 * g + 64]),
                         gl=ca(TM[b][:, 256 + 3 * own[0]:256 + 3 * own[0] + 6]), posc=posc,
                         w1k=f32(nsa_ck_w1[0]), w1v=f32(nsa_cv_w1[0]), w2k=f32(nsa_ck_w2[0]), w2v=f32(nsa_cv_w2[0]),
                         posk=f32(nsa_pos_k[0]), posv=f32(nsa_pos_v[0]), **cm))
    r_nsa = _run(nc, maps)
    nc, cm = build_sb(T)
    maps = []
    for i in range(8):
        b, hg = i // 4, i % 4
        maps.append(dict(qT=ca(FM[b][8 + hg].reshape(2, 64, T)), kT=ca(FM[b][12 + hg].reshape(2, 64, T)),
                         v=ca(TM[b][:, 280 + 128 * hg:280 + 128 * hg + 128]), **cm))
    r_sb = _run(nc, maps)
    OT = []
    for b in range(B):
        OT.append(np.concatenate([r_nsa[b * 4 + hg]["oT"] for hg in range(4)] + [r_sb[b * 4 + hg]["oT"] for hg in range(4)], axis=0))
    x1 = run_post(0, OT, x, f32(hyb_w_out[0]), False)
    FM, TM = run_pre(1, x1, f32(diff_w_qkv[0]))
    nc, cm = build_diff(T)
    lam = ca(np.stack([f32(diff_lq1[0]), f32(diff_lk1[0]), f32(diff_lq2[0]), f32(diff_lk2[0])]))
    maps = []
    for i in range(8):
        b, hg = i // 4, i % 4
        maps.append(dict(qT=ca(FM[b][2 * hg:2 * hg + 2].reshape(4, 64, T)), kT=ca(FM[b][8 + 2 * hg:8 + 2 * hg + 2].reshape(4, 64, T)),
                         v=ca(TM[b][:, 256 * hg:256 * hg + 256]), lam=lam, subln=f32(diff_subln[0])[None, :], **cm))
    r_d = _run(nc, maps)
    OT = [np.concatenate([r_d[b * 4 + hg]["oT"] for hg in range(4)], axis=0) for b in range(B)]
    out = run_post(1, OT, x1, f32(diff_w_out[0]), True)
    return out.astype(np.float32)
```

```python
import math
from contextlib import ExitStack
import numpy as np
import ml_dtypes
import concourse.bass as bass
import concourse.mybir as mybir
from concourse.bass_utils import run_bass_kernel_spmd

F32 = mybir.dt.float32
BF16 = mybir.dt.bfloat16
I32 = mybir.dt.int32
AF = mybir.ActivationFunctionType
ALU = mybir.AluOpType
AX = mybir.AxisListType
NPBF = ml_dtypes.bfloat16

SAME_ENGINE_SYNC = True
N_DMA_SEMS = 16


class Res:
    __slots__ = ("name", "w", "rs")

    def __init__(self, name):
        self.name = name
        self.w = None
        self.rs = []


class Tile:
    def __init__(self, h, name):
        self.h = h
        self.r = Res(name)
        self._subs = {}
        self.name = name

    def __getitem__(self, k):
        return self.h[k]

    def sub(self, key):
        s = self._subs.get(key)
        if s is None:
            s = Res(f"{self.name}/{key}")
            self._subs[key] = s
        return s


class Sched:
    ENG = ("pe", "act", "dve", "pool", "sp")

    def __init__(self, nc, stack):
        self.nc = nc
        self.stack = stack
        self.ops = {e: [] for e in self.ENG}
        self.cnt = {e: 0 for e in self.ENG}
        self.known = {e: {} for e in self.ENG}
        self.dma_cnt = [0] * N_DMA_SEMS
        self.dma_rr = 0
        self.sems = {}
        for e in ("pe", "act", "dve", "pool"):
            self.sems[e] = stack.enter_context(nc.semaphore("s_" + e))
        for i in range(N_DMA_SEMS):
            self.sems[("d", i)] = stack.enter_context(nc.semaphore(f"s_d{i}"))
        self.out_events = []
        self.n_names = 0

    def sb(self, shape, dtype, name=None):
        self.n_names += 1
        name = f"{name or 't'}_{self.n_names}"
        h = self.stack.enter_context(self.nc.sbuf_tensor(name, list(shape), dtype))
        return Tile(h, name)

    def ps(self, shape, dtype, name=None):
        self.n_names += 1
        name = f"{name or 'p'}_{self.n_names}"
        h = self.stack.enter_context(self.nc.psum_tensor(name, list(shape), dtype))
        return Tile(h, name)

    def _deps(self, eng, r, w):
        deps = {}
        def add(ev):
            if ev is None:
                return
            k, v = ev
            if deps.get(k, 0) < v:
                deps[k] = v
        for x in r:
            add(x.w)
        for x in w:
            add(x.w)
            for ev in x.rs:
                add(ev)
        waits = []
        kn = self.known[eng]
        for k, v in deps.items():
            if k == eng and (eng == "pe" or not SAME_ENGINE_SYNC):
                continue
            if kn.get(k, 0) >= v:
                continue
            kn[k] = v
            waits.append((k, v))
        return waits

    def _commit(self, ev, r, w):
        for x in r:
            x.rs.append(ev)
        for x in w:
            x.w = ev
            x.rs = []

    @staticmethod
    def _res(lst):
        out = []
        for x in lst:
            out.append(x.r if isinstance(x, Tile) else x)
        return out

    def op(self, eng, fn, r=(), w=()):
        r = self._res(r)
        w = self._res(w)
        waits = self._deps(eng, r, w)
        self.cnt[eng] += 1
        ev = (eng, self.cnt[eng])
        self.ops[eng].append((waits, fn, (eng, 1)))
        self._commit(ev, r, w)
        return ev

    def dma(self, eng, out, in_, r=(), w=(), is_output=False, **kw):
        r = self._res(r)
        w = self._res(w)
        waits = self._deps(eng, r, w)
        si = self.dma_rr
        self.dma_rr = (self.dma_rr + 1) % N_DMA_SEMS
        key = ("d", si)
        prev = 16 * self.dma_cnt[si]
        kn = self.known[eng]
        if prev > 0 and kn.get(key, 0) < prev:
            kn[key] = prev
            waits.append((key, prev))
        self.dma_cnt[si] += 1
        ev = (key, 16 * self.dma_cnt[si])
        def fn(e, out=out, in_=in_, kw=kw):
            return e.dma_start(out=out, in_=in_, **kw)
        self.ops[eng].append((waits, fn, (key, 16)))
        self._commit(ev, r, w)
        if is_output:
            self.out_events.append(ev)
        return ev

    def dma_fn(self, eng, fn, r=(), w=(), is_output=False):
        r = self._res(r); w = self._res(w)
        waits = self._deps(eng, r, w)
        si = self.dma_rr
        self.dma_rr = (self.dma_rr + 1) % N_DMA_SEMS
        key = ("d", si)
        prev = 16 * self.dma_cnt[si]
        kn = self.known[eng]
        if prev > 0 and kn.get(key, 0) < prev:
            kn[key] = prev
            waits.append((key, prev))
        self.dma_cnt[si] += 1
        ev = (key, 16 * self.dma_cnt[si])
        self.ops[eng].append((waits, fn, (key, 16)))
        self._commit(ev, r, w)
        if is_output:
            self.out_events.append(ev)
        return ev

    def cc(self, kind, ins, outs, groups, r=(), w=()):
        def fn(e):
            return e.collective_compute(kind, ALU.bypass, replica_groups=groups, ins=list(ins), outs=list(outs))
        return self.dma_fn("pool", fn, r=r, w=w)

    def barrier(self):
        evs = [(e, self.cnt[e]) for e in ("pe", "act", "dve", "pool") if self.cnt[e] > 0]
        evs += [(("d", i), 16 * self.dma_cnt[i]) for i in range(N_DMA_SEMS) if self.dma_cnt[i] > 0]
        for eng in self.ENG:
            kn = self.known[eng]
            waits = []
            for (k, v) in evs:
                if kn.get(k, 0) >= v:
                    continue
                kn[k] = v
                waits.append((k, v))
            if waits:
                self.ops[eng].append((waits, None, None))

    def finish(self):
        final = {}
        for (k, v) in self.out_events:
            if final.get(k, 0) < v:
                final[k] = v
        waits = [(k, v) for k, v in final.items()]
        self.ops["sp"].append((waits, None, None))

    def emit(self):
        nc = self.nc
        sems = self.sems
        ops = self.ops
        def run(engname, e):
            for (waits, fn, inc) in ops[engname]:
                for (k, v) in waits:
                    e.wait_ge(sems[k], v)
                if fn is not None:
                    ins = fn(e)
                    ins.then_inc(sems[inc[0]], inc[1])
        with nc.Block() as block:
            @block.tensor
            def _(e):
                run("pe", e)
            @block.scalar
            def _(e):
                run("act", e)
            @block.vector
            def _(e):
                run("dve", e)
            @block.gpsimd
            def _(e):
                run("pool", e)
            @block.sync
            def _(e):
                run("sp", e)


D = 1024
DFF = 2816
NFC = DFF // 128
EPS = 1e-6
NEG = -30000.0
ROPE_THETA = 500000.0
LAMBDA_INIT = 0.8 - 0.6 * math.exp(-0.3 * 1)
C1 = 6.28125
C2 = 2 * math.pi - 6.28125


def new_nc():
    return bass.Bass("TRN2", target_bir_lowering=False)


def din(nc, name, shape, dt):
    return nc.dram_tensor(name, list(shape), dt, kind="ExternalInput").ap()


def dout(nc, name, shape, dt):
    return nc.dram_tensor(name, list(shape), dt, kind="ExternalOutput").ap()


def host_consts():
    c = {}
    c["ident"] = np.eye(128, dtype=np.float32).astype(NPBF)
    p = np.arange(128)
    sw = np.zeros((128, 128), np.float32)
    for m in range(128):
        r = m % 64
        if r < 8:
            sw[m + 8, m] = 1
        elif r < 16:
            sw[m - 8, m] = 1
    c["pswap"] = sw.astype(NPBF)
    rc = np.zeros((128, 2), np.float32)
    for m in range(128):
        r = m % 64
        if r < 16:
            rc[m, 0] = ROPE_THETA ** (-(2 * (r % 8)) / 16.0)
            rc[m, 1] = -1.0 if r < 8 else 1.0
    c["ropec"] = rc
    n = np.arange(512)[None, :]
    pp = p[:, None]
    mle = np.stack([np.where(n >= 128 * d + pp, 0.0, NEG) for d in range(4)])
    mlt = np.stack([np.where(n > 128 * d + pp, 0.0, NEG) for d in range(4)])
    mwin = np.stack([np.where(n < 128 * d + pp, 0.0, NEG) for d in range(4)])
    c["mle"] = mle.astype(NPBF)
    c["mlt"] = mlt.astype(NPBF)
    c["mwin"] = mwin.astype(NPBF)
    tri = np.where(p[:, None] >= p[None, :], -1.0, 0.0)
    c["negtri"] = tri.astype(NPBF)
    c["negones"] = (-np.ones((128, 128), np.float32)).astype(NPBF)
    return c


def rope_tables(S, pos_ap, ntok, ropec, name):
    Ct = S.sb([128, ntok], F32, name + "C")
    St = S.sb([128, ntok], F32, name + "S")
    with ExitStack() as st:
        old = S.stack
        S.stack = st
        posi = S.sb([128, ntok], I32, "posi")
        ang = S.sb([128, ntok], F32, "ang")
        u = S.sb([128, ntok], F32, "u")
        ki = S.sb([128, ntok], I32, "ki")
        kf = S.sb([128, ntok], F32, "kf")
        S.dma("sp", posi[:], pos_ap.partition_broadcast(128), w=[posi])
        S.op("dve", lambda e: e.tensor_copy(out=ang[:], in_=posi[:]), r=[posi], w=[ang])
        S.op("dve", lambda e: e.tensor_scalar(out=ang[:], in0=ang[:], scalar1=ropec[:, 0:1], scalar2=None, op0=ALU.mult, op1=ALU.bypass), r=[ang, ropec], w=[ang])
        for (off, dst, sgn) in ((0.5 * math.pi, Ct, False), (0.0, St, True)):
            S.op("dve", lambda e, off=off: e.tensor_single_scalar(out=u[:], in_=ang[:], scalar=off, op=ALU.add), r=[ang], w=[u])
            S.op("dve", lambda e: e.tensor_single_scalar(out=ki[:], in_=u[:], scalar=1.0 / (2 * math.pi), op=ALU.mult), r=[u], w=[ki])
            S.op("dve", lambda e: e.tensor_copy(out=kf[:], in_=ki[:]), r=[ki], w=[kf])
            S.op("dve", lambda e: e.scalar_tensor_tensor(out=u[:], in0=kf[:], scalar=-C1, in1=u[:], op0=ALU.mult, op1=ALU.add), r=[kf, u], w=[u])
            S.op("dve", lambda e: e.scalar_tensor_tensor(out=u[:], in0=kf[:], scalar=-C2, in1=u[:], op0=ALU.mult, op1=ALU.add), r=[kf, u], w=[u])
            S.op("dve", lambda e: e.tensor_scalar(out=kf[:], in0=u[:], scalar1=math.pi, scalar2=2 * math.pi, op0=ALU.is_gt, op1=ALU.mult), r=[u], w=[kf])
            S.op("dve", lambda e: e.tensor_sub(out=u[:], in0=u[:], in1=kf[:]), r=[u, kf], w=[u])
            S.op("dve", lambda e: e.tensor_scalar(out=u[:], in0=u[:], scalar1=math.pi, scalar2=-math.pi, op0=ALU.min, op1=ALU.max), r=[u], w=[u])
            S.op("act", lambda e, dst=dst: e.activation(out=dst[:], in_=u[:], func=AF.Sin), r=[u], w=[dst])
            if sgn:
                S.op("dve", lambda e, dst=dst: e.tensor_scalar(out=dst[:], in0=dst[:], scalar1=ropec[:, 1:2], scalar2=None, op0=ALU.mult, op1=ALU.bypass), r=[dst, ropec], w=[dst])
        S.barrier()
        S.stack = old
    return Ct, St


def load_const(S, ap, shape, dt, name):
    t = S.sb(shape, dt, name)
    S.dma("sp", t[:], ap, w=[t])
    return t


def rstd_from_ssq(S, ssq, rstd, n, ntok=128, cols=None):
    sl = (slice(0, ntok), slice(None) if cols is None else cols)
    S.op("dve", lambda e: e.tensor_scalar(out=rstd[sl], in0=ssq[sl], scalar1=1.0 / n, scalar2=EPS, op0=ALU.mult, op1=ALU.add), r=[ssq], w=[rstd])
    S.op("act", lambda e: e.activation(out=rstd[sl], in_=rstd[sl], func=AF.Ln), r=[rstd], w=[rstd])
    S.op("act", lambda e: e.activation(out=rstd[sl], in_=rstd[sl], func=AF.Exp, scale=-0.5), r=[rstd], w=[rstd])


def load_weight_bf16(S, Wb, w_ap, nk, ncols, stage_cols=1024, col0=0, name="wst"):
    stg = [S.sb([128, stage_cols], F32, name) for _ in range(2)]
    i = 0
    for k in range(nk):
        for c0 in range(0, ncols, stage_cols):
            cw = min(stage_cols, ncols - c0)
            s = stg[i % 2]
            i += 1
            S.dma("sp", s[:, 0:cw], w_ap[k * 128:(k + 1) * 128, col0 + c0:col0 + c0 + cw], w=[s])
            S.op("pool", lambda e, s=s, k=k, c0=c0, cw=cw: e.tensor_copy(out=Wb[:, k, c0:c0 + cw], in_=s[:, 0:cw]), r=[s], w=[Wb])


def norm_to_hT(S, x_t, ntok, tok0, hT, a_t, sh_t, ident, scr):
    junk, ssq, rstd, xn, pT = scr
    S.op("act", lambda e: e.activation(out=junk[0:ntok, :], in_=x_t[0:ntok, :], func=AF.Square, accum_out=ssq[0:ntok, :]), r=[x_t], w=[junk, ssq])
    rstd_from_ssq(S, ssq, rstd, D, ntok)
    S.op("act", lambda e: e.activation(out=xn[0:ntok, :], in_=x_t[0:ntok, :], func=AF.Copy, scale=rstd[0:ntok, :]), r=[x_t, rstd], w=[xn])
    for k in range(8):
        S.op("pe", lambda e, k=k: e.transpose(out=pT[:, k * 128:k * 128 + ntok], in_=xn[0:ntok, k * 128:(k + 1) * 128], identity=ident[0:ntok, 0:ntok]), r=[xn, ident], w=[pT])
    for k in range(8):
        eng = "dve" if k % 2 == 0 else "pool"
        eng = "dve"
        S.op(eng, lambda e, k=k: e.tensor_scalar(out=hT[:, k, tok0:tok0 + ntok], in0=pT[:, k * 128:k * 128 + ntok], scalar1=a_t[:, k:k + 1], scalar2=sh_t[:, k:k + 1], op0=ALU.mult, op1=ALU.add), r=[pT, a_t, sh_t], w=[hT.sub(tok0)])


def norm_scratch(S):
    return [(S.sb([128, D], BF16, "junk"), S.sb([128, 1], F32, "ssq"), S.sb([128, 1], F32, "rstd"),
             S.sb([128, D], BF16, "xn"), S.ps([128, D], BF16, "pT")) for _ in range(2)]


def mod_vectors(S, mv_ap):
    mv = S.sb([128, 3, 8], F32, "mv")
    S.dma("sp", mv[:], mv_ap.rearrange("r (k p) -> p r k", p=128), w=[mv], allow_slow_non_contiguous=True)
    a = S.sb([128, 8], F32, "a")
    S.op("dve", lambda e: e.tensor_single_scalar(out=a[:], in_=mv[:, 1, :], scalar=1.0, op=ALU.add), r=[mv], w=[a])
    S.op("dve", lambda e: e.tensor_mul(out=a[:], in0=a[:], in1=mv[:, 0, :]), r=[a, mv], w=[a])
    sh = S.sb([128, 8], F32, "sh")
    S.op("dve", lambda e: e.tensor_copy(out=sh[:], in_=mv[:, 2, :]), r=[mv], w=[sh])
    return a, sh


def emit_pre(S, T_loc, colspec, NC, x_ap, pos_ap, mv_ap, w_ap, fm_ap, tm_ap, cst, x_res=None):
    NT = T_loc // 128
    TG = min(512, T_loc)
    NTG = T_loc // TG
    ident, pswap, ropec = cst["ident"], cst["pswap"], cst["ropec"]
    a, sh = mod_vectors(S, mv_ap)
    hT = S.sb([128, 8, T_loc], BF16, "hT")
    Wb = S.sb([128, 8, NC], BF16, "Wb")
    need_rope = any(c.get("rope") for c in colspec)
    if need_rope:
        Ct, St = rope_tables(S, pos_ap, T_loc, ropec, "rp")
    load_weight_bf16(S, Wb, w_ap, 8, NC)
    scr = norm_scratch(S)
    xt = [S.sb([128, D], F32, "xt") for _ in range(2)]
    for tt in range(NT):
        x_t = xt[tt % 2]
        S.dma("sp", x_t[:], x_ap[tt * 128:(tt + 1) * 128, :], r=[x_res] if x_res else [], w=[x_t])
        norm_to_hT(S, x_t, 128, tt * 128, hT, a, sh, ident, scr[tt % 2])
    hT_all = [hT.sub(tt * 128) for tt in range(NT)]
    psA = [S.ps([128, 512], F32, "psA") for _ in range(2)]
    psB = S.ps([128, 512], F32, "psB")
    xb = [S.sb([128, 512], BF16, "xb") for _ in range(2)]
    t1 = [S.sb([128, 512], F32, "t1") for _ in range(2)]
    t2 = [S.sb([128, 512], F32, "t2") for _ in range(2)]
    ob = [S.sb([128, 512], BF16, "ob") for _ in range(3)]
    it = 0
    fmi = 0
    tmoff = 0
    for c in colspec:
        if c["kind"] == "fm":
            c0 = c["col"]
            for tg in range(NTG):
                it += 1
                ps = psA[it % 2]
                tsl = slice(tg * TG, (tg + 1) * TG)
                for k in range(8):
                    S.op("pe", lambda e, ps=ps, k=k, c0=c0, tsl=tsl: e.matmul(ps[:, 0:TG], lhsT=Wb[:, k, c0:c0 + 128], rhs=hT[:, k, tsl], start=(k == 0), stop=(k == 7)),
                         r=[Wb] + hT_all[tg * (TG // 128):(tg + 1) * (TG // 128)], w=[ps])
                o = ob[it % 3]
                if not c.get("rope"):
                    S.op("act", lambda e, ps=ps, o=o, sc=c.get("scale", 1.0): e.activation(out=o[:, 0:TG], in_=ps[:, 0:TG], func=AF.Copy, scale=sc), r=[ps], w=[o])
                else:
                    b = xb[it % 2]; u1 = t1[it % 2]; u2 = t2[it % 2]
                    S.op("act", lambda e, ps=ps, b=b: e.activation(out=b[:, 0:TG], in_=ps[:, 0:TG], func=AF.Copy), r=[ps], w=[b])
                    S.op("pe", lambda e, b=b: e.matmul(psB[:, 0:TG], lhsT=pswap[:], rhs=b[:, 0:TG], start=True, stop=True), r=[b, pswap], w=[psB])
                    S.op("dve", lambda e, b=b, u1=u1, tsl=tsl: e.tensor_mul(out=u1[:, 0:TG], in0=b[:, 0:TG], in1=Ct[:, tsl]), r=[b, Ct], w=[u1])
                    S.op("dve", lambda e, u2=u2, tsl=tsl: e.tensor_mul(out=u2[:, 0:TG], in0=psB[:, 0:TG], in1=St[:, tsl]), r=[psB, St], w=[u2])
                    S.op("pool", lambda e, o=o, u1=u1, u2=u2: e.tensor_add(out=o[:, 0:TG], in0=u1[:, 0:TG], in1=u2[:, 0:TG]), r=[u1, u2], w=[o])
                S.dma("sp", fm_ap[fmi, :, tsl], o[:, 0:TG], r=[o], is_output=True)
            fmi += 1
        else:
            c0, n = c["col"], c["n"]
            for tt in range(NT):
                for cc in range(0, n, 512):
                    cw = min(512, n - cc)
                    it += 1
                    ps = psA[it % 2]
                    for k in range(8):
                        S.op("pe", lambda e, ps=ps, k=k, tt=tt, cc=cc, cw=cw, c0=c0: e.matmul(ps[:, 0:cw], lhsT=hT[:, k, tt * 128:(tt + 1) * 128], rhs=Wb[:, k, c0 + cc:c0 + cc + cw], start=(k == 0), stop=(k == 7)),
                             r=[Wb, hT_all[tt]], w=[ps])
                    o = ob[it % 3]
                    S.op("act", lambda e, ps=ps, o=o, cw=cw: e.activation(out=o[:, 0:cw], in_=ps[:, 0:cw], func=AF.Copy), r=[ps], w=[o])
                    S.dma("sp", tm_ap[tt * 128:(tt + 1) * 128, tmoff + cc:tmoff + cc + cw], o[:, 0:cw], r=[o], is_output=True)
            tmoff += n


def colspec_l0():
    cs = []
    for i in range(4):
        cs.append(dict(kind="fm", col=128 * i, rope=True))
    cs.append(dict(kind="fm", col=512))
    cs.append(dict(kind="fm", col=640))
    cs.append(dict(kind="fm", col=768, rope=True))
    cs.append(dict(kind="fm", col=1024, rope=True))
    for i in range(4):
        cs.append(dict(kind="fm", col=1304 + 128 * i, scale=0.125))
    for i in range(4):
        cs.append(dict(kind="fm", col=1816 + 128 * i))
    cs.append(dict(kind="tm", col=896, n=128))
    cs.append(dict(kind="tm", col=1152, n=128))
    cs.append(dict(kind="tm", col=1280, n=24))
    cs.append(dict(kind="tm", col=2328, n=512))
    return cs, 16, 792


def colspec_l1():
    cs = []
    for i in range(8):
        cs.append(dict(kind="fm", col=128 * i, rope=True))
    for i in range(8):
        cs.append(dict(kind="fm", col=1024 + 128 * i, rope=True))
    cs.append(dict(kind="tm", col=2048, n=1024))
    return cs, 16, 1024


def load_csts(S, nc, names):
    hc = host_consts()
    out = {}
    for n in names:
        arr = hc[n]
        dt = BF16 if arr.dtype == NPBF else F32
        ap = din(nc, "c_" + n, arr.shape, dt)
        if arr.ndim == 3:
            t = S.sb([arr.shape[1], arr.shape[0], arr.shape[2]], dt, n)
            S.dma("sp", t[:], ap.rearrange("d p n -> p d n"), w=[t])
        else:
            t = S.sb(list(arr.shape), dt, n)
            S.dma("sp", t[:], ap, w=[t])
        out[n] = t
    return out, {"c_" + n: hc[n] for n in names}


def build_pre(T_loc, layer):
    cs, nfm, ntm = colspec_l0() if layer == 0 else colspec_l1()
    NC = 2840 if layer == 0 else 3072
    nc = new_nc()
    x = din(nc, "x", [T_loc, D], F32)
    pos = din(nc, "pos", [1, T_loc], I32)
    mv = din(nc, "mv", [3, D], F32)
    w = din(nc, "w", [D, NC], F32)
    fm = dout(nc, "fm", [nfm, 128, T_loc], BF16)
    tm = dout(nc, "tm", [T_loc, ntm], BF16)
    with ExitStack() as st:
        S = Sched(nc, st)
        cst, cmap = load_csts(S, nc, ["ident", "pswap", "ropec"])
        emit_pre(S, T_loc, cs, NC, x, pos, mv, w, fm, tm, cst)
        S.finish(); S.emit()
    return nc, cmap


def emit_post(S, T_loc, oT_ap, x_ap, wo_ap, mvec_ap, mvf_ap, wg_ap, wu_ap, wd_ap, cw_ap, cb_ap, flag_ap, xo_ap, cst, final_g_ap=None):
    TE = T_loc + 2
    NT = T_loc // 128
    ident = cst["ident"]
    tiles = [(0, 2)] + [(2 + 128 * i, 128) for i in range(NT)]
    a_f, sh_f = mod_vectors(S, mvf_ap)
    gm_b = S.sb([128, D], F32, "gm_b"); gf_b = S.sb([128, D], F32, "gf_b")
    S.dma("sp", gm_b[:], mvec_ap[0:1, :].partition_broadcast(128), w=[gm_b])
    S.dma("sp", gf_b[:], mvec_ap[1:2, :].partition_broadcast(128), w=[gf_b])
    if final_g_ap is not None:
        nf_b = S.sb([128, D], F32, "nf_b")
        S.dma("sp", nf_b[:], final_g_ap.partition_broadcast(128), w=[nf_b])
    flag = S.sb([128, 1], F32, "flag")
    S.dma("sp", flag[:], flag_ap.partition_broadcast(128), w=[flag])
    cw = S.sb([128, 3, NFC], F32, "cw"); cb = S.sb([128, NFC], F32, "cb")
    S.dma("sp", cw[:], cw_ap.rearrange("r (c p) -> p r c", p=128), w=[cw], allow_slow_non_contiguous=True)
    S.dma("sp", cb[:], cb_ap.rearrange("r (c p) -> p (r c)", p=128), w=[cb], allow_slow_non_contiguous=True)
    hT = S.sb([128, 8, TE], BF16, "hT")
    psA = [S.ps([128, 512], F32, "psA") for _ in range(2)]
    psB = [S.ps([128, 512], F32, "psB") for _ in range(2)]
    with ExitStack() as st:
        old = S.stack; S.stack = st
        oT = S.sb([128, 8, TE], BF16, "oT")
        S.dma("sp", oT[:], oT_ap.rearrange("(k p) t -> p k t", p=128), w=[oT])
        Wo = S.sb([128, 8, D], BF16, "Wo")
        load_weight_bf16(S, Wo, wo_ap, 8, D)
        scr = norm_scratch(S)
        xt = [S.sb([128, D], F32, "xt") for _ in range(2)]
        tmp = [S.sb([128, D], F32, "tmp") for _ in range(2)]
        for ti, (r0, n) in enumerate(tiles):
            x_t = xt[ti % 2]; t_t = tmp[ti % 2]
            S.dma("sp", x_t[0:n, :], x_ap[r0:r0 + n, :], w=[x_t])
            for half in range(2):
                ps = psA[half]
                for k in range(8):
                    S.op("pe", lambda e, ps=ps, k=k, r0=r0, n=n, half=half: e.matmul(ps[0:n, :], lhsT=oT[:, k, r0:r0 + n], rhs=Wo[:, k, half * 512:(half + 1) * 512], start=(k == 0), stop=(k == 7)), r=[oT, Wo], w=[ps])
                S.op("dve", lambda e, ps=ps, n=n, half=half, t_t=t_t: e.tensor_mul(out=t_t[0:n, half * 512:(half + 1) * 512], in0=ps[0:n, :], in1=gm_b[0:n, half * 512:(half + 1) * 512]), r=[ps, gm_b], w=[t_t])
            S.op("pool", lambda e, n=n, x_t=x_t, t_t=t_t: e.tensor_add(out=x_t[0:n, :], in0=x_t[0:n, :], in1=t_t[0:n, :]), r=[t_t, x_t], w=[x_t])
            if ti > 0:
                S.dma("sp", xo_ap[r0 - 2:r0 - 2 + n, :], x_t[0:n, :], r=[x_t], w=[S_xo(S)], is_output=True)
            norm_to_hT(S, x_t, n, r0, hT, a_f, sh_f, ident, scr[ti % 2])
        S.barrier()
        S.stack = old
    hT_all = [hT.sub(r0) for (r0, n) in tiles]
    actT = S.sb([128, NFC, T_loc], BF16, "actT")
    Wd = S.sb([128, NFC, D], BF16, "Wd")
    with ExitStack() as st:
        old = S.stack; S.stack = st
        wst = [S.sb([128, 8, 256], F32, "wst") for _ in range(1)]
        Wgu = [S.sb([128, 8, 256], BF16, "Wgu") for _ in range(2)]
        gx = [S.sb([128, 514], F32, "gx") for _ in range(2)]
        tc_ = [S.sb([128, 512], F32, "tc") for _ in range(2)]
        sg = [S.sb([128, 512], F32, "sg") for _ in range(2)]
        wdst = [S.sb([128, 512], F32, "wdst") for _ in range(1)]
        TG = min(512, T_loc)
        groups = [(0, 2)] + [(2 + TG * i, TG) for i in range(T_loc // TG)]
        it = 0
        for fc in range(NFC):
            ws = wst[0]; wb = Wgu[fc % 2]
            S.dma("sp", ws[:, :, 0:128], wg_ap[:, fc * 128:(fc + 1) * 128].rearrange("(k p) c -> p k c", p=128), w=[ws])
            S.dma("sp", ws[:, :, 128:256], wu_ap[:, fc * 128:(fc + 1) * 128].rearrange("(k p) c -> p k c", p=128), w=[ws])
            S.op("pool", lambda e, ws=ws, wb=wb: e.tensor_copy(out=wb[:], in_=ws[:]), r=[ws], w=[wb])
            wd_s = wdst[0]
            for hh in range(2):
                S.dma("sp", wd_s[:], wd_ap[fc * 128:(fc + 1) * 128, hh * 512:(hh + 1) * 512], w=[wd_s])
                S.op("pool", lambda e, wd_s=wd_s, fc=fc, hh=hh: e.tensor_copy(out=Wd[:, fc, hh * 512:(hh + 1) * 512], in_=wd_s[:]), r=[wd_s], w=[Wd.sub((fc, hh))])
            for gi, (r0, n) in enumerate(groups):
                it += 1
                pg = psA[it % 2]; pu = psB[it % 2]
                g = gx[it % 2]; gprev = gx[(it - 1) % 2]
                hdeps = [hT_all[i] for i, (tr0, tn) in enumerate(tiles) if tr0 >= r0 and tr0 < r0 + n]
                for k in range(8):
                    S.op("pe", lambda e, pg=pg, k=k, r0=r0, n=n, wb=wb: e.matmul(pg[:, 0:n], lhsT=wb[:, k, 0:128], rhs=hT[:, k, r0:r0 + n], start=(k == 0), stop=(k == 7)), r=[wb] + hdeps, w=[pg])
                if gi == 0:
                    gnext = gx[(it + 1) % 2]
                    S.op("dve", lambda e, pg=pg, gnext=gnext: e.tensor_scalar(out=gnext[:, 0:2], in0=pg[:, 0:2], scalar1=flag[:, 0:1], scalar2=None, op0=ALU.mult, op1=ALU.bypass), r=[pg, flag], w=[gnext])
                    continue
                for k in range(8):
                    S.op("pe", lambda e, pu=pu, k=k, r0=r0, n=n, wb=wb: e.matmul(pu[:, 0:n], lhsT=wb[:, k, 128:256], rhs=hT[:, k, r0:r0 + n], start=(k == 0), stop=(k == 7)), r=[wb] + hdeps, w=[pu])
                S.op("act", lambda e, pg=pg, g=g, n=n: e.activation(out=g[:, 2:2 + n], in_=pg[:, 0:n], func=AF.Copy), r=[pg], w=[g])
                if gi < len(groups) - 1:
                    gnext = gx[(it + 1) % 2]
                    S.op("pool", lambda e, g=g, gnext=gnext, n=n: e.tensor_copy(out=gnext[:, 0:2], in_=g[:, n:n + 2]), r=[g], w=[gnext])
                t = tc_[it % 2]; s = sg[it % 2]
                S.op("dve", lambda e, g=g, t=t, n=n, fc=fc: e.tensor_scalar(out=t[:, 0:n], in0=g[:, 2:2 + n], scalar1=cw[:, 2, fc:fc + 1], scalar2=cb[:, fc:fc + 1], op0=ALU.mult, op1=ALU.add), r=[g, cw, cb], w=[t])
                S.op("dve", lambda e, g=g, t=t, n=n, fc=fc: e.scalar_tensor_tensor(out=t[:, 0:n], in0=g[:, 1:1 + n], scalar=cw[:, 1, fc:fc + 1], in1=t[:, 0:n], op0=ALU.mult, op1=ALU.add), r=[g, cw, t], w=[t])
                S.op("dve", lambda e, g=g, t=t, n=n, fc=fc: e.scalar_tensor_tensor(out=t[:, 0:n], in0=g[:, 0:n], scalar=cw[:, 0, fc:fc + 1], in1=t[:, 0:n], op0=ALU.mult, op1=ALU.add), r=[g, cw, t], w=[t])
                S.op("act", lambda e, t=t, s=s, n=n: e.activation(out=s[:, 0:n], in_=t[:, 0:n], func=AF.Silu), r=[t], w=[s])
                S.op("dve", lambda e, s=s, pu=pu, n=n, fc=fc, r0=r0: e.tensor_mul(out=actT[:, fc, r0 - 2:r0 - 2 + n], in0=pu[:, 0:n], in1=s[:, 0:n]), r=[pu, s], w=[actT.sub((fc, r0))])
        S.barrier()
        S.stack = old
    xm = [S.sb([128, D], F32, "xm") for _ in range(2)]
    tmp = [S.sb([128, D], F32, "tmp2") for _ in range(2)]
    junk = S.sb([128, D], BF16, "junk2"); ssq = S.sb([128, 1], F32, "ssq2"); rstd = S.sb([128, 1], F32, "rstd2")
    for tt in range(NT):
        x_t = xm[tt % 2]; t_t = tmp[tt % 2]
        S.dma("sp", x_t[:], xo_ap[tt * 128:(tt + 1) * 128, :], r=[S_xo(S)], w=[x_t])
        for half in range(2):
            ps = psA[half]
            for fc in range(NFC):
                S.op("pe", lambda e, ps=ps, fc=fc, tt=tt, half=half: e.matmul(ps[:], lhsT=actT[:, fc, tt * 128:(tt + 1) * 128], rhs=Wd[:, fc, half * 512:(half + 1) * 512], start=(fc == 0), stop=(fc == NFC - 1)), r=[actT, Wd] + [Wd.sub((fc, half))], w=[ps])
            S.op("dve", lambda e, ps=ps, half=half, t_t=t_t: e.tensor_mul(out=t_t[:, half * 512:(half + 1) * 512], in0=ps[:], in1=gf_b[:, half * 512:(half + 1) * 512]), r=[ps, gf_b], w=[t_t])
        S.op("pool", lambda e, x_t=x_t, t_t=t_t: e.tensor_add(out=t_t[:], in0=x_t[:], in1=t_t[:]), r=[t_t, x_t], w=[t_t])
        if final_g_ap is not None:
            S.op("act", lambda e, t_t=t_t: e.activation(out=junk[:], in_=t_t[:], func=AF.Square, accum_out=ssq[:]), r=[t_t], w=[junk, ssq])
            rstd_from_ssq(S, ssq, rstd, D)
            S.op("dve", lambda e, t_t=t_t: e.scalar_tensor_tensor(out=t_t[:], in0=t_t[:], scalar=rstd[:, 0:1], in1=nf_b[:], op0=ALU.mult, op1=ALU.mult), r=[t_t, rstd, nf_b], w=[t_t])
        S.dma("sp", xo_ap[tt * 128:(tt + 1) * 128, :], t_t[:], r=[t_t], w=[S_xo(S)], is_output=True)


def S_xo(S):
    if not hasattr(S, "_xo"):
        S._xo = Res("xo_dram")
    return S._xo


def build_post(T_loc, final):
    nc = new_nc()
    oT = din(nc, "oT", [D, T_loc + 2], BF16); x = din(nc, "x", [T_loc + 2, D], F32)
    wo = din(nc, "wo", [D, D], F32); mvec = din(nc, "mvec", [2, D], F32); mvf = din(nc, "mvf", [3, D], F32)
    wg = din(nc, "wg", [D, DFF], F32); wu = din(nc, "wu", [D, DFF], F32); wd = din(nc, "wd", [DFF, D], F32)
    cw = din(nc, "cw", [3, DFF], F32); cb = din(nc, "cb", [1, DFF], F32); flag = din(nc, "flag", [1, 1], F32)
    fg = din(nc, "fg", [1, D], F32) if final else None
    xo = dout(nc, "xo", [T_loc, D], F32)
    with ExitStack() as st:
        S = Sched(nc, st)
        cst, cmap = load_csts(S, nc, ["ident"])
        emit_post(S, T_loc, oT, x, wo, mvec, mvf, wg, wu, wd, cw, cb, flag, xo, cst, final_g_ap=fg)
        S.finish(); S.emit()
    return nc, cmap


def emit_diff(S, T, qT_ap, kT_ap, v_ap, lam_ap, subln_ap, oT_ap, cst):
    NKB = T // 128
    NQT = T // 512
    ident, mle = cst["ident"], cst["mle"]
    qT = [S.sb([128, T], BF16, "qT") for _ in range(4)]
    kT = [S.sb([128, T], BF16, "kT") for _ in range(4)]
    for i in range(4):
        S.op("pool", lambda e, i=i: e.memset(qT[i][64:128, :], 0.0), w=[qT[i]])
        S.op("pool", lambda e, i=i: e.memset(kT[i][64:128, :], 0.0), w=[kT[i]])
        S.dma("sp", qT[i][0:64, :], qT_ap[i], w=[qT[i]])
        S.dma("sp", kT[i][0:64, :], kT_ap[i], w=[kT[i]])
    Va = S.sb([128, NKB, 2, 129], BF16, "Va")
    S.op("pool", lambda e: e.memset(Va[:], 1.0), w=[Va])
    for h in range(2):
        S.dma("sp", Va[:, :, h, 0:128], v_ap[:, h * 128:(h + 1) * 128].rearrange("(kb p) d -> p kb d", p=128), w=[Va])
    lv = S.sb([128, 4, 64], F32, "lv")
    S.dma("sp", lv[:], lam_ap.rearrange("a d -> (a d)").partition_broadcast(128).rearrange("p (a d) -> p a d", a=4), w=[lv])
    pr = S.sb([128, 2, 64], F32, "pr")
    S.op("dve", lambda e: e.tensor_mul(out=pr[:, 0, :], in0=lv[:, 0, :], in1=lv[:, 1, :]), r=[lv], w=[pr])
    S.op("dve", lambda e: e.tensor_mul(out=pr[:, 1, :], in0=lv[:, 2, :], in1=lv[:, 3, :]), r=[lv, pr], w=[pr])
    sm = S.sb([128, 2], F32, "sm")
    S.op("dve", lambda e: e.tensor_reduce(out=sm[:], in_=pr[:], axis=AX.X, op=ALU.add), r=[pr], w=[sm])
    S.op("act", lambda e: e.activation(out=sm[:], in_=sm[:], func=AF.Exp), r=[sm], w=[sm])
    nlam = S.sb([128, 1], F32, "nlam")
    S.op("dve", lambda e: e.tensor_sub(out=nlam[:], in0=sm[:, 1:2], in1=sm[:, 0:1]), r=[sm], w=[nlam])
    S.op("dve", lambda e: e.tensor_single_scalar(out=nlam[:], in_=nlam[:], scalar=-LAMBDA_INIT, op=ALU.add), r=[nlam], w=[nlam])
    gsub = S.sb([128, 128], F32, "gsub")
    S.dma("sp", gsub[:], subln_ap.partition_broadcast(128), w=[gsub])
    S.op("dve", lambda e: e.tensor_single_scalar(out=gsub[:], in_=gsub[:], scalar=1.0 - LAMBDA_INIT, op=ALU.mult), r=[gsub], w=[gsub])

    ps_s = [S.ps([128, 512], F32, "ps_s") for _ in range(2)]
    ps_o = [S.ps([128, 2, 129], F32, "ps_o") for _ in range(4)]
    ps_t = S.ps([128, 512], BF16, "ps_t")
    pT = [S.sb([128, 512], BF16, "pT") for _ in range(3)]
    osb = [S.sb([128, 4, 128], F32, "osb") for _ in range(2)]
    rl = S.sb([128, 8], F32, "rl")
    od = S.sb([128, 4, 128], F32, "od")
    junk = S.sb([128, 128], BF16, "junk")
    ssq = S.sb([128, 4], F32, "ssq")
    rstd = S.sb([128, 4], F32, "rstd")
    onb = S.sb([128, 4, 128], BF16, "onb")
    ost = [S.sb([128, 512], BF16, "ost") for _ in range(2)]
    itc = [0]
    for h in range(2):
        for qt in range(NQT):
            qsl = slice(qt * 512, (qt + 1) * 512)
            for j in range(2):
                q_t, k_t = qT[h * 2 + j], kT[h * 2 + j]
                nkb = 4 * qt + 4
                started = [False, False]
                def stage1(kb, j=j, q_t=q_t, k_t=k_t, qt=qt, qsl=qsl):
                    d = kb - 4 * qt
                    itc[0] += 1
                    ps = ps_s[itc[0] % 2]
                    p_t = pT[itc[0] % 3]
                    S.op("pe", lambda e: e.matmul(ps[:], lhsT=k_t[:, kb * 128:(kb + 1) * 128], rhs=q_t[:, qsl], start=True, stop=(d < 0)), r=[k_t, q_t], w=[ps])
                    if d >= 0:
                        S.op("pe", lambda e: e.matmul(ps[:], lhsT=ident[:], rhs=mle[:, d, :], start=False, stop=True), r=[ident, mle], w=[ps])
                    S.op("act", lambda e: e.activation(out=p_t[:], in_=ps[:], func=AF.Exp, scale=0.125), r=[ps], w=[p_t])
                    return p_t

                def stage2(kb, p_t, j=j, qt=qt, h=h, started=started):
                    d = kb - 4 * qt
                    for sub in range(max(d, 0), 4):
                        bank = ps_o[j * 2 + sub // 2]
                        st = not started[sub // 2]
                        started[sub // 2] = True
                        S.op("pe", lambda e, bank=bank, sub=sub, st=st: e.matmul(bank[:, sub % 2, :], lhsT=p_t[:, sub * 128:(sub + 1) * 128], rhs=Va[:, kb, h, :], start=st, stop=(kb == 4 * qt + sub), skip_group_check=True), r=[p_t, Va], w=[bank])

                nxt = stage1(0)
                for kb in range(nkb):
                    cur = nxt
                    if kb + 1 < nkb:
                        nxt = stage1(kb + 1)
                    stage2(kb, cur)
                for sub in range(4):
                    bank = ps_o[j * 2 + sub // 2]
                    c = j * 4 + sub
                    S.op("dve", lambda e, bank=bank, sub=sub, c=c: e.reciprocal(out=rl[:, c:c + 1], in_=bank[:, sub % 2, 128:129]), r=[bank], w=[rl])
                    S.op("dve", lambda e, bank=bank, sub=sub, c=c, j=j: e.tensor_scalar(out=osb[j][:, sub, :], in0=bank[:, sub % 2, 0:128], scalar1=rl[:, c:c + 1], scalar2=None, op0=ALU.mult, op1=ALU.bypass), r=[bank, rl], w=[osb[j]])
            S.op("dve", lambda e: e.scalar_tensor_tensor(out=od[:], in0=osb[1][:], scalar=nlam[:, 0:1], in1=osb[0][:], op0=ALU.mult, op1=ALU.add), r=[osb[0], osb[1], nlam], w=[od])
            for sub in range(4):
                S.op("act", lambda e, sub=sub: e.activation(out=junk[:], in_=od[:, sub, :], func=AF.Square, accum_out=ssq[:, sub:sub + 1]), r=[od], w=[junk, ssq])
            rstd_from_ssq(S, ssq, rstd, 128)
            for sub in range(4):
                S.op("dve", lambda e, sub=sub: e.scalar_tensor_tensor(out=onb[:, sub, :], in0=od[:, sub, :], scalar=rstd[:, sub:sub + 1], in1=gsub[:], op0=ALU.mult, op1=ALU.mult), r=[od, rstd, gsub], w=[onb])
            for sub in range(4):
                S.op("pe", lambda e, sub=sub: e.transpose(out=ps_t[:, sub * 128:(sub + 1) * 128], in_=onb[:, sub, :], identity=ident[:]), r=[onb, ident], w=[ps_t])
            o_s = ost[(h * NQT + qt) % 2]
            S.op("act", lambda e, o_s=o_s: e.activation(out=o_s[:], in_=ps_t[:], func=AF.Copy), r=[ps_t], w=[o_s])
            S.dma("sp", oT_ap[h * 128:(h + 1) * 128, qsl], o_s[:], r=[o_s], is_output=True)


def build_diff(T):
    nc = new_nc()
    qT = din(nc, "qT", [4, 64, T], BF16); kT = din(nc, "kT", [4, 64, T], BF16)
    v = din(nc, "v", [T, 256], BF16); lam = din(nc, "lam", [4, 64], F32); subln = din(nc, "subln", [1, 128], F32)
    oT = dout(nc, "oT", [256, T], BF16)
    with ExitStack() as st:
        S = Sched(nc, st)
        cst, cmap = load_csts(S, nc, ["ident", "mle"])
        emit_diff(S, T, qT, kT, v, lam, subln, oT, cst)
        S.finish(); S.emit()
    return nc, cmap


def emit_sb(S, T, qT_ap, kT_ap, v_ap, oT_ap, cst, banks=None):
    NKB = T // 128
    NQT = T // 512
    ident, mlt, negtri, negones = cst["ident"], cst["mlt"], cst["negtri"], cst["negones"]
    qT = [S.sb([128, T], BF16, "sqT") for _ in range(2)]
    kT = [S.sb([128, T], BF16, "skT") for _ in range(2)]
    for i in range(2):
        S.op("pool", lambda e, i=i: e.memset(qT[i][64:128, :], 0.0), w=[qT[i]])
        S.op("pool", lambda e, i=i: e.memset(kT[i][64:128, :], 0.0), w=[kT[i]])
        S.dma("sp", qT[i][0:64, :], qT_ap[i], w=[qT[i]])
        S.dma("sp", kT[i][0:64, :], kT_ap[i], w=[kT[i]])
    V = S.sb([128, NKB, 128], BF16, "sV")
    S.dma("sp", V[:], v_ap.rearrange("(kb p) d -> p kb d", p=128), w=[V])
    ps_z = [S.ps([128, 512], F32, "ps_z") for _ in range(2)]
    ps_c = [S.ps([128, 512], F32, "ps_c") for _ in range(2)]
    ps_o = [S.ps([128, 4, 64], F32, "ps_o") for _ in range(2)]
    ps_t = S.ps([128, 512], BF16, "ps_t")
    E = [S.sb([128, 512], F32, "E") for _ in range(2)]
    sp = [S.sb([128, 512], BF16, "sp") for _ in range(2)]
    Racc = [S.sb([128, 512], BF16, "Racc") for _ in range(2)]
    aT = [S.sb([128, 512], BF16, "aT") for _ in range(2)]
    ob = S.sb([128, 4, 64], BF16, "ob")
    ost = [S.sb([64, 512], BF16, "ost") for _ in range(2)]
    it = 0
    for j in range(2):
        for qt in range(NQT):
            qsl = slice(qt * 512, (qt + 1) * 512)
            po = ps_o[(j * NQT + qt) % 2]
            first_o = True
            ri = 0
            for kb in range(4 * qt + 3, -1, -1):
                d = kb - 4 * qt
                it += 1
                pz = ps_z[it % 2]; pc = ps_c[it % 2]; e_t = E[it % 2]; s_t = sp[it % 2]; a_t = aT[it % 2]
                first = (kb == 4 * qt + 3)
                def zmm(e, ps, last, kb=kb, qsl=qsl, d=d, j=j):
                    pass
                S.op("pe", lambda e, pz=pz, kb=kb, qsl=qsl, d=d, j=j: e.matmul(pz[:], lhsT=kT[j][:, kb * 128:(kb + 1) * 128], rhs=qT[j][:, qsl], start=True, stop=(d < 0)), r=[kT[j], qT[j]], w=[pz])
                if d >= 0:
                    S.op("pe", lambda e, pz=pz, d=d: e.matmul(pz[:], lhsT=ident[:], rhs=mlt[:, d, :], start=False, stop=True), r=[ident, mlt], w=[pz])
                S.op("act", lambda e, pz=pz, e_t=e_t: e.activation(out=e_t[:], in_=pz[:], func=AF.Exp), r=[pz], w=[e_t])
                S.op("act", lambda e, e_t=e_t, s_t=s_t: e.activation(out=s_t[:], in_=e_t[:], func=AF.Ln, bias=1.0), r=[e_t], w=[s_t])
                S.op("pe", lambda e, pc=pc, kb=kb, qsl=qsl, j=j: e.matmul(pc[:], lhsT=kT[j][:, kb * 128:(kb + 1) * 128], rhs=qT[j][:, qsl], start=True, stop=False), r=[kT[j], qT[j]], w=[pc])
                if d >= 0:
                    S.op("pe", lambda e, pc=pc, d=d: e.matmul(pc[:], lhsT=ident[:], rhs=mlt[:, d, :], start=False, stop=False), r=[ident, mlt], w=[pc])
                S.op("pe", lambda e, pc=pc, s_t=s_t, first=first: e.matmul(pc[:], lhsT=negtri[:], rhs=s_t[:], start=False, stop=first), r=[negtri, s_t], w=[pc])
                if not first:
                    rc = Racc[ri % 2]
                    S.op("pe", lambda e, pc=pc, rc=rc: e.matmul(pc[:], lhsT=negones[:], rhs=rc[:], start=False, stop=True), r=[negones, rc], w=[pc])
                    if kb > 0:
                        rn = Racc[(ri + 1) % 2]
                        S.op("pool", lambda e, rc=rc, rn=rn, s_t=s_t: e.tensor_add(out=rn[:], in0=rc[:], in1=s_t[:]), r=[rc, s_t], w=[rn])
                        ri += 1
                else:
                    rn = Racc[ri % 2]
                    S.op("pool", lambda e, rn=rn, s_t=s_t: e.tensor_copy(out=rn[:], in_=s_t[:]), r=[s_t], w=[rn])
                S.op("act", lambda e, pc=pc, a_t=a_t: e.activation(out=a_t[:], in_=pc[:], func=AF.Exp), r=[pc], w=[a_t])
                for sub in range(max(d, 0), 4):
                    S.op("pe", lambda e, po=po, sub=sub, a_t=a_t, kb=kb, st=first_o, j=j: e.matmul(po[:, sub, :], lhsT=a_t[:, sub * 128:(sub + 1) * 128], rhs=V[:, kb, j * 64:(j + 1) * 64], start=st, stop=(kb == 0), skip_group_check=True), r=[a_t, V], w=[po])
                    first_o = False
            S.op("act", lambda e, po=po: e.activation(out=ob[:], in_=po[:], func=AF.Copy), r=[po], w=[ob])
            for sub in range(4):
                S.op("pe", lambda e, sub=sub: e.transpose(out=ps_t[0:64, sub * 128:(sub + 1) * 128], in_=ob[:, sub, :], identity=ident[:]), r=[ob, ident], w=[ps_t])
            o_s = ost[(j * NQT + qt) % 2]
            S.op("dve", lambda e, o_s=o_s: e.tensor_copy(out=o_s[:], in_=ps_t[0:64, :]), r=[ps_t], w=[o_s])
            S.dma("sp", oT_ap[j * 64:(j + 1) * 64, qsl], o_s[:], r=[o_s], is_output=True)


def build_sb(T):
    nc = new_nc()
    qT = din(nc, "qT", [2, 64, T], BF16); kT = din(nc, "kT", [2, 64, T], BF16)
    v = din(nc, "v", [T, 128], BF16)
    oT = dout(nc, "oT", [128, T], BF16)
    with ExitStack() as st:
        S = Sched(nc, st)
        cst, cmap = load_csts(S, nc, ["ident", "mlt", "negtri", "negones"])
        emit_sb(S, T, qT, kT, v, oT, cst)
        S.finish(); S.emit()
    return nc, cmap


def nsa_consts(T):
    c = {}
    p = np.arange(128)[:, None]
    cc = np.arange(512)[None, :]
    c["mbase"] = (16.0 * cc + 31.0 - p).astype(np.float32)
    n = np.arange(128)[None, :]
    c["dmat"] = (n - (p >= 64)).astype(np.float32)
    c["col0"] = np.broadcast_to((n == 0), (128, 128)).astype(np.float32)
    key = np.arange(T)[None, :]
    c["eall"] = ((key // 64) == p).astype(np.float32).astype(NPBF)
    return c


def emit_nsa(S, T, A, cst):
    NKB = T // 128
    NQT = T // 512
    NCc = (T - 32) // 16 + 1
    NCB = (NCc + 127) // 128
    ident, pswap, ropec, mle, mwin = cst["ident"], cst["pswap"], cst["ropec"], cst["mle"], cst["mwin"]
    mbase, dmat, col0, eall = cst["mbase"], cst["dmat"], cst["col0"], cst["eall"]
    ps_s = [S.ps([128, 512], F32, "ps_s") for _ in range(2)]
    ps_os = S.ps([128, 4, 65], F32, "ps_os")
    ps_ow = S.ps([128, 4, 65], F32, "ps_ow")
    ps_oc = S.ps([128, 2, 65], F32, "ps_oc")
    ps_t = S.ps([128, 512], BF16, "ps_t")
    ps_m = S.ps([128, 512], F32, "ps_m")
    kcmpT = S.sb([128, 512], BF16, "kcmpT")
    vcmp = S.sb([128, 4, 65], BF16, "vcmp")
    S.op("pool", lambda e: e.memset(kcmpT[:], 0.0), w=[kcmpT])
    S.op("pool", lambda e: e.memset(vcmp[:], 1.0), w=[vcmp])
    with ExitStack() as st:
        old = S.stack; S.stack = st
        Cc, Sc = rope_tables(S, A["posc"], 512, ropec, "rc")
        def _cmp(which):
            xT = S.sb([64, T], BF16, "cxT")
            S.dma("sp", xT[:], A["kcT"] if which == 0 else A["vcT"], w=[xT])
            W1 = S.sb([64, 32, 256], BF16, "W1")
            w1st = [S.sb([64, 4, 256], F32, "w1st") for _ in range(2)]
            w1v = (A["w1k"] if which == 0 else A["w1v"]).rearrange("(l d) h -> d l h", d=64)
            for li in range(8):
                s = w1st[li % 2]
                S.dma("sp", s[:], w1v[:, li * 4:(li + 1) * 4, :], w=[s])
                S.op("pool", lambda e, s=s, li=li: e.tensor_copy(out=W1[:, li * 4:(li + 1) * 4, :], in_=s[:]), r=[s], w=[W1])
            posf = S.sb([64, 32], F32, "posf"); posb = S.sb([64, 32], BF16, "posb")
            S.dma("sp", posf[:], (A["posk"] if which == 0 else A["posv"]).rearrange("l d -> d l"), w=[posf], allow_slow_non_contiguous=True)
            S.op("dve", lambda e: e.tensor_copy(out=posb[:], in_=posf[:]), r=[posf], w=[posb])
            w2f = S.sb([128, 2, 64], F32, "w2f"); W2 = S.sb([128, 2, 64], BF16, "W2")
            S.dma("sp", w2f[:], (A["w2k"] if which == 0 else A["w2v"]).rearrange("(c p) d -> p c d", p=128), w=[w2f])
            S.op("dve", lambda e: e.tensor_copy(out=W2[:], in_=w2f[:]), r=[w2f], w=[W2])
            hidT = S.sb([128, 2, 512], BF16, "hidT")
            S.op("pool", lambda e: e.memset(hidT[:], 0.0), w=[hidT])
            b1 = S.sb([128, 2], F32, "b1")
            for hc in range(2):
                for l in range(32):
                    S.op("pe", lambda e, l=l, hc=hc: e.matmul(ps_m[:, 0:1], lhsT=W1[:, l, hc * 128:(hc + 1) * 128], rhs=posb[:, l:l + 1], start=(l == 0), stop=(l == 31)), r=[W1, posb], w=[ps_m])
                S.op("dve", lambda e, hc=hc: e.tensor_copy(out=b1[:, hc:hc + 1], in_=ps_m[:, 0:1]), r=[ps_m], w=[b1])
                ps = ps_s[hc]
                for l in range(32):
                    S.op("pe", lambda e, l=l, hc=hc, ps=ps: e.matmul(ps[:, 0:NCc], lhsT=W1[:, l, hc * 128:(hc + 1) * 128], rhs=xT[:, l:l + 16 * (NCc - 1) + 1:16], start=(l == 0), stop=(l == 31)), r=[W1, xT], w=[ps])
                S.op("act", lambda e, hc=hc, ps=ps: e.activation(out=hidT[:, hc, 0:NCc], in_=ps[:, 0:NCc], func=AF.Silu, bias=b1[:, hc:hc + 1]), r=[ps, b1], w=[hidT])
            if "dbg_kcmp" in A:
                hf = S.sb([128, 2, 512], F32, "hf")
                S.op("dve", lambda e: e.tensor_copy(out=hf[:], in_=hidT[:]), r=[hidT], w=[hf])
                S.dma("sp", A["dbg_hid"][which], hf[:], r=[hf], is_output=True)
                S.dma("sp", A["dbg_b1"][which], b1[:], r=[b1], is_output=True)
            if which == 0:
                ktok = S.sb([128, 4, 64], BF16, "ktok")
                S.op("pool", lambda e: e.memset(ktok[:], 0.0), w=[ktok])
                for cb in range(NCB):
                    nb = min(128, NCc - cb * 128)
                    for hc in range(2):
                        S.op("pe", lambda e, hc=hc, cb=cb, nb=nb: e.matmul(ps_m[0:nb, 0:64], lhsT=hidT[:, hc, cb * 128:cb * 128 + nb], rhs=W2[:, hc, :], start=(hc == 0), stop=(hc == 1)), r=[W2, hidT], w=[ps_m])
                    S.op("act", lambda e, cb=cb, nb=nb: e.activation(out=ktok[0:nb, cb, :], in_=ps_m[0:nb, 0:64], func=AF.Copy), r=[ps_m], w=[ktok])
                for cb in range(4):
                    S.op("pe", lambda e, cb=cb: e.transpose(out=ps_t[0:64, cb * 128:(cb + 1) * 128], in_=ktok[:, cb, :], identity=ident[:]), r=[ktok, ident], w=[ps_t])
                kb_ = S.sb([64, 512], BF16, "kb_"); u1 = S.sb([64, 512], F32, "u1"); u2 = S.sb([64, 512], F32, "u2")
                S.op("act", lambda e: e.activation(out=kb_[:], in_=ps_t[0:64, :], func=AF.Copy), r=[ps_t], w=[kb_])
                S.op("pe", lambda e: e.matmul(ps_s[0][:, :], lhsT=pswap[0:64, :], rhs=kb_[:], start=True, stop=True), r=[pswap, kb_], w=[ps_s[0]])
                S.op("dve", lambda e: e.tensor_mul(out=u1[:], in0=kb_[:], in1=Cc[0:64, :]), r=[kb_, Cc], w=[u1])
                S.op("dve", lambda e: e.tensor_mul(out=u2[:], in0=ps_s[0][0:64, :], in1=Sc[0:64, :]), r=[ps_s[0], Sc], w=[u2])
                S.op("pool", lambda e: e.tensor_add(out=kcmpT[0:64, 0:NCc], in0=u1[:, 0:NCc], in1=u2[:, 0:NCc]), r=[u1, u2], w=[kcmpT])
                if "dbg_kcmp" in A:
                    S.dma("sp", A["dbg_u1"], u1[:], r=[u1], is_output=True)
                    S.dma("sp", A["dbg_u2"], u2[:], r=[u2], is_output=True)
                    S.dma("sp", A["dbg_cc"], Cc[0:64, :], r=[Cc], is_output=True)
                    S.dma("sp", A["dbg_sc"], Sc[0:64, :], r=[Sc], is_output=True)
            else:
                for cb in range(NCB):
                    nb = min(128, NCc - cb * 128)
                    for hc in range(2):
                        S.op("pe", lambda e, hc=hc, cb=cb, nb=nb: e.matmul(ps_m[0:nb, 0:64], lhsT=hidT[:, hc, cb * 128:cb * 128 + nb], rhs=W2[:, hc, :], start=(hc == 0), stop=(hc == 1)), r=[W2, hidT], w=[ps_m])
                    S.op("act", lambda e, cb=cb, nb=nb: e.activation(out=vcmp[0:nb, cb, 0:64], in_=ps_m[0:nb, 0:64], func=AF.Copy), r=[ps_m], w=[vcmp])
            S.barrier()
        for _w in (0, 1):
            _cmp(_w)
        S.stack = old
    if "dbg_kcmp" in A:
        kcf = S.sb([64, 512], F32, "kcf"); vcf = S.sb([128, 4, 65], F32, "vcf")
        S.op("dve", lambda e: e.tensor_copy(out=kcf[:], in_=kcmpT[0:64, :]), r=[kcmpT], w=[kcf])
        S.op("dve", lambda e: e.tensor_copy(out=vcf[:], in_=vcmp[:]), r=[vcmp], w=[vcf])
        S.dma("sp", A["dbg_kcmp"], kcf[:], r=[kcf], is_output=True)
        S.dma("sp", A["dbg_vcmp"], vcf[:], r=[vcf], is_output=True)
    qn = [S.sb([128, T], BF16, "qn") for _ in range(4)]
    for i in range(4):
        S.op("pool", lambda e, i=i: e.memset(qn[i][64:128, :], 0.0), w=[qn[i]])
        S.dma("sp", qn[i][0:64, :], A["qnT"][i], w=[qn[i]])
    ksT = S.sb([128, T], BF16, "ksT"); kwT = S.sb([128, T], BF16, "kwT")
    S.op("pool", lambda e: e.memset(ksT[64:128, :], 0.0), w=[ksT]); S.op("pool", lambda e: e.memset(kwT[64:128, :], 0.0), w=[kwT])
    S.dma("sp", ksT[0:64, :], A["ksT"], w=[ksT]); S.dma("sp", kwT[0:64, :], A["kwT"], w=[kwT])
    vsa = S.sb([128, NKB, 65], BF16, "vsa"); vwa = S.sb([128, NKB, 65], BF16, "vwa")
    S.op("pool", lambda e: e.memset(vsa[:], 1.0), w=[vsa]); S.op("pool", lambda e: e.memset(vwa[:], 1.0), w=[vwa])
    S.dma("sp", vsa[:, :, 0:64], A["vs"].rearrange("(kb p) d -> p kb d", p=128), w=[vsa])
    S.dma("sp", vwa[:, :, 0:64], A["vw"].rearrange("(kb p) d -> p kb d", p=128), w=[vwa])
    madd = S.sb([128, 512], F32, "madd")
    sm = [S.sb([128, 512], F32, "sm") for _ in range(2)]
    P = [S.sb([128, 512], F32, "P") for _ in range(2)]
    Pb = [S.sb([128, 512], BF16, "Pb") for _ in range(2)]
    PT = [S.sb([128, 512], BF16, "PT") for _ in range(2)]
    lc = S.sb([128, 4], F32, "lc"); rlc = S.sb([128, 4], F32, "rlc")
    Ps4 = S.sb([128, 512], F32, "Ps4")
    imp = S.sb([128, 128], F32, "imp")
    v_ = S.sb([128, 128], F32, "v_"); f_ = S.sb([128, 128], F32, "f_"); vf_ = S.sb([128, 128], F32, "vf_"); ad_ = S.sb([128, 128], F32, "ad_")
    score = S.sb([128, 128], F32, "score"); sc2 = S.sb([128, 128], F32, "sc2"); m8 = S.sb([128, 16], F32, "m8")
    selb = S.sb([128, 128], BF16, "selb")
    selT = [S.sb([128, 512], BF16, "selT") for _ in range(2)]
    ocs = [S.sb([128, 4, 2, 65], F32, "ocs") for _ in range(2)]
    pT = [S.sb([128, 512], BF16, "pT") for _ in range(3)]
    oss = S.sb([128, 4, 65], F32, "oss")
    glb = S.sb([128, 4, 6], BF16, "glb"); gg = S.sb([128, 4, 6], F32, "gg")
    ww = S.sb([128, 4, 3], F32, "ww")
    oacc = S.sb([128, 4, 64], F32, "oacc"); ob = S.sb([128, 4, 64], BF16, "ob")
    ost = [S.sb([64, 512], BF16, "ost") for _ in range(2)]
    itc = [0]
    def _qt(qt):
        qsl = slice(qt * 512, (qt + 1) * 512)
        oc_t = ocs[qt % 2]; sT = selT[qt % 2]
        def _sub(sub):
            qs = qt * 4 + sub
            q1 = slice(qs * 128, (qs + 1) * 128)
            S.op("dve", lambda e, qs=qs: e.tensor_scalar(out=madd[:], in0=mbase[:], scalar1=float(128 * qs), scalar2=NEG, op0=ALU.is_gt, op1=ALU.mult), r=[mbase], w=[madd])
            for i in range(4):
                itc[0] += 1; it = itc[0]
                ps = ps_s[it % 2]; s_ = sm[it % 2]; p_ = P[it % 2]
                S.op("pe", lambda e, ps=ps, i=i, q1=q1: e.matmul(ps[:], lhsT=qn[i][:, q1], rhs=kcmpT[:], start=True, stop=True), r=[qn[i], kcmpT], w=[ps])
                S.op("dve", lambda e, ps=ps, s_=s_: e.scalar_tensor_tensor(out=s_[:], in0=ps[:], scalar=0.125, in1=madd[:], op0=ALU.mult, op1=ALU.add), r=[ps, madd], w=[s_])
                S.op("act", lambda e, s_=s_, p_=p_, i=i: e.activation(out=p_[:], in_=s_[:], func=AF.Exp, accum_out=lc[:, i:i + 1]), r=[s_], w=[p_, lc])
                S.op("dve", lambda e, i=i: e.tensor_single_scalar(out=rlc[:, i:i + 1], in_=lc[:, i:i + 1], scalar=1e-30, op=ALU.max), r=[lc], w=[rlc])
                S.op("dve", lambda e, i=i: e.reciprocal(out=rlc[:, i:i + 1], in_=rlc[:, i:i + 1]), r=[rlc], w=[rlc])
                if i == 0:
                    S.op("dve", lambda e, p_=p_, i=i: e.tensor_scalar(out=Ps4[:], in0=p_[:], scalar1=rlc[:, i:i + 1], scalar2=None, op0=ALU.mult, op1=ALU.bypass), r=[p_, rlc], w=[Ps4])
                else:
                    S.op("dve", lambda e, p_=p_, i=i: e.scalar_tensor_tensor(out=Ps4[:], in0=p_[:], scalar=rlc[:, i:i + 1], in1=Ps4[:], op0=ALU.mult, op1=ALU.add), r=[p_, rlc, Ps4], w=[Ps4])
                if i < 2:
                    pb = Pb[i]; pt = PT[i]
                    S.op("pool", lambda e, p_=p_, pb=pb: e.tensor_copy(out=pb[:], in_=p_[:]), r=[p_], w=[pb])
                    for cb in range(NCB):
                        S.op("pe", lambda e, pb=pb, cb=cb: e.transpose(out=ps_t[:, cb * 128:(cb + 1) * 128], in_=pb[:, cb * 128:(cb + 1) * 128], identity=ident[:]), r=[pb, ident], w=[ps_t])
                    S.op("act", lambda e, pt=pt: e.activation(out=pt[:, 0:NCB * 128], in_=ps_t[:, 0:NCB * 128], func=AF.Copy), r=[ps_t], w=[pt])
                    for cb in range(NCB):
                        S.op("pe", lambda e, pt=pt, cb=cb, i=i: e.matmul(ps_oc[:, i, :], lhsT=pt[:, cb * 128:(cb + 1) * 128], rhs=vcmp[:, cb, :], start=(cb == 0), stop=(cb == NCB - 1)), r=[pt, vcmp], w=[ps_oc])
                    S.op("act", lambda e, i=i, sub=sub, oc_t=oc_t: e.activation(out=oc_t[:, sub, i, :], in_=ps_oc[:, i, :], func=AF.Copy), r=[ps_oc], w=[oc_t])
            S.op("dve", lambda e: e.tensor_reduce(out=imp[:], in_=Ps4[:].rearrange("p (n f) -> p n f", f=4), axis=AX.X, op=ALU.add), r=[Ps4], w=[imp])
            S.op("dve", lambda e: e.tensor_add(out=imp[:, 1:128], in0=imp[:, 1:128], in1=Ps4[:, 3:508:4]), r=[imp, Ps4], w=[imp])
            S.op("dve", lambda e, qs=qs: e.tensor_single_scalar(out=v_[:], in_=dmat[:], scalar=float(2 * qs), op=ALU.is_le), r=[dmat], w=[v_])
            S.op("dve", lambda e, qs=qs: e.scalar_tensor_tensor(out=f_[:], in0=dmat[:], scalar=float(2 * qs - 1), in1=v_[:], op0=ALU.is_ge, op1=ALU.mult), r=[dmat, v_], w=[f_])
            S.op("dve", lambda e: e.tensor_max(out=f_[:], in0=f_[:], in1=col0[:]), r=[f_, col0], w=[f_])
            S.op("dve", lambda e: e.tensor_sub(out=vf_[:], in0=v_[:], in1=f_[:]), r=[v_, f_], w=[vf_])
            S.op("dve", lambda e: e.scalar_tensor_tensor(out=ad_[:], in0=f_[:], scalar=-1.0, in1=v_[:], op0=ALU.add, op1=ALU.add), r=[f_, v_], w=[ad_])
            S.op("dve", lambda e: e.tensor_mul(out=score[:], in0=imp[:], in1=vf_[:]), r=[imp, vf_], w=[score])
            S.op("dve", lambda e: e.scalar_tensor_tensor(out=score[:], in0=ad_[:], scalar=1e9, in1=score[:], op0=ALU.mult, op1=ALU.add), r=[ad_, score], w=[score])
            S.op("dve", lambda e: e.max(out=m8[:, 0:8], in_=score[:]), r=[score], w=[m8])
            S.op("dve", lambda e: e.match_replace(out=sc2[:], in_to_replace=m8[:, 0:8], in_values=score[:], imm_value=-3e9), r=[score, m8], w=[sc2])
            S.op("dve", lambda e: e.max(out=m8[:, 8:16], in_=sc2[:]), r=[sc2, m8], w=[m8])
            S.op("dve", lambda e: e.tensor_scalar(out=sc2[:], in0=score[:], scalar1=m8[:, 15:16], scalar2=None, op0=ALU.is_ge, op1=ALU.bypass), r=[score, m8], w=[sc2])
            S.op("dve", lambda e: e.tensor_scalar(out=selb[:], in0=sc2[:], scalar1=-1.0, scalar2=-NEG, op0=ALU.add, op1=ALU.mult), r=[sc2], w=[selb])
            if "dbg_kcmp" in A and qs == A["dbg_qs"]:
                S.dma("sp", A["dbg_imp"], imp[:], r=[imp], is_output=True)
                S.dma("sp", A["dbg_score"], score[:], r=[score], is_output=True)
                S.dma("sp", A["dbg_sel"], sc2[:], r=[sc2], is_output=True)
                S.dma("sp", A["dbg_m8"], m8[:], r=[m8], is_output=True)
                S.dma("sp", A["dbg_ps4"], Ps4[:], r=[Ps4], is_output=True)
            S.op("pe", lambda e: e.transpose(out=ps_t[:, 0:128], in_=selb[:], identity=ident[:]), r=[selb, ident], w=[ps_t])
            S.op("act", lambda e, sub=sub, sT=sT: e.activation(out=sT[:, sub * 128:(sub + 1) * 128], in_=ps_t[:, 0:128], func=AF.Copy), r=[ps_t], w=[sT])
        for _s in range(4):
            _sub(_s)
        S.dma("sp", glb[:], A["gl"][qsl, :].rearrange("(s p) c -> p s c", p=128), w=[glb], allow_slow_non_contiguous=True)
        S.op("act", lambda e: e.activation(out=gg[:], in_=glb[:], func=AF.Exp, scale=-1.0), r=[glb], w=[gg])
        S.op("dve", lambda e: e.tensor_single_scalar(out=gg[:], in_=gg[:], scalar=1.0, op=ALU.add), r=[gg], w=[gg])
        S.op("dve", lambda e: e.reciprocal(out=gg[:], in_=gg[:]), r=[gg], w=[gg])
        def _head(i):
            fo = [True]
            def s1_sel(kb):
                d = kb - 4 * qt
                itc[0] += 1
                ps = ps_s[itc[0] % 2]; p_t = pT[itc[0] % 3]
                S.op("pe", lambda e: e.matmul(ps[:], lhsT=ksT[:, kb * 128:(kb + 1) * 128], rhs=qn[i][:, qsl], start=True, stop=False), r=[ksT, qn[i]], w=[ps])
                S.op("pe", lambda e: e.matmul(ps[:], lhsT=eall[:, kb * 128:(kb + 1) * 128], rhs=sT[:], start=False, stop=(d < 0)), r=[eall, sT], w=[ps])
                if d >= 0:
                    S.op("pe", lambda e: e.matmul(ps[:], lhsT=ident[:], rhs=mle[:, d, :], start=False, stop=True), r=[ident, mle], w=[ps])
                S.op("act", lambda e: e.activation(out=p_t[:], in_=ps[:], func=AF.Exp, scale=0.125), r=[ps], w=[p_t])
                return p_t
            def s2_sel(kb, p_t):
                d = kb - 4 * qt
                for sub in range(max(d, 0), 4):
                    st = fo[0]; fo[0] = False
                    S.op("pe", lambda e, sub=sub, st=st: e.matmul(ps_os[:, sub, :], lhsT=p_t[:, sub * 128:(sub + 1) * 128], rhs=vsa[:, kb, :], start=st, stop=(kb == 4 * qt + sub), skip_group_check=True), r=[p_t, vsa], w=[ps_os])
            kbs = list(range(4 * qt + 4))
            nxt = s1_sel(kbs[0])
            for n_, kb in enumerate(kbs):
                cur = nxt
                if n_ + 1 < len(kbs):
                    nxt = s1_sel(kbs[n_ + 1])
                s2_sel(kb, cur)
            S.op("act", lambda e: e.activation(out=oss[:], in_=ps_os[:], func=AF.Copy), r=[ps_os], w=[oss])
            fw_ = [True]
            def s1_win(kb):
                d = kb - 4 * qt
                itc[0] += 1
                ps = ps_s[itc[0] % 2]; p_t = pT[itc[0] % 3]
                S.op("pe", lambda e: e.matmul(ps[:], lhsT=kwT[:, kb * 128:(kb + 1) * 128], rhs=qn[i][:, qsl], start=True, stop=False), r=[kwT, qn[i]], w=[ps])
                mk = mle[:, d, :] if d >= 0 else mwin[:, d + 4, :]
                S.op("pe", lambda e: e.matmul(ps[:], lhsT=ident[:], rhs=mk, start=False, stop=True), r=[ident, mle, mwin], w=[ps])
                S.op("act", lambda e: e.activation(out=p_t[:], in_=ps[:], func=AF.Exp, scale=0.125), r=[ps], w=[p_t])
                return p_t
            def s2_win(kb, p_t):
                d = kb - 4 * qt
                subs = range(d, 4) if d >= 0 else range(0, d + 5)
                for sub in subs:
                    st = fw_[0]; fw_[0] = False
                    S.op("pe", lambda e, sub=sub, st=st: e.matmul(ps_ow[:, sub, :], lhsT=p_t[:, sub * 128:(sub + 1) * 128], rhs=vwa[:, kb, :], start=st, stop=(kb == 4 * qt + sub), skip_group_check=True), r=[p_t, vwa], w=[ps_ow])
            kbs = list(range(max(0, 4 * qt - 4), 4 * qt + 4))
            nxt = s1_win(kbs[0])
            for n_, kb in enumerate(kbs):
                cur = nxt
                if n_ + 1 < len(kbs):
                    nxt = s1_win(kbs[n_ + 1])
                s2_win(kb, cur)
            S.op("dve", lambda e, i=i: e.tensor_single_scalar(out=ww[:, :, 0], in_=oc_t[:, :, i, 64], scalar=1e-30, op=ALU.max), r=[oc_t], w=[ww])
            S.op("dve", lambda e: e.tensor_copy(out=ww[:, :, 1], in_=oss[:, :, 64]), r=[oss, ww], w=[ww])
            S.op("dve", lambda e: e.tensor_copy(out=ww[:, :, 2], in_=ps_ow[:, :, 64]), r=[ps_ow, ww], w=[ww])
            S.op("dve", lambda e: e.reciprocal(out=ww[:], in_=ww[:]), r=[ww], w=[ww])
            S.op("dve", lambda e, i=i: e.tensor_mul(out=ww[:], in0=ww[:], in1=gg[:, :, i * 3:(i + 1) * 3]), r=[ww, gg], w=[ww])
            if "dbg_kcmp" in A and qt == A["dbg_qs"] // 4 and i == 0:
                owf = S.sb([128, 4, 65], F32, "owf")
                S.op("dve", lambda e: e.tensor_copy(out=owf[:], in_=ps_ow[:]), r=[ps_ow], w=[owf])
                S.dma("sp", A["dbg_ow"], owf[:], r=[owf], is_output=True)
                S.dma("sp", A["dbg_os"], oss[:], r=[oss], is_output=True)
                S.dma("sp", A["dbg_oc"], oc_t[:], r=[oc_t], is_output=True)
                S.dma("sp", A["dbg_ww"], ww[:], r=[ww], is_output=True)
            for sub in range(4):
                S.op("dve", lambda e, sub=sub, i=i: e.tensor_scalar(out=oacc[:, sub, :], in0=oc_t[:, sub, i, 0:64], scalar1=ww[:, sub, 0:1], scalar2=None, op0=ALU.mult, op1=ALU.bypass), r=[oc_t, ww], w=[oacc])
                S.op("dve", lambda e, sub=sub: e.scalar_tensor_tensor(out=oacc[:, sub, :], in0=oss[:, sub, 0:64], scalar=ww[:, sub, 1:2], in1=oacc[:, sub, :], op0=ALU.mult, op1=ALU.add), r=[oss, ww, oacc], w=[oacc])
                S.op("dve", lambda e, sub=sub: e.scalar_tensor_tensor(out=ob[:, sub, :], in0=ps_ow[:, sub, 0:64], scalar=ww[:, sub, 2:3], in1=oacc[:, sub, :], op0=ALU.mult, op1=ALU.add), r=[ps_ow, ww, oacc], w=[ob])
            for sub in range(4):
                S.op("pe", lambda e, sub=sub: e.transpose(out=ps_t[0:64, sub * 128:(sub + 1) * 128], in_=ob[:, sub, :], identity=ident[:]), r=[ob, ident], w=[ps_t])
            o_s = ost[(qt * 2 + i) % 2]
            S.op("act", lambda e, o_s=o_s: e.activation(out=o_s[:], in_=ps_t[0:64, :], func=AF.Copy), r=[ps_t], w=[o_s])
            S.dma("sp", A["oT"][i * 64:(i + 1) * 64, qsl], o_s[:], r=[o_s], is_output=True)
        for _i in range(2):
            _head(_i)
    for _q in range(NQT):
        _qt(_q)


NSA_IN = dict(qnT=lambda T: ([4, 64, T], BF16), kcT=lambda T: ([64, T], BF16), vcT=lambda T: ([64, T], BF16),
              ksT=lambda T: ([64, T], BF16), kwT=lambda T: ([64, T], BF16), vs=lambda T: ([T, 64], BF16), vw=lambda T: ([T, 64], BF16),
              gl=lambda T: ([T, 6], BF16), posc=lambda T: ([1, 512], I32),
              w1k=lambda T: ([2048, 256], F32), w1v=lambda T: ([2048, 256], F32), w2k=lambda T: ([256, 64], F32), w2v=lambda T: ([256, 64], F32),
              posk=lambda T: ([32, 64], F32), posv=lambda T: ([32, 64], F32))


def load_csts_extra(S, nc, arrs):
    out = {}
    for n, arr in arrs.items():
        dt = BF16 if arr.dtype == NPBF else F32
        ap = din(nc, "c_" + n, arr.shape, dt)
        t = S.sb(list(arr.shape), dt, n)
        S.dma("sp", t[:], ap, w=[t])
        out[n] = t
    return out, {"c_" + n: a for n, a in arrs.items()}


def build_nsa(T, dbg_qs=None):
    nc = new_nc()
    A = {}
    if dbg_qs is not None:
        A["dbg_qs"] = dbg_qs
        for n, shp in dict(dbg_kcmp=[64, 512], dbg_vcmp=[128, 4, 65], dbg_imp=[128, 128], dbg_score=[128, 128], dbg_sel=[128, 128], dbg_m8=[128, 16], dbg_ps4=[128, 512],
                           dbg_hid=[2, 128, 2, 512], dbg_b1=[2, 128, 2], dbg_u1=[64, 512], dbg_u2=[64, 512], dbg_cc=[64, 512], dbg_sc=[64, 512], dbg_ow=[128, 4, 65], dbg_os=[128, 4, 65], dbg_oc=[128, 4, 2, 65], dbg_ww=[128, 4, 3]).items():
            A[n] = dout(nc, n, shp, F32)
    for n, f in NSA_IN.items():
        shp, dt = f(T)
        A[n] = din(nc, n, shp, dt)
    A["oT"] = dout(nc, "oT", [128, T], BF16)
    with ExitStack() as st:
        S = Sched(nc, st)
        cst, cmap = load_csts(S, nc, ["ident", "pswap", "ropec", "mle", "mwin"])
        c2, cmap2 = load_csts_extra(S, nc, nsa_consts(T))
        cst.update(c2); cmap.update(cmap2)
        emit_nsa(S, T, A, cst)
        S.finish(); S.emit()
    return nc, cmap


def build_mod():
    nc = new_nc()
    cT = din(nc, "cT", [128, 8, 2], F32); w = din(nc, "w", [2, D, 768], F32); bias = din(nc, "bias", [2, 768], F32)
    out = dout(nc, "modp", [2, 2, 768], F32)
    with ExitStack() as st:
        S = Sched(nc, st)
        ct = S.sb([128, 8, 2], F32, "ct"); cond = S.sb([128, 8, 2], F32, "cond")
        S.dma("sp", ct[:], cT, w=[ct])
        S.op("act", lambda e: e.activation(out=cond[:], in_=ct[:], func=AF.Silu), r=[ct], w=[cond])
        ps = [S.ps([128, 512], F32, "ps") for _ in range(2)]
        for l in range(2):
            W = S.sb([128, 8, 768], F32, "W")
            S.dma("sp", W[:], w[l].rearrange("(k p) c -> p k c", p=128), w=[W])
            bt = S.sb([2, 768], F32, "bt")
            S.dma("sp", bt[:], bias[l:l + 1, :].partition_broadcast(2), w=[bt])
            ot = S.sb([2, 768], F32, "ot")
            for half in range(2):
                p = ps[half]
                for k in range(8):
                    S.op("pe", lambda e, p=p, k=k, half=half, W=W: e.matmul(p[0:2, 0:384], lhsT=cond[:, k, :], rhs=W[:, k, half * 384:(half + 1) * 384], start=(k == 0), stop=(k == 7)), r=[cond, W], w=[p])
                S.op("dve", lambda e, p=p, half=half, ot=ot, bt=bt: e.tensor_add(out=ot[:, half * 384:(half + 1) * 384], in0=p[0:2, 0:384], in1=bt[:, half * 384:(half + 1) * 384]), r=[p, bt], w=[ot])
            S.dma("sp", out[l], ot[:], r=[ot], is_output=True)
        S.finish(); S.emit()
    return nc, {}


def _run(nc, in_maps):
    res = run_bass_kernel_spmd(nc, in_maps, core_ids=list(range(8)))
    return res.results


def kernel(x, c, positions, mod_w, mod_b, norm_mix, norm_ffn, ffn_w_gate, ffn_w_up, ffn_conv_w, ffn_conv_b, ffn_w_down,
           hyb_w_in, nsa_pos_k, nsa_pos_v, nsa_ck_w1, nsa_ck_w2, nsa_cv_w1, nsa_cv_w2, hyb_w_out, diff_w_qkv,
           diff_lq1, diff_lk1, diff_lq2, diff_lk2, diff_subln, diff_w_out, norm_f):
    f32 = lambda a: np.ascontiguousarray(np.asarray(a), dtype=np.float32)
    x = f32(x); c = f32(c); positions = np.ascontiguousarray(np.asarray(positions), dtype=np.int32)
    B, T, _ = x.shape
    TL = T // 4
    ca = np.ascontiguousarray
    nc, cm = build_mod()
    cT = ca(c.T.reshape(8, 128, 2).transpose(1, 0, 2))
    mw = f32(mod_w); mb = f32(mod_b)
    r = _run(nc, [dict(cT=cT, w=ca(mw[:, :, i * 768:(i + 1) * 768]), bias=ca(mb[:, i * 768:(i + 1) * 768])) for i in range(8)])
    mod = np.concatenate([r[i]["modp"] for i in range(8)], axis=-1)
    sh_m, sc_m, g_m, sh_f, sc_f, g_f = [mod[..., k * D:(k + 1) * D] for k in range(6)]
    nmix = f32(norm_mix); nffn = f32(norm_ffn)

    def run_pre(layer, xin, w):
        nc, cm = build_pre(TL, layer)
        maps = []
        for i in range(8):
            b, j = i // 4, i % 4
            maps.append(dict(x=ca(xin[b, j * TL:(j + 1) * TL]), pos=ca(positions[b:b + 1, j * TL:(j + 1) * TL]),
                             mv=ca(np.stack([nmix[layer], sc_m[layer, b], sh_m[layer, b]])), w=w, **cm))
        r = _run(nc, maps)
        FM = [np.concatenate([r[b * 4 + j]["fm"] for j in range(4)], axis=2) for b in range(B)]
        TM = [np.concatenate([r[b * 4 + j]["tm"] for j in range(4)], axis=0) for b in range(B)]
        return FM, TM

    def run_post(layer, OT, xin, wo, final):
        nc, cm = build_post(TL, final)
        maps = []
        for i in range(8):
            b, j = i // 4, i % 4
            oT = np.zeros((D, TL + 2), NPBF); xe = np.zeros((TL + 2, D), np.float32)
            lo = j * TL - 2
            if j > 0:
                oT[:] = OT[b][:, lo:lo + TL + 2]; xe[:] = xin[b, lo:lo + TL + 2]
            else:
                oT[:, 2:] = OT[b][:, 0:TL]; xe[2:] = xin[b, 0:TL]
            m = dict(oT=oT, x=xe, wo=wo, mvec=ca(np.stack([g_m[layer, b], g_f[layer, b]])),
                     mvf=ca(np.stack([nffn[layer], sc_f[layer, b], sh_f[layer, b]])),
                     wg=f32(ffn_w_gate[layer]), wu=f32(ffn_w_up[layer]), wd=f32(ffn_w_down[layer]),
                     cw=f32(ffn_conv_w[layer]), cb=f32(ffn_conv_b[layer])[None, :], flag=np.array([[1.0 if j > 0 else 0.0]], np.float32), **cm)
            if final:
                m["fg"] = f32(norm_f)[None, :]
            maps.append(m)
        r = _run(nc, maps)
        return np.stack([np.concatenate([r[b * 4 + j]["xo"] for j in range(4)], axis=0) for b in range(B)])

    FM, TM = run_pre(0, x, f32(hyb_w_in[0]))
    nc, cm = build_nsa(T)
    maps = []
    NCc = (T - 32) // 16 + 1
    for i in range(8):
        b, hg = i // 4, i % 4
        g = hg // 2
        own = [2 * hg, 2 * hg + 1]
        order = own + [h for h in range(4 * g, 4 * g + 4) if h not in own]
        posc = np.zeros((1, 512), np.int32); posc[0, :NCc] = positions[b, 31::16][:NCc]
        gs = slice(64 * g, 64 * g + 64)
        maps.append(dict(qnT=ca(np.stack([FM[b][h // 2, (h % 2) * 64:(h % 2) * 64 + 64] for h in order])),
                         kcT=ca(FM[b][4, gs]), vcT=ca(FM[b][5, gs]), ksT=ca(FM[b][6, gs]), kwT=ca(FM[b][7, gs]),
                         vs=ca(TM[b][:, 64 * g:64 * g + 64]), vw=ca(TM[b][:, 128 + 64 * g:128 + 64 * g + 64]),
                         gl=ca(TM[b][:, 256 + 3 * own[0]:256 + 3 * own[0] + 6]), posc=posc,
                         w1k=f32(nsa_ck_w1[0]), w1v=f32(nsa_cv_w1[0]), w2k=f32(nsa_ck_w2[0]), w2v=f32(nsa_cv_w2[0]),
                         posk=f32(nsa_pos_k[0]), posv=f32(nsa_pos_v[0]), **cm))
    r_nsa = _run(nc, maps)
    nc, cm = build_sb(T)
    maps = []
    for i in range(8):
        b, hg = i // 4, i % 4
        maps.append(dict(qT=ca(FM[b][8 + hg].reshape(2, 64, T)), kT=ca(FM[b][12 + hg].reshape(2, 64, T)),
                         v=ca(TM[b][:, 280 + 128 * hg:280 + 128 * hg + 128]), **cm))
    r_sb = _run(nc, maps)
    OT = []
    for b in range(B):
        OT.append(np.concatenate([r_nsa[b * 4 + hg]["oT"] for hg in range(4)] + [r_sb[b * 4 + hg]["oT"] for hg in range(4)], axis=0))
    x1 = run_post(0, OT, x, f32(hyb_w_out[0]), False)
    FM, TM = run_pre(1, x1, f32(diff_w_qkv[0]))
    nc, cm = build_diff(T)
    lam = ca(np.stack([f32(diff_lq1[0]), f32(diff_lk1[0]), f32(diff_lq2[0]), f32(diff_lk2[0])]))
    maps = []
    for i in range(8):
        b, hg = i // 4, i % 4
        maps.append(dict(qT=ca(FM[b][2 * hg:2 * hg + 2].reshape(4, 64, T)), kT=ca(FM[b][8 + 2 * hg:8 + 2 * hg + 2].reshape(4, 64, T)),
                         v=ca(TM[b][:, 256 * hg:256 * hg + 256]), lam=lam, subln=f32(diff_subln[0])[None, :], **cm))
    r_d = _run(nc, maps)
    OT = [np.concatenate([r_d[b * 4 + hg]["oT"] for hg in range(4)], axis=0) for b in range(B)]
    out = run_post(1, OT, x1, f32(diff_w_out[0]), True)
    return out.astype(np.float32)
```

```python
import math
from contextlib import ExitStack
import numpy as np
import ml_dtypes
import concourse.bass as bass
import concourse.mybir as mybir
from concourse.bass_utils import run_bass_kernel_spmd

F32 = mybir.dt.float32
BF16 = mybir.dt.bfloat16
I32 = mybir.dt.int32
AF = mybir.ActivationFunctionType
ALU = mybir.AluOpType
AX = mybir.AxisListType
NPBF = ml_dtypes.bfloat16

SAME_ENGINE_SYNC = True
N_DMA_SEMS = 16


class Res:
    __slots__ = ("name", "w", "rs")

    def __init__(self, name):
        self.name = name
        self.w = None
        self.rs = []


class Tile:
    def __init__(self, h, name):
        self.h = h
        self.r = Res(name)
        self._subs = {}
        self.name = name

    def __getitem__(self, k):
        return self.h[k]

    def sub(self, key):
        s = self._subs.get(key)
        if s is None:
            s = Res(f"{self.name}/{key}")
            self._subs[key] = s
        return s


class Sched:
    ENG = ("pe", "act", "dve", "pool", "sp")

    def __init__(self, nc, stack):
        self.nc = nc
        self.stack = stack
        self.ops = {e: [] for e in self.ENG}
        self.cnt = {e: 0 for e in self.ENG}
        self.known = {e: {} for e in self.ENG}
        self.dma_cnt = [0] * N_DMA_SEMS
        self.dma_rr = 0
        self.sems = {}
        for e in ("pe", "act", "dve", "pool"):
            self.sems[e] = stack.enter_context(nc.semaphore("s_" + e))
        for i in range(N_DMA_SEMS):
            self.sems[("d", i)] = stack.enter_context(nc.semaphore(f"s_d{i}"))
        self.out_events = []
        self.n_names = 0

    def sb(self, shape, dtype, name=None):
        self.n_names += 1
        name = f"{name or 't'}_{self.n_names}"
        h = self.stack.enter_context(self.nc.sbuf_tensor(name, list(shape), dtype))
        return Tile(h, name)

    def ps(self, shape, dtype, name=None):
        self.n_names += 1
        name = f"{name or 'p'}_{self.n_names}"
        h = self.stack.enter_context(self.nc.psum_tensor(name, list(shape), dtype))
        return Tile(h, name)

    def _deps(self, eng, r, w):
        deps = {}
        def add(ev):
            if ev is None:
                return
            k, v = ev
            if deps.get(k, 0) < v:
                deps[k] = v
        for x in r:
            add(x.w)
        for x in w:
            add(x.w)
            for ev in x.rs:
                add(ev)
        waits = []
        kn = self.known[eng]
        for k, v in deps.items():
            if k == eng and (eng == "pe" or not SAME_ENGINE_SYNC):
                continue
            if kn.get(k, 0) >= v:
                continue
            kn[k] = v
            waits.append((k, v))
        return waits

    def _commit(self, ev, r, w):
        for x in r:
            x.rs.append(ev)
        for x in w:
            x.w = ev
            x.rs = []

    @staticmethod
    def _res(lst):
        out = []
        for x in lst:
            out.append(x.r if isinstance(x, Tile) else x)
        return out

    def op(self, eng, fn, r=(), w=()):
        r = self._res(r)
        w = self._res(w)
        waits = self._deps(eng, r, w)
        self.cnt[eng] += 1
        ev = (eng, self.cnt[eng])
        self.ops[eng].append((waits, fn, (eng, 1)))
        self._commit(ev, r, w)
        return ev

    def dma(self, eng, out, in_, r=(), w=(), is_output=False, **kw):
        r = self._res(r)
        w = self._res(w)
        waits = self._deps(eng, r, w)
        si = self.dma_rr
        self.dma_rr = (self.dma_rr + 1) % N_DMA_SEMS
        key = ("d", si)
        prev = 16 * self.dma_cnt[si]
        kn = self.known[eng]
        if prev > 0 and kn.get(key, 0) < prev:
            kn[key] = prev
            waits.append((key, prev))
        self.dma_cnt[si] += 1
        ev = (key, 16 * self.dma_cnt[si])
        def fn(e, out=out, in_=in_, kw=kw):
            return e.dma_start(out=out, in_=in_, **kw)
        self.ops[eng].append((waits, fn, (key, 16)))
        self._commit(ev, r, w)
        if is_output:
            self.out_events.append(ev)
        return ev

    def dma_fn(self, eng, fn, r=(), w=(), is_output=False):
        r = self._res(r); w = self._res(w)
        waits = self._deps(eng, r, w)
        si = self.dma_rr
        self.dma_rr = (self.dma_rr + 1) % N_DMA_SEMS
        key = ("d", si)
        prev = 16 * self.dma_cnt[si]
        kn = self.known[eng]
        if prev > 0 and kn.get(key, 0) < prev:
            kn[key] = prev
            waits.append((key, prev))
        self.dma_cnt[si] += 1
        ev = (key, 16 * self.dma_cnt[si])
        self.ops[eng].append((waits, fn, (key, 16)))
        self._commit(ev, r, w)
        if is_output:
            self.out_events.append(ev)
        return ev

    def cc(self, kind, ins, outs, groups, r=(), w=()):
        def fn(e):
            return e.collective_compute(kind, ALU.bypass, replica_groups=groups, ins=list(ins), outs=list(outs))
        return self.dma_fn("pool", fn, r=r, w=w)

    def barrier(self):
        evs = [(e, self.cnt[e]) for e in ("pe", "act", "dve", "pool") if self.cnt[e] > 0]
        evs += [(("d", i), 16 * self.dma_cnt[i]) for i in range(N_DMA_SEMS) if self.dma_cnt[i] > 0]
        for eng in self.ENG:
            kn = self.known[eng]
            waits = []
            for (k, v) in evs:
                if kn.get(k, 0) >= v:
                    continue
                kn[k] = v
                waits.append((k, v))
            if waits:
                self.ops[eng].append((waits, None, None))

    def finish(self):
        final = {}
        for (k, v) in self.out_events:
            if final.get(k, 0) < v:
                final[k] = v
        waits = [(k, v) for k, v in final.items()]
        self.ops["sp"].append((waits, None, None))

    def emit(self):
        nc = self.nc
        sems = self.sems
        ops = self.ops
        def run(engname, e):
            for (waits, fn, inc) in ops[engname]:
                for (k, v) in waits:
                    e.wait_ge(sems[k], v)
                if fn is not None:
                    ins = fn(e)
                    ins.then_inc(sems[inc[0]], inc[1])
        with nc.Block() as block:
            @block.tensor
            def _(e):
                run("pe", e)
            @block.scalar
            def _(e):
                run("act", e)
            @block.vector
            def _(e):
                run("dve", e)
            @block.gpsimd
            def _(e):
                run("pool", e)
            @block.sync
            def _(e):
                run("sp", e)


D = 1024
DFF = 2816
NFC = DFF // 128
EPS = 1e-6
NEG = -30000.0
ROPE_THETA = 500000.0
LAMBDA_INIT = 0.8 - 0.6 * math.exp(-0.3 * 1)
C1 = 6.28125
C2 = 2 * math.pi - 6.28125


def new_nc():
    return bass.Bass("TRN2", target_bir_lowering=False)


def din(nc, name, shape, dt):
    return nc.dram_tensor(name, list(shape), dt, kind="ExternalInput").ap()


def dout(nc, name, shape, dt):
    return nc.dram_tensor(name, list(shape), dt, kind="ExternalOutput").ap()


def host_consts():
    c = {}
    c["ident"] = np.eye(128, dtype=np.float32).astype(NPBF)
    p = np.arange(128)
    sw = np.zeros((128, 128), np.float32)
    for m in range(128):
        r = m % 64
        if r < 8:
            sw[m + 8, m] = 1
        elif r < 16:
            sw[m - 8, m] = 1
    c["pswap"] = sw.astype(NPBF)
    rc = np.zeros((128, 2), np.float32)
    for m in range(128):
        r = m % 64
        if r < 16:
            rc[m, 0] = ROPE_THETA ** (-(2 * (r % 8)) / 16.0)
            rc[m, 1] = -1.0 if r < 8 else 1.0
    c["ropec"] = rc
    n = np.arange(512)[None, :]
    pp = p[:, None]
    mle = np.stack([np.where(n >= 128 * d + pp, 0.0, NEG) for d in range(4)])
    mlt = np.stack([np.where(n > 128 * d + pp, 0.0, NEG) for d in range(4)])
    mwin = np.stack([np.where(n < 128 * d + pp, 0.0, NEG) for d in range(4)])
    c["mle"] = mle.astype(NPBF)
    c["mlt"] = mlt.astype(NPBF)
    c["mwin"] = mwin.astype(NPBF)
    tri = np.where(p[:, None] >= p[None, :], -1.0, 0.0)
    c["negtri"] = tri.astype(NPBF)
    c["negones"] = (-np.ones((128, 128), np.float32)).astype(NPBF)
    return c


def rope_tables(S, pos_ap, ntok, ropec, name):
    Ct = S.sb([128, ntok], F32, name + "C")
    St = S.sb([128, ntok], F32, name + "S")
    with ExitStack() as st:
        old = S.stack
        S.stack = st
        posi = S.sb([128, ntok], I32, "posi")
        ang = S.sb([128, ntok], F32, "ang")
        u = S.sb([128, ntok], F32, "u")
        ki = S.sb([128, ntok], I32, "ki")
        kf = S.sb([128, ntok], F32, "kf")
        S.dma("sp", posi[:], pos_ap.partition_broadcast(128), w=[posi])
        S.op("dve", lambda e: e.tensor_copy(out=ang[:], in_=posi[:]), r=[posi], w=[ang])
        S.op("dve", lambda e: e.tensor_scalar(out=ang[:], in0=ang[:], scalar1=ropec[:, 0:1], scalar2=None, op0=ALU.mult, op1=ALU.bypass), r=[ang, ropec], w=[ang])
        for (off, dst, sgn) in ((0.5 * math.pi, Ct, False), (0.0, St, True)):
            S.op("dve", lambda e, off=off: e.tensor_single_scalar(out=u[:], in_=ang[:], scalar=off, op=ALU.add), r=[ang], w=[u])
            S.op("dve", lambda e: e.tensor_single_scalar(out=ki[:], in_=u[:], scalar=1.0 / (2 * math.pi), op=ALU.mult), r=[u], w=[ki])
            S.op("dve", lambda e: e.tensor_copy(out=kf[:], in_=ki[:]), r=[ki], w=[kf])
            S.op("dve", lambda e: e.scalar_tensor_tensor(out=u[:], in0=kf[:], scalar=-C1, in1=u[:], op0=ALU.mult, op1=ALU.add), r=[kf, u], w=[u])
            S.op("dve", lambda e: e.scalar_tensor_tensor(out=u[:], in0=kf[:], scalar=-C2, in1=u[:], op0=ALU.mult, op1=ALU.add), r=[kf, u], w=[u])
            S.op("dve", lambda e: e.tensor_scalar(out=kf[:], in0=u[:], scalar1=math.pi, scalar2=2 * math.pi, op0=ALU.is_gt, op1=ALU.mult), r=[u], w=[kf])
            S.op("dve", lambda e: e.tensor_sub(out=u[:], in0=u[:], in1=kf[:]), r=[u, kf], w=[u])
            S.op("dve", lambda e: e.tensor_scalar(out=u[:], in0=u[:], scalar1=math.pi, scalar2=-math.pi, op0=ALU.min, op1=ALU.max), r=[u], w=[u])
            S.op("act", lambda e, dst=dst: e.activation(out=dst[:], in_=u[:], func=AF.Sin), r=[u], w=[dst])
            if sgn:
                S.op("dve", lambda e, dst=dst: e.tensor_scalar(out=dst[:], in0=dst[:], scalar1=ropec[:, 1:2], scalar2=None, op0=ALU.mult, op1=ALU.bypass), r=[dst, ropec], w=[dst])
        S.barrier()
        S.stack = old
    return Ct, St


def load_const(S, ap, shape, dt, name):
    t = S.sb(shape, dt, name)
    S.dma("sp", t[:], ap, w=[t])
    return t


def rstd_from_ssq(S, ssq, rstd, n, ntok=128, cols=None):
    sl = (slice(0, ntok), slice(None) if cols is None else cols)
    S.op("dve", lambda e: e.tensor_scalar(out=rstd[sl], in0=ssq[sl], scalar1=1.0 / n, scalar2=EPS, op0=ALU.mult, op1=ALU.add), r=[ssq], w=[rstd])
    S.op("act", lambda e: e.activation(out=rstd[sl], in_=rstd[sl], func=AF.Ln), r=[rstd], w=[rstd])
    S.op("act", lambda e: e.activation(out=rstd[sl], in_=rstd[sl], func=AF.Exp, scale=-0.5), r=[rstd], w=[rstd])


def load_weight_bf16(S, Wb, w_ap, nk, ncols, stage_cols=1024, col0=0, name="wst"):
    stg = [S.sb([128, stage_cols], F32, name) for _ in range(2)]
    i = 0
    for k in range(nk):
        for c0 in range(0, ncols, stage_cols):
            cw = min(stage_cols, ncols - c0)
            s = stg[i % 2]
            i += 1
            S.dma("sp", s[:, 0:cw], w_ap[k * 128:(k + 1) * 128, col0 + c0:col0 + c0 + cw], w=[s])
            S.op("pool", lambda e, s=s, k=k, c0=c0, cw=cw: e.tensor_copy(out=Wb[:, k, c0:c0 + cw], in_=s[:, 0:cw]), r=[s], w=[Wb])


def norm_to_hT(S, x_t, ntok, tok0, hT, a_t, sh_t, ident, scr):
    junk, ssq, rstd, xn, pT = scr
    S.op("act", lambda e: e.activation(out=junk[0:ntok, :], in_=x_t[0:ntok, :], func=AF.Square, accum_out=ssq[0:ntok, :]), r=[x_t], w=[junk, ssq])
    rstd_from_ssq(S, ssq, rstd, D, ntok)
    S.op("act", lambda e: e.activation(out=xn[0:ntok, :], in_=x_t[0:ntok, :], func=AF.Copy, scale=rstd[0:ntok, :]), r=[x_t, rstd], w=[xn])
    for k in range(8):
        S.op("pe", lambda e, k=k: e.transpose(out=pT[:, k * 128:k * 128 + ntok], in_=xn[0:ntok, k * 128:(k + 1) * 128], identity=ident[0:ntok, 0:ntok]), r=[xn, ident], w=[pT])
    for k in range(8):
        eng = "dve" if k % 2 == 0 else "pool"
        eng = "dve"
        S.op(eng, lambda e, k=k: e.tensor_scalar(out=hT[:, k, tok0:tok0 + ntok], in0=pT[:, k * 128:k * 128 + ntok], scalar1=a_t[:, k:k + 1], scalar2=sh_t[:, k:k + 1], op0=ALU.mult, op1=ALU.add), r=[pT, a_t, sh_t], w=[hT.sub(tok0)])


def norm_scratch(S):
    return [(S.sb([128, D], BF16, "junk"), S.sb([128, 1], F32, "ssq"), S.sb([128, 1], F32, "rstd"),
             S.sb([128, D], BF16, "xn"), S.ps([128, D], BF16, "pT")) for _ in range(2)]


def mod_vectors(S, mv_ap):
    mv = S.sb([128, 3, 8], F32, "mv")
    S.dma("sp", mv[:], mv_ap.rearrange("r (k p) -> p r k", p=128), w=[mv], allow_slow_non_contiguous=True)
    a = S.sb([128, 8], F32, "a")
    S.op("dve", lambda e: e.tensor_single_scalar(out=a[:], in_=mv[:, 1, :], scalar=1.0, op=ALU.add), r=[mv], w=[a])
    S.op("dve", lambda e: e.tensor_mul(out=a[:], in0=a[:], in1=mv[:, 0, :]), r=[a, mv], w=[a])
    sh = S.sb([128, 8], F32, "sh")
    S.op("dve", lambda e: e.tensor_copy(out=sh[:], in_=mv[:, 2, :]), r=[mv], w=[sh])
    return a, sh


def emit_pre(S, T_loc, colspec, NC, x_ap, pos_ap, mv_ap, w_ap, fm_ap, tm_ap, cst, x_res=None):
    NT = T_loc // 128
    TG = min(512, T_loc)
    NTG = T_loc // TG
    ident, pswap, ropec = cst["ident"], cst["pswap"], cst["ropec"]
    a, sh = mod_vectors(S, mv_ap)
    hT = S.sb([128, 8, T_loc], BF16, "hT")
    Wb = S.sb([128, 8, NC], BF16, "Wb")
    need_rope = any(c.get("rope") for c in colspec)
    if need_rope:
        Ct, St = rope_tables(S, pos_ap, T_loc, ropec, "rp")
    load_weight_bf16(S, Wb, w_ap, 8, NC)
    scr = norm_scratch(S)
    xt = [S.sb([128, D], F32, "xt") for _ in range(2)]
    for tt in range(NT):
        x_t = xt[tt % 2]
        S.dma("sp", x_t[:], x_ap[tt * 128:(tt + 1) * 128, :], r=[x_res] if x_res else [], w=[x_t])
        norm_to_hT(S, x_t, 128, tt * 128, hT, a, sh, ident, scr[tt % 2])
    hT_all = [hT.sub(tt * 128) for tt in range(NT)]
    psA = [S.ps([128, 512], F32, "psA") for _ in range(2)]
    psB = S.ps([128, 512], F32, "psB")
    xb = [S.sb([128, 512], BF16, "xb") for _ in range(2)]
    t1 = [S.sb([128, 512], F32, "t1") for _ in range(2)]
    t2 = [S.sb([128, 512], F32, "t2") for _ in range(2)]
    ob = [S.sb([128, 512], BF16, "ob") for _ in range(3)]
    it = 0
    fmi = 0
    tmoff = 0
    for c in colspec:
        if c["kind"] == "fm":
            c0 = c["col"]
            for tg in range(NTG):
                it += 1
                ps = psA[it % 2]
                tsl = slice(tg * TG, (tg + 1) * TG)
                for k in range(8):
                    S.op("pe", lambda e, ps=ps, k=k, c0=c0, tsl=tsl: e.matmul(ps[:, 0:TG], lhsT=Wb[:, k, c0:c0 + 128], rhs=hT[:, k, tsl], start=(k == 0), stop=(k == 7)),
                         r=[Wb] + hT_all[tg * (TG // 128):(tg + 1) * (TG // 128)], w=[ps])
                o = ob[it % 3]
                if not c.get("rope"):
                    S.op("act", lambda e, ps=ps, o=o, sc=c.get("scale", 1.0): e.activation(out=o[:, 0:TG], in_=ps[:, 0:TG], func=AF.Copy, scale=sc), r=[ps], w=[o])
                else:
                    b = xb[it % 2]; u1 = t1[it % 2]; u2 = t2[it % 2]
                    S.op("act", lambda e, ps=ps, b=b: e.activation(out=b[:, 0:TG], in_=ps[:, 0:TG], func=AF.Copy), r=[ps], w=[b])
                    S.op("pe", lambda e, b=b: e.matmul(psB[:, 0:TG], lhsT=pswap[:], rhs=b[:, 0:TG], start=True, stop=True), r=[b, pswap], w=[psB])
                    S.op("dve", lambda e, b=b, u1=u1, tsl=tsl: e.tensor_mul(out=u1[:, 0:TG], in0=b[:, 0:TG], in1=Ct[:, tsl]), r=[b, Ct], w=[u1])
                    S.op("dve", lambda e, u2=u2, tsl=tsl: e.tensor_mul(out=u2[:, 0:TG], in0=psB[:, 0:TG], in1=St[:, tsl]), r=[psB, St], w=[u2])
                    S.op("pool", lambda e, o=o, u1=u1, u2=u2: e.tensor_add(out=o[:, 0:TG], in0=u1[:, 0:TG], in1=u2[:, 0:TG]), r=[u1, u2], w=[o])
                S.dma("sp", fm_ap[fmi, :, tsl], o[:, 0:TG], r=[o], is_output=True)
            fmi += 1
        else:
            c0, n = c["col"], c["n"]
            for tt in range(NT):
                for cc in range(0, n, 512):
                    cw = min(512, n - cc)
                    it += 1
                    ps = psA[it % 2]
                    for k in range(8):
                        S.op("pe", lambda e, ps=ps, k=k, tt=tt, cc=cc, cw=cw, c0=c0: e.matmul(ps[:, 0:cw], lhsT=hT[:, k, tt * 128:(tt + 1) * 128], rhs=Wb[:, k, c0 + cc:c0 + cc + cw], start=(k == 0), stop=(k == 7)),
                             r=[Wb, hT_all[tt]], w=[ps])
                    o = ob[it % 3]
                    S.op("act", lambda e, ps=ps, o=o, cw=cw: e.activation(out=o[:, 0:cw], in_=ps[:, 0:cw], func=AF.Copy), r=[ps], w=[o])
                    S.dma("sp", tm_ap[tt * 128:(tt + 1) * 128, tmoff + cc:tmoff + cc + cw], o[:, 0:cw], r=[o], is_output=True)
            tmoff += n


def colspec_l0():
    cs = []
    for i in range(4):
        cs.append(dict(kind="fm", col=128 * i, rope=True))
    cs.append(dict(kind="fm", col=512))
    cs.append(dict(kind="fm", col=640))
    cs.append(dict(kind="fm", col=768, rope=True))
    cs.append(dict(kind="fm", col=1024, rope=True))
    for i in range(4):
        cs.append(dict(kind="fm", col=1304 + 128 * i, scale=0.125))
    for i in range(4):
        cs.append(dict(kind="fm", col=1816 + 128 * i))
    cs.append(dict(kind="tm", col=896, n=128))
    cs.append(dict(kind="tm", col=1152, n=128))
    cs.append(dict(kind="tm", col=1280, n=24))
    cs.append(dict(kind="tm", col=2328, n=512))
    return cs, 16, 792


def colspec_l1():
    cs = []
    for i in range(8):
        cs.append(dict(kind="fm", col=128 * i, rope=True))
    for i in range(8):
        cs.append(dict(kind="fm", col=1024 + 128 * i, rope=True))
    cs.append(dict(kind="tm", col=2048, n=1024))
    return cs, 16, 1024


def load_csts(S, nc, names):
    hc = host_consts()
    out = {}
    for n in names:
        arr = hc[n]
        dt = BF16 if arr.dtype == NPBF else F32
        ap = din(nc, "c_" + n, arr.shape, dt)
        if arr.ndim == 3:
            t = S.sb([arr.shape[1], arr.shape[0], arr.shape[2]], dt, n)
            S.dma("sp", t[:], ap.rearrange("d p n -> p d n"), w=[t])
        else:
            t = S.sb(list(arr.shape), dt, n)
            S.dma("sp", t[:], ap, w=[t])
        out[n] = t
    return out, {"c_" + n: hc[n] for n in names}


def build_pre(T_loc, layer):
    cs, nfm, ntm = colspec_l0() if layer == 0 else colspec_l1()
    NC = 2840 if layer == 0 else 3072
    nc = new_nc()
    x = din(nc, "x", [T_loc, D], F32)
    pos = din(nc, "pos", [1, T_loc], I32)
    mv = din(nc, "mv", [3, D], F32)
    w = din(nc, "w", [D, NC], F32)
    fm = dout(nc, "fm", [nfm, 128, T_loc], BF16)
    tm = dout(nc, "tm", [T_loc, ntm], BF16)
    with ExitStack() as st:
        S = Sched(nc, st)
        cst, cmap = load_csts(S, nc, ["ident", "pswap", "ropec"])
        emit_pre(S, T_loc, cs, NC, x, pos, mv, w, fm, tm, cst)
        S.finish(); S.emit()
    return nc, cmap


def emit_post(S, T_loc, oT_ap, x_ap, wo_ap, mvec_ap, mvf_ap, wg_ap, wu_ap, wd_ap, cw_ap, cb_ap, flag_ap, xo_ap, cst, final_g_ap=None):
    TE = T_loc + 2
    NT = T_loc // 128
    ident = cst["ident"]
    tiles = [(0, 2)] + [(2 + 128 * i, 128) for i in range(NT)]
    a_f, sh_f = mod_vectors(S, mvf_ap)
    gm_b = S.sb([128, D], F32, "gm_b"); gf_b = S.sb([128, D], F32, "gf_b")
    S.dma("sp", gm_b[:], mvec_ap[0:1, :].partition_broadcast(128), w=[gm_b])
    S.dma("sp", gf_b[:], mvec_ap[1:2, :].partition_broadcast(128), w=[gf_b])
    if final_g_ap is not None:
        nf_b = S.sb([128, D], F32, "nf_b")
        S.dma("sp", nf_b[:], final_g_ap.partition_broadcast(128), w=[nf_b])
    flag = S.sb([128, 1], F32, "flag")
    S.dma("sp", flag[:], flag_ap.partition_broadcast(128), w=[flag])
    cw = S.sb([128, 3, NFC], F32, "cw"); cb = S.sb([128, NFC], F32, "cb")
    S.dma("sp", cw[:], cw_ap.rearrange("r (c p) -> p r c", p=128), w=[cw], allow_slow_non_contiguous=True)
    S.dma("sp", cb[:], cb_ap.rearrange("r (c p) -> p (r c)", p=128), w=[cb], allow_slow_non_contiguous=True)
    hT = S.sb([128, 8, TE], BF16, "hT")
    psA = [S.ps([128, 512], F32, "psA") for _ in range(2)]
    psB = [S.ps([128, 512], F32, "psB") for _ in range(2)]
    with ExitStack() as st:
        old = S.stack; S.stack = st
        oT = S.sb([128, 8, TE], BF16, "oT")
        S.dma("sp", oT[:], oT_ap.rearrange("(k p) t -> p k t", p=128), w=[oT])
        Wo = S.sb([128, 8, D], BF16, "Wo")
        load_weight_bf16(S, Wo, wo_ap, 8, D)
        scr = norm_scratch(S)
        xt = [S.sb([128, D], F32, "xt") for _ in range(2)]
        tmp = [S.sb([128, D], F32, "tmp") for _ in range(2)]
        for ti, (r0, n) in enumerate(tiles):
            x_t = xt[ti % 2]; t_t = tmp[ti % 2]
            S.dma("sp", x_t[0:n, :], x_ap[r0:r0 + n, :], w=[x_t])
            for half in range(2):
                ps = psA[half]
                for k in range(8):
                    S.op("pe", lambda e, ps=ps, k=k, r0=r0, n=n, half=half: e.matmul(ps[0:n, :], lhsT=oT[:, k, r0:r0 + n], rhs=Wo[:, k, half * 512:(half + 1) * 512], start=(k == 0), stop=(k == 7)), r=[oT, Wo], w=[ps])
                S.op("dve", lambda e, ps=ps, n=n, half=half, t_t=t_t: e.tensor_mul(out=t_t[0:n, half * 512:(half + 1) * 512], in0=ps[0:n, :], in1=gm_b[0:n, half * 512:(half + 1) * 512]), r=[ps, gm_b], w=[t_t])
            S.op("pool", lambda e, n=n, x_t=x_t, t_t=t_t: e.tensor_add(out=x_t[0:n, :], in0=x_t[0:n, :], in1=t_t[0:n, :]), r=[t_t, x_t], w=[x_t])
            if ti > 0:
                S.dma("sp", xo_ap[r0 - 2:r0 - 2 + n, :], x_t[0:n, :], r=[x_t], w=[S_xo(S)], is_output=True)
            norm_to_hT(S, x_t, n, r0, hT, a_f, sh_f, ident, scr[ti % 2])
        S.barrier()
        S.stack = old
    hT_all = [hT.sub(r0) for (r0, n) in tiles]
    actT = S.sb([128, NFC, T_loc], BF16, "actT")
    Wd = S.sb([128, NFC, D], BF16, "Wd")
    with ExitStack() as st:
        old = S.stack; S.stack = st
        wst = [S.sb([128, 8, 256], F32, "wst") for _ in range(1)]
        Wgu = [S.sb([128, 8, 256], BF16, "Wgu") for _ in range(2)]
        gx = [S.sb([128, 514], F32, "gx") for _ in range(2)]
        tc_ = [S.sb([128, 512], F32, "tc") for _ in range(2)]
        sg = [S.sb([128, 512], F32, "sg") for _ in range(2)]
        wdst = [S.sb([128, 512], F32, "wdst") for _ in range(1)]
        TG = min(512, T_loc)
        groups = [(0, 2)] + [(2 + TG * i, TG) for i in range(T_loc // TG)]
        it = 0
        for fc in range(NFC):
            ws = wst[0]; wb = Wgu[fc % 2]
            S.dma("sp", ws[:, :, 0:128], wg_ap[:, fc * 128:(fc + 1) * 128].rearrange("(k p) c -> p k c", p=128), w=[ws])
            S.dma("sp", ws[:, :, 128:256], wu_ap[:, fc * 128:(fc + 1) * 128].rearrange("(k p) c -> p k c", p=128), w=[ws])
            S.op("pool", lambda e, ws=ws, wb=wb: e.tensor_copy(out=wb[:], in_=ws[:]), r=[ws], w=[wb])
            wd_s = wdst[0]
            for hh in range(2):
                S.dma("sp", wd_s[:], wd_ap[fc * 128:(fc + 1) * 128, hh * 512:(hh + 1) * 512], w=[wd_s])
                S.op("pool", lambda e, wd_s=wd_s, fc=fc, hh=hh: e.tensor_copy(out=Wd[:, fc, hh * 512:(hh + 1) * 512], in_=wd_s[:]), r=[wd_s], w=[Wd.sub((fc, hh))])
            for gi, (r0, n) in enumerate(groups):
                it += 1
                pg = psA[it % 2]; pu = psB[it % 2]
                g = gx[it % 2]; gprev = gx[(it - 1) % 2]
                hdeps = [hT_all[i] for i, (tr0, tn) in enumerate(tiles) if tr0 >= r0 and tr0 < r0 + n]
                for k in range(8):
                    S.op("pe", lambda e, pg=pg, k=k, r0=r0, n=n, wb=wb: e.matmul(pg[:, 0:n], lhsT=wb[:, k, 0:128], rhs=hT[:, k, r0:r0 + n], start=(k == 0), stop=(k == 7)), r=[wb] + hdeps, w=[pg])
                if gi == 0:
                    gnext = gx[(it + 1) % 2]
                    S.op("dve", lambda e, pg=pg, gnext=gnext: e.tensor_scalar(out=gnext[:, 0:2], in0=pg[:, 0:2], scalar1=flag[:, 0:1], scalar2=None, op0=ALU.mult, op1=ALU.bypass), r=[pg, flag], w=[gnext])
                    continue
                for k in range(8):
                    S.op("pe", lambda e, pu=pu, k=k, r0=r0, n=n, wb=wb: e.matmul(pu[:, 0:n], lhsT=wb[:, k, 128:256], rhs=hT[:, k, r0:r0 + n], start=(k == 0), stop=(k == 7)), r=[wb] + hdeps, w=[pu])
                S.op("act", lambda e, pg=pg, g=g, n=n: e.activation(out=g[:, 2:2 + n], in_=pg[:, 0:n], func=AF.Copy), r=[pg], w=[g])
                if gi < len(groups) - 1:
                    gnext = gx[(it + 1) % 2]
                    S.op("pool", lambda e, g=g, gnext=gnext, n=n: e.tensor_copy(out=gnext[:, 0:2], in_=g[:, n:n + 2]), r=[g], w=[gnext])
                t = tc_[it % 2]; s = sg[it % 2]
                S.op("dve", lambda e, g=g, t=t, n=n, fc=fc: e.tensor_scalar(out=t[:, 0:n], in0=g[:, 2:2 + n], scalar1=cw[:, 2, fc:fc + 1], scalar2=cb[:, fc:fc + 1], op0=ALU.mult, op1=ALU.add), r=[g, cw, cb], w=[t])
                S.op("dve", lambda e, g=g, t=t, n=n, fc=fc: e.scalar_tensor_tensor(out=t[:, 0:n], in0=g[:, 1:1 + n], scalar=cw[:, 1, fc:fc + 1], in1=t[:, 0:n], op0=ALU.mult, op1=ALU.add), r=[g, cw, t], w=[t])
                S.op("dve", lambda e, g=g, t=t, n=n, fc=fc: e.scalar_tensor_tensor(out=t[:, 0:n], in0=g[:, 0:n], scalar=cw[:, 0, fc:fc + 1], in1=t[:, 0:n], op0=ALU.mult, op1=ALU.add), r=[g, cw, t], w=[t])
                S.op("act", lambda e, t=t, s=s, n=n: e.activation(out=s[:, 0:n], in_=t[:, 0:n], func=AF.Silu), r=[t], w=[s])
                S.op("dve", lambda e, s=s, pu=pu, n=n, fc=fc, r0=r0: e.tensor_mul(out=actT[:, fc, r0 - 2:r0 - 2 + n], in0=pu[:, 0:n], in1=s[:, 0:n]), r=[pu, s], w=[actT.sub((fc, r0))])
        S.barrier()
        S.stack = old
    xm = [S.sb([128, D], F32, "xm") for _ in range(2)]
    tmp = [S.sb([128, D], F32, "tmp2") for _ in range(2)]
    junk = S.sb([128, D], BF16, "junk2"); ssq = S.sb([128, 1], F32, "ssq2"); rstd = S.sb([128, 1], F32, "rstd2")
    for tt in range(NT):
        x_t = xm[tt % 2]; t_t = tmp[tt % 2]
        S.dma("sp", x_t[:], xo_ap[tt * 128:(tt + 1) * 128, :], r=[S_xo(S)], w=[x_t])
        for half in range(2):
            ps = psA[half]
            for fc in range(NFC):
                S.op("pe", lambda e, ps=ps, fc=fc, tt=tt, half=half: e.matmul(ps[:], lhsT=actT[:, fc, tt * 128:(tt + 1) * 128], rhs=Wd[:, fc, half * 512:(half + 1) * 512], start=(fc == 0), stop=(fc == NFC - 1)), r=[actT, Wd] + [Wd.sub((fc, half))], w=[ps])
            S.op("dve", lambda e, ps=ps, half=half, t_t=t_t: e.tensor_mul(out=t_t[:, half * 512:(half + 1) * 512], in0=ps[:], in1=gf_b[:, half * 512:(half + 1) * 512]), r=[ps, gf_b], w=[t_t])
        S.op("pool", lambda e, x_t=x_t, t_t=t_t: e.tensor_add(out=t_t[:], in0=x_t[:], in1=t_t[:]), r=[t_t, x_t], w=[t_t])
        if final_g_ap is not None:
            S.op("act", lambda e, t_t=t_t: e.activation(out=junk[:], in_=t_t[:], func=AF.Square, accum_out=ssq[:]), r=[t_t], w=[junk, ssq])
            rstd_from_ssq(S, ssq, rstd, D)
            S.op("dve", lambda e, t_t=t_t: e.scalar_tensor_tensor(out=t_t[:], in0=t_t[:], scalar=rstd[:, 0:1], in1=nf_b[:], op0=ALU.mult, op1=ALU.mult), r=[t_t, rstd, nf_b], w=[t_t])
        S.dma("sp", xo_ap[tt * 128:(tt + 1) * 128, :], t_t[:], r=[t_t], w=[S_xo(S)], is_output=True)


def S_xo(S):
    if not hasattr(S, "_xo"):
        S._xo = Res("xo_dram")
    return S._xo


def build_post(T_loc, final):
    nc = new_nc()
    oT = din(nc, "oT", [D, T_loc + 2], BF16); x = din(nc, "x", [T_loc + 2, D], F32)
    wo = din(nc, "wo", [D, D], F32); mvec = din(nc, "mvec", [2, D], F32); mvf = din(nc, "mvf", [3, D], F32)
    wg = din(nc, "wg", [D, DFF], F32); wu = din(nc, "wu", [D, DFF], F32); wd = din(nc, "wd", [DFF, D], F32)
    cw = din(nc, "cw", [3, DFF], F32); cb = din(nc, "cb", [1, DFF], F32); flag = din(nc, "flag", [1, 1], F32)
    fg = din(nc, "fg", [1, D], F32) if final else None
    xo = dout(nc, "xo", [T_loc, D], F32)
    with ExitStack() as st:
        S = Sched(nc, st)
        cst, cmap = load_csts(S, nc, ["ident"])
        emit_post(S, T_loc, oT, x, wo, mvec, mvf, wg, wu, wd, cw, cb, flag, xo, cst, final_g_ap=fg)
        S.finish(); S.emit()
    return nc, cmap


def emit_diff(S, T, qT_ap, kT_ap, v_ap, lam_ap, subln_ap, oT_ap, cst):
    NKB = T // 128
    NQT = T // 512
    ident, mle = cst["ident"], cst["mle"]
    qT = [S.sb([128, T], BF16, "qT") for _ in range(4)]
    kT = [S.sb([128, T], BF16, "kT") for _ in range(4)]
    for i in range(4):
        S.op("pool", lambda e, i=i: e.memset(qT[i][64:128, :], 0.0), w=[qT[i]])
        S.op("pool", lambda e, i=i: e.memset(kT[i][64:128, :], 0.0), w=[kT[i]])
        S.dma("sp", qT[i][0:64, :], qT_ap[i], w=[qT[i]])
        S.dma("sp", kT[i][0:64, :], kT_ap[i], w=[kT[i]])
    Va = S.sb([128, NKB, 2, 129], BF16, "Va")
    S.op("pool", lambda e: e.memset(Va[:], 1.0), w=[Va])
    for h in range(2):
        S.dma("sp", Va[:, :, h, 0:128], v_ap[:, h * 128:(h + 1) * 128].rearrange("(kb p) d -> p kb d", p=128), w=[Va])
    lv = S.sb([128, 4, 64], F32, "lv")
    S.dma("sp", lv[:], lam_ap.rearrange("a d -> (a d)").partition_broadcast(128).rearrange("p (a d) -> p a d", a=4), w=[lv])
    pr = S.sb([128, 2, 64], F32, "pr")
    S.op("dve", lambda e: e.tensor_mul(out=pr[:, 0, :], in0=lv[:, 0, :], in1=lv[:, 1, :]), r=[lv], w=[pr])
    S.op("dve", lambda e: e.tensor_mul(out=pr[:, 1, :], in0=lv[:, 2, :], in1=lv[:, 3, :]), r=[lv, pr], w=[pr])
    sm = S.sb([128, 2], F32, "sm")
    S.op("dve", lambda e: e.tensor_reduce(out=sm[:], in_=pr[:], axis=AX.X, op=ALU.add), r=[pr], w=[sm])
    S.op("act", lambda e: e.activation(out=sm[:], in_=sm[:], func=AF.Exp), r=[sm], w=[sm])
    nlam = S.sb([128, 1], F32, "nlam")
    S.op("dve", lambda e: e.tensor_sub(out=nlam[:], in0=sm[:, 1:2], in1=sm[:, 0:1]), r=[sm], w=[nlam])
    S.op("dve", lambda e: e.tensor_single_scalar(out=nlam[:], in_=nlam[:], scalar=-LAMBDA_INIT, op=ALU.add), r=[nlam], w=[nlam])
    gsub = S.sb([128, 128], F32, "gsub")
    S.dma("sp", gsub[:], subln_ap.partition_broadcast(128), w=[gsub])
    S.op("dve", lambda e: e.tensor_single_scalar(out=gsub[:], in_=gsub[:], scalar=1.0 - LAMBDA_INIT, op=ALU.mult), r=[gsub], w=[gsub])

    ps_s = [S.ps([128, 512], F32, "ps_s") for _ in range(2)]
    ps_o = [S.ps([128, 2, 129], F32, "ps_o") for _ in range(4)]
    ps_t = S.ps([128, 512], BF16, "ps_t")
    pT = [S.sb([128, 512], BF16, "pT") for _ in range(3)]
    osb = [S.sb([128, 4, 128], F32, "osb") for _ in range(2)]
    rl = S.sb([128, 8], F32, "rl")
    od = S.sb([128, 4, 128], F32, "od")
    junk = S.sb([128, 128], BF16, "junk")
    ssq = S.sb([128, 4], F32, "ssq")
    rstd = S.sb([128, 4], F32, "rstd")
    onb = S.sb([128, 4, 128], BF16, "onb")
    ost = [S.sb([128, 512], BF16, "ost") for _ in range(2)]
    itc = [0]
    for h in range(2):
        for qt in range(NQT):
            qsl = slice(qt * 512, (qt + 1) * 512)
            for j in range(2):
                q_t, k_t = qT[h * 2 + j], kT[h * 2 + j]
                nkb = 4 * qt + 4
                started = [False, False]
                def stage1(kb, j=j, q_t=q_t, k_t=k_t, qt=qt, qsl=qsl):
                    d = kb - 4 * qt
                    itc[0] += 1
                    ps = ps_s[itc[0] % 2]
                    p_t = pT[itc[0] % 3]
                    S.op("pe", lambda e: e.matmul(ps[:], lhsT=k_t[:, kb * 128:(kb + 1) * 128], rhs=q_t[:, qsl], start=True, stop=(d < 0)), r=[k_t, q_t], w=[ps])
                    if d >= 0:
                        S.op("pe", lambda e: e.matmul(ps[:], lhsT=ident[:], rhs=mle[:, d, :], start=False, stop=True), r=[ident, mle], w=[ps])
                    S.op("act", lambda e: e.activation(out=p_t[:], in_=ps[:], func=AF.Exp, scale=0.125), r=[ps], w=[p_t])
                    return p_t

                def stage2(kb, p_t, j=j, qt=qt, h=h, started=started):
                    d = kb - 4 * qt
                    for sub in range(max(d, 0), 4):
                        bank = ps_o[j * 2 + sub // 2]
                        st = not started[sub // 2]
                        started[sub // 2] = True
                        S.op("pe", lambda e, bank=bank, sub=sub, st=st: e.matmul(bank[:, sub % 2, :], lhsT=p_t[:, sub * 128:(sub + 1) * 128], rhs=Va[:, kb, h, :], start=st, stop=(kb == 4 * qt + sub), skip_group_check=True), r=[p_t, Va], w=[bank])

                nxt = stage1(0)
                for kb in range(nkb):
                    cur = nxt
                    if kb + 1 < nkb:
                        nxt = stage1(kb + 1)
                    stage2(kb, cur)
                for sub in range(4):
                    bank = ps_o[j * 2 + sub // 2]
                    c = j * 4 + sub
                    S.op("dve", lambda e, bank=bank, sub=sub, c=c: e.reciprocal(out=rl[:, c:c + 1], in_=bank[:, sub % 2, 128:129]), r=[bank], w=[rl])
                    S.op("dve", lambda e, bank=bank, sub=sub, c=c, j=j: e.tensor_scalar(out=osb[j][:, sub, :], in0=bank[:, sub % 2, 0:128], scalar1=rl[:, c:c + 1], scalar2=None, op0=ALU.mult, op1=ALU.bypass), r=[bank, rl], w=[osb[j]])
            S.op("dve", lambda e: e.scalar_tensor_tensor(out=od[:], in0=osb[1][:], scalar=nlam[:, 0:1], in1=osb[0][:], op0=ALU.mult, op1=ALU.add), r=[osb[0], osb[1], nlam], w=[od])
            for sub in range(4):
                S.op("act", lambda e, sub=sub: e.activation(out=junk[:], in_=od[:, sub, :], func=AF.Square, accum_out=ssq[:, sub:sub + 1]), r=[od], w=[junk, ssq])
            rstd_from_ssq(S, ssq, rstd, 128)
            for sub in range(4):
                S.op("dve", lambda e, sub=sub: e.scalar_tensor_tensor(out=onb[:, sub, :], in0=od[:, sub, :], scalar=rstd[:, sub:sub + 1], in1=gsub[:], op0=ALU.mult, op1=ALU.mult), r=[od, rstd, gsub], w=[onb])
            for sub in range(4):
                S.op("pe", lambda e, sub=sub: e.transpose(out=ps_t[:, sub * 128:(sub + 1) * 128], in_=onb[:, sub, :], identity=ident[:]), r=[onb, ident], w=[ps_t])
            o_s = ost[(h * NQT + qt) % 2]
            S.op("act", lambda e, o_s=o_s: e.activation(out=o_s[:], in_=ps_t[:], func=AF.Copy), r=[ps_t], w=[o_s])
            S.dma("sp", oT_ap[h * 128:(h + 1) * 128, qsl], o_s[:], r=[o_s], is_output=True)


def build_diff(T):
    nc = new_nc()
    qT = din(nc, "qT", [4, 64, T], BF16); kT = din(nc, "kT", [4, 64, T], BF16)
    v = din(nc, "v", [T, 256], BF16); lam = din(nc, "lam", [4, 64], F32); subln = din(nc, "subln", [1, 128], F32)
    oT = dout(nc, "oT", [256, T], BF16)
    with ExitStack() as st:
        S = Sched(nc, st)
        cst, cmap = load_csts(S, nc, ["ident", "mle"])
        emit_diff(S, T, qT, kT, v, lam, subln, oT, cst)
        S.finish(); S.emit()
    return nc, cmap


def emit_sb(S, T, qT_ap, kT_ap, v_ap, oT_ap, cst, banks=None):
    NKB = T // 128
    NQT = T // 512
    ident, mlt, negtri, negones = cst["ident"], cst["mlt"], cst["negtri"], cst["negones"]
    qT = [S.sb([128, T], BF16, "sqT") for _ in range(2)]
    kT = [S.sb([128, T], BF16, "skT") for _ in range(2)]
    for i in range(2):
        S.op("pool", lambda e, i=i: e.memset(qT[i][64:128, :], 0.0), w=[qT[i]])
        S.op("pool", lambda e, i=i: e.memset(kT[i][64:128, :], 0.0), w=[kT[i]])
        S.dma("sp", qT[i][0:64, :], qT_ap[i], w=[qT[i]])
        S.dma("sp", kT[i][0:64, :], kT_ap[i], w=[kT[i]])
    V = S.sb([128, NKB, 128], BF16, "sV")
    S.dma("sp", V[:], v_ap.rearrange("(kb p) d -> p kb d", p=128), w=[V])
    ps_z = [S.ps([128, 512], F32, "ps_z") for _ in range(2)]
    ps_c = [S.ps([128, 512], F32, "ps_c") for _ in range(2)]
    ps_o = [S.ps([128, 4, 64], F32, "ps_o") for _ in range(2)]
    ps_t = S.ps([128, 512], BF16, "ps_t")
    E = [S.sb([128, 512], F32, "E") for _ in range(2)]
    sp = [S.sb([128, 512], BF16, "sp") for _ in range(2)]
    Racc = [S.sb([128, 512], BF16, "Racc") for _ in range(2)]
    aT = [S.sb([128, 512], BF16, "aT") for _ in range(3)]
    ob = S.sb([128, 4, 64], BF16, "ob")
    ost = [S.sb([64, 512], BF16, "ost") for _ in range(2)]
    g = [0, 0]
    for j in range(2):
        for qt in range(NQT):
            qsl = slice(qt * 512, (qt + 1) * 512)
            po = ps_o[(j * NQT + qt) % 2]
            kbs = list(range(4 * qt + 3, -1, -1))
            N = len(kbs)
            st_o = [True]
            ri = [0]
            sps = {}; ats = {}

            def stA(n, j=j, qt=qt, qsl=qsl):
                kb = kbs[n]; d = kb - 4 * qt
                g[0] += 1
                pz = ps_z[g[0] % 2]; e_t = E[g[0] % 2]; s_t = sp[g[0] % 2]
                S.op("pe", lambda e: e.matmul(pz[:], lhsT=kT[j][:, kb * 128:(kb + 1) * 128], rhs=qT[j][:, qsl], start=True, stop=(d < 0)), r=[kT[j], qT[j]], w=[pz])
                if d >= 0:
                    S.op("pe", lambda e: e.matmul(pz[:], lhsT=ident[:], rhs=mlt[:, d, :], start=False, stop=True), r=[ident, mlt], w=[pz])
                S.op("act", lambda e: e.activation(out=e_t[:], in_=pz[:], func=AF.Exp), r=[pz], w=[e_t])
                S.op("act", lambda e: e.activation(out=s_t[:], in_=e_t[:], func=AF.Ln, bias=1.0), r=[e_t], w=[s_t])
                sps[n] = s_t

            def stB(n, j=j, qt=qt, qsl=qsl):
                kb = kbs[n]; d = kb - 4 * qt
                first = (n == 0)
                s_t = sps.pop(n)
                g[1] += 1
                pc = ps_c[g[1] % 2]; a_t = aT[g[1] % 3]
                S.op("pe", lambda e: e.matmul(pc[:], lhsT=kT[j][:, kb * 128:(kb + 1) * 128], rhs=qT[j][:, qsl], start=True, stop=False), r=[kT[j], qT[j]], w=[pc])
                if d >= 0:
                    S.op("pe", lambda e: e.matmul(pc[:], lhsT=ident[:], rhs=mlt[:, d, :], start=False, stop=False), r=[ident, mlt], w=[pc])
                S.op("pe", lambda e: e.matmul(pc[:], lhsT=negtri[:], rhs=s_t[:], start=False, stop=first), r=[negtri, s_t], w=[pc])
                if not first:
                    rc = Racc[ri[0] % 2]
                    S.op("pe", lambda e: e.matmul(pc[:], lhsT=negones[:], rhs=rc[:], start=False, stop=True), r=[negones, rc], w=[pc])
                    if kb > 0:
                        rn = Racc[(ri[0] + 1) % 2]
                        S.op("pool", lambda e: e.tensor_add(out=rn[:], in0=rc[:], in1=s_t[:]), r=[rc, s_t], w=[rn])
                        ri[0] += 1
                else:
                    rn = Racc[ri[0] % 2]
                    S.op("pool", lambda e: e.tensor_copy(out=rn[:], in_=s_t[:]), r=[s_t], w=[rn])
                S.op("act", lambda e: e.activation(out=a_t[:], in_=pc[:], func=AF.Exp), r=[pc], w=[a_t])
                ats[n] = a_t

            def stC(n, j=j, qt=qt, po=po):
                kb = kbs[n]; d = kb - 4 * qt
                a_t = ats.pop(n)
                for sub in range(max(d, 0), 4):
                    st = st_o[0]; st_o[0] = False
                    S.op("pe", lambda e, sub=sub, st=st: e.matmul(po[:, sub, :], lhsT=a_t[:, sub * 128:(sub + 1) * 128], rhs=V[:, kb, j * 64:(j + 1) * 64], start=st, stop=(kb == 0), skip_group_check=True), r=[a_t, V], w=[po])

            stA(0)
            if N > 1:
                stA(1)
            for n in range(N):
                stB(n)
                if n + 2 < N:
                    stA(n + 2)
                if n >= 1:
                    stC(n - 1)
            stC(N - 1)
            S.op("act", lambda e, po=po: e.activation(out=ob[:], in_=po[:], func=AF.Copy), r=[po], w=[ob])
            for sub in range(4):
                S.op("pe", lambda e, sub=sub: e.transpose(out=ps_t[0:64, sub * 128:(sub + 1) * 128], in_=ob[:, sub, :], identity=ident[:]), r=[ob, ident], w=[ps_t])
            o_s = ost[(j * NQT + qt) % 2]
            S.op("dve", lambda e, o_s=o_s: e.tensor_copy(out=o_s[:], in_=ps_t[0:64, :]), r=[ps_t], w=[o_s])
            S.dma("sp", oT_ap[j * 64:(j + 1) * 64, qsl], o_s[:], r=[o_s], is_output=True)


def build_sb(T):
    nc = new_nc()
    qT = din(nc, "qT", [2, 64, T], BF16); kT = din(nc, "kT", [2, 64, T], BF16)
    v = din(nc, "v", [T, 128], BF16)
    oT = dout(nc, "oT", [128, T], BF16)
    with ExitStack() as st:
        S = Sched(nc, st)
        cst, cmap = load_csts(S, nc, ["ident", "mlt", "negtri", "negones"])
        emit_sb(S, T, qT, kT, v, oT, cst)
        S.finish(); S.emit()
    return nc, cmap


def nsa_consts(T):
    c = {}
    p = np.arange(128)[:, None]
    cc = np.arange(512)[None, :]
    c["mbase"] = (16.0 * cc + 31.0 - p).astype(np.float32)
    n = np.arange(128)[None, :]
    c["dmat"] = (n - (p >= 64)).astype(np.float32)
    c["col0"] = np.broadcast_to((n == 0), (128, 128)).astype(np.float32)
    key = np.arange(T)[None, :]
    c["eall"] = ((key // 64) == p).astype(np.float32).astype(NPBF)
    return c


def emit_nsa(S, T, A, cst):
    NKB = T // 128
    NQT = T // 512
    NCc = (T - 32) // 16 + 1
    NCB = (NCc + 127) // 128
    ident, pswap, ropec, mle, mwin = cst["ident"], cst["pswap"], cst["ropec"], cst["mle"], cst["mwin"]
    mbase, dmat, col0, eall = cst["mbase"], cst["dmat"], cst["col0"], cst["eall"]
    ps_s = [S.ps([128, 512], F32, "ps_s") for _ in range(2)]
    ps_os = S.ps([128, 4, 65], F32, "ps_os")
    ps_ow = S.ps([128, 4, 65], F32, "ps_ow")
    ps_oc = S.ps([128, 2, 65], F32, "ps_oc")
    ps_t = S.ps([128, 512], BF16, "ps_t")
    ps_m = S.ps([128, 512], F32, "ps_m")
    kcmpT = S.sb([128, 512], BF16, "kcmpT")
    vcmp = S.sb([128, 4, 65], BF16, "vcmp")
    S.op("pool", lambda e: e.memset(kcmpT[:], 0.0), w=[kcmpT])
    S.op("pool", lambda e: e.memset(vcmp[:], 1.0), w=[vcmp])
    with ExitStack() as st:
        old = S.stack; S.stack = st
        Cc, Sc = rope_tables(S, A["posc"], 512, ropec, "rc")
        def _cmp(which):
            xT = S.sb([64, T], BF16, "cxT")
            S.dma("sp", xT[:], A["kcT"] if which == 0 else A["vcT"], w=[xT])
            W1 = S.sb([64, 32, 256], BF16, "W1")
            w1st = [S.sb([64, 4, 256], F32, "w1st") for _ in range(2)]
            w1v = (A["w1k"] if which == 0 else A["w1v"]).rearrange("(l d) h -> d l h", d=64)
            for li in range(8):
                s = w1st[li % 2]
                S.dma("sp", s[:], w1v[:, li * 4:(li + 1) * 4, :], w=[s])
                S.op("pool", lambda e, s=s, li=li: e.tensor_copy(out=W1[:, li * 4:(li + 1) * 4, :], in_=s[:]), r=[s], w=[W1])
            posf = S.sb([64, 32], F32, "posf"); posb = S.sb([64, 32], BF16, "posb")
            S.dma("sp", posf[:], (A["posk"] if which == 0 else A["posv"]).rearrange("l d -> d l"), w=[posf], allow_slow_non_contiguous=True)
            S.op("dve", lambda e: e.tensor_copy(out=posb[:], in_=posf[:]), r=[posf], w=[posb])
            w2f = S.sb([128, 2, 64], F32, "w2f"); W2 = S.sb([128, 2, 64], BF16, "W2")
            S.dma("sp", w2f[:], (A["w2k"] if which == 0 else A["w2v"]).rearrange("(c p) d -> p c d", p=128), w=[w2f])
            S.op("dve", lambda e: e.tensor_copy(out=W2[:], in_=w2f[:]), r=[w2f], w=[W2])
            hidT = S.sb([128, 2, 512], BF16, "hidT")
            S.op("pool", lambda e: e.memset(hidT[:], 0.0), w=[hidT])
            b1 = S.sb([128, 2], F32, "b1")
            for hc in range(2):
                for l in range(32):
                    S.op("pe", lambda e, l=l, hc=hc: e.matmul(ps_m[:, 0:1], lhsT=W1[:, l, hc * 128:(hc + 1) * 128], rhs=posb[:, l:l + 1], start=(l == 0), stop=(l == 31)), r=[W1, posb], w=[ps_m])
                S.op("dve", lambda e, hc=hc: e.tensor_copy(out=b1[:, hc:hc + 1], in_=ps_m[:, 0:1]), r=[ps_m], w=[b1])
                ps = ps_s[hc]
                for l in range(32):
                    S.op("pe", lambda e, l=l, hc=hc, ps=ps: e.matmul(ps[:, 0:NCc], lhsT=W1[:, l, hc * 128:(hc + 1) * 128], rhs=xT[:, l:l + 16 * (NCc - 1) + 1:16], start=(l == 0), stop=(l == 31)), r=[W1, xT], w=[ps])
                S.op("act", lambda e, hc=hc, ps=ps: e.activation(out=hidT[:, hc, 0:NCc], in_=ps[:, 0:NCc], func=AF.Silu, bias=b1[:, hc:hc + 1]), r=[ps, b1], w=[hidT])
            if "dbg_kcmp" in A:
                hf = S.sb([128, 2, 512], F32, "hf")
                S.op("dve", lambda e: e.tensor_copy(out=hf[:], in_=hidT[:]), r=[hidT], w=[hf])
                S.dma("sp", A["dbg_hid"][which], hf[:], r=[hf], is_output=True)
                S.dma("sp", A["dbg_b1"][which], b1[:], r=[b1], is_output=True)
            if which == 0:
                ktok = S.sb([128, 4, 64], BF16, "ktok")
                S.op("pool", lambda e: e.memset(ktok[:], 0.0), w=[ktok])
                for cb in range(NCB):
                    nb = min(128, NCc - cb * 128)
                    for hc in range(2):
                        S.op("pe", lambda e, hc=hc, cb=cb, nb=nb: e.matmul(ps_m[0:nb, 0:64], lhsT=hidT[:, hc, cb * 128:cb * 128 + nb], rhs=W2[:, hc, :], start=(hc == 0), stop=(hc == 1)), r=[W2, hidT], w=[ps_m])
                    S.op("act", lambda e, cb=cb, nb=nb: e.activation(out=ktok[0:nb, cb, :], in_=ps_m[0:nb, 0:64], func=AF.Copy), r=[ps_m], w=[ktok])
                for cb in range(4):
                    S.op("pe", lambda e, cb=cb: e.transpose(out=ps_t[0:64, cb * 128:(cb + 1) * 128], in_=ktok[:, cb, :], identity=ident[:]), r=[ktok, ident], w=[ps_t])
                kb_ = S.sb([64, 512], BF16, "kb_"); u1 = S.sb([64, 512], F32, "u1"); u2 = S.sb([64, 512], F32, "u2")
                S.op("act", lambda e: e.activation(out=kb_[:], in_=ps_t[0:64, :], func=AF.Copy), r=[ps_t], w=[kb_])
                S.op("pe", lambda e: e.matmul(ps_s[0][:, :], lhsT=pswap[0:64, :], rhs=kb_[:], start=True, stop=True), r=[pswap, kb_], w=[ps_s[0]])
                S.op("dve", lambda e: e.tensor_mul(out=u1[:], in0=kb_[:], in1=Cc[0:64, :]), r=[kb_, Cc], w=[u1])
                S.op("dve", lambda e: e.tensor_mul(out=u2[:], in0=ps_s[0][0:64, :], in1=Sc[0:64, :]), r=[ps_s[0], Sc], w=[u2])
                S.op("pool", lambda e: e.tensor_add(out=kcmpT[0:64, 0:NCc], in0=u1[:, 0:NCc], in1=u2[:, 0:NCc]), r=[u1, u2], w=[kcmpT])
                if "dbg_kcmp" in A:
                    S.dma("sp", A["dbg_u1"], u1[:], r=[u1], is_output=True)
                    S.dma("sp", A["dbg_u2"], u2[:], r=[u2], is_output=True)
                    S.dma("sp", A["dbg_cc"], Cc[0:64, :], r=[Cc], is_output=True)
                    S.dma("sp", A["dbg_sc"], Sc[0:64, :], r=[Sc], is_output=True)
            else:
                for cb in range(NCB):
                    nb = min(128, NCc - cb * 128)
                    for hc in range(2):
                        S.op("pe", lambda e, hc=hc, cb=cb, nb=nb: e.matmul(ps_m[0:nb, 0:64], lhsT=hidT[:, hc, cb * 128:cb * 128 + nb], rhs=W2[:, hc, :], start=(hc == 0), stop=(hc == 1)), r=[W2, hidT], w=[ps_m])
                    S.op("act", lambda e, cb=cb, nb=nb: e.activation(out=vcmp[0:nb, cb, 0:64], in_=ps_m[0:nb, 0:64], func=AF.Copy), r=[ps_m], w=[vcmp])
            S.barrier()
        for _w in (0, 1):
            _cmp(_w)
        S.stack = old
    if "dbg_kcmp" in A:
        kcf = S.sb([64, 512], F32, "kcf"); vcf = S.sb([128, 4, 65], F32, "vcf")
        S.op("dve", lambda e: e.tensor_copy(out=kcf[:], in_=kcmpT[0:64, :]), r=[kcmpT], w=[kcf])
        S.op("dve", lambda e: e.tensor_copy(out=vcf[:], in_=vcmp[:]), r=[vcmp], w=[vcf])
        S.dma("sp", A["dbg_kcmp"], kcf[:], r=[kcf], is_output=True)
        S.dma("sp", A["dbg_vcmp"], vcf[:], r=[vcf], is_output=True)
    qn = [S.sb([128, T], BF16, "qn") for _ in range(4)]
    for i in range(4):
        S.op("pool", lambda e, i=i: e.memset(qn[i][64:128, :], 0.0), w=[qn[i]])
        S.dma("sp", qn[i][0:64, :], A["qnT"][i], w=[qn[i]])
    ksT = S.sb([128, T], BF16, "ksT"); kwT = S.sb([128, T], BF16, "kwT")
    S.op("pool", lambda e: e.memset(ksT[64:128, :], 0.0), w=[ksT]); S.op("pool", lambda e: e.memset(kwT[64:128, :], 0.0), w=[kwT])
    S.dma("sp", ksT[0:64, :], A["ksT"], w=[ksT]); S.dma("sp", kwT[0:64, :], A["kwT"], w=[kwT])
    vsa = S.sb([128, NKB, 65], BF16, "vsa"); vwa = S.sb([128, NKB, 65], BF16, "vwa")
    S.op("pool", lambda e: e.memset(vsa[:], 1.0), w=[vsa]); S.op("pool", lambda e: e.memset(vwa[:], 1.0), w=[vwa])
    S.dma("sp", vsa[:, :, 0:64], A["vs"].rearrange("(kb p) d -> p kb d", p=128), w=[vsa])
    S.dma("sp", vwa[:, :, 0:64], A["vw"].rearrange("(kb p) d -> p kb d", p=128), w=[vwa])
    madd = S.sb([128, 512], F32, "madd")
    sm = [S.sb([128, 512], F32, "sm") for _ in range(2)]
    P = [S.sb([128, 512], F32, "P") for _ in range(2)]
    Pb = [S.sb([128, 512], BF16, "Pb") for _ in range(2)]
    PT = [S.sb([128, 512], BF16, "PT") for _ in range(2)]
    lc = S.sb([128, 4], F32, "lc"); rlc = S.sb([128, 4], F32, "rlc")
    Ps4 = S.sb([128, 512], F32, "Ps4")
    imp = S.sb([128, 128], F32, "imp")
    v_ = S.sb([128, 128], F32, "v_"); f_ = S.sb([128, 128], F32, "f_"); vf_ = S.sb([128, 128], F32, "vf_"); ad_ = S.sb([128, 128], F32, "ad_")
    score = S.sb([128, 128], F32, "score"); sc2 = S.sb([128, 128], F32, "sc2"); m8 = S.sb([128, 16], F32, "m8")
    selb = S.sb([128, 128], BF16, "selb")
    selT = [S.sb([128, 512], BF16, "selT") for _ in range(2)]
    ocs = [S.sb([128, 4, 2, 65], F32, "ocs") for _ in range(2)]
    pT = [S.sb([128, 512], BF16, "pT") for _ in range(3)]
    oss = S.sb([128, 4, 65], F32, "oss")
    glb = S.sb([128, 4, 6], BF16, "glb"); gg = S.sb([128, 4, 6], F32, "gg")
    ww = S.sb([128, 4, 3], F32, "ww")
    oacc = S.sb([128, 4, 64], F32, "oacc"); ob = S.sb([128, 4, 64], BF16, "ob")
    ost = [S.sb([64, 512], BF16, "ost") for _ in range(2)]
    itc = [0]
    def _qt(qt):
        qsl = slice(qt * 512, (qt + 1) * 512)
        oc_t = ocs[qt % 2]; sT = selT[qt % 2]
        def _sub(sub):
            qs = qt * 4 + sub
            q1 = slice(qs * 128, (qs + 1) * 128)
            S.op("dve", lambda e, qs=qs: e.tensor_scalar(out=madd[:], in0=mbase[:], scalar1=float(128 * qs), scalar2=NEG, op0=ALU.is_gt, op1=ALU.mult), r=[mbase], w=[madd])
            for i in range(4):
                itc[0] += 1; it = itc[0]
                ps = ps_s[it % 2]; s_ = sm[it % 2]; p_ = P[it % 2]
                S.op("pe", lambda e, ps=ps, i=i, q1=q1: e.matmul(ps[:], lhsT=qn[i][:, q1], rhs=kcmpT[:], start=True, stop=True), r=[qn[i], kcmpT], w=[ps])
                S.op("dve", lambda e, ps=ps, s_=s_: e.scalar_tensor_tensor(out=s_[:], in0=ps[:], scalar=0.125, in1=madd[:], op0=ALU.mult, op1=ALU.add), r=[ps, madd], w=[s_])
                S.op("act", lambda e, s_=s_, p_=p_, i=i: e.activation(out=p_[:], in_=s_[:], func=AF.Exp, accum_out=lc[:, i:i + 1]), r=[s_], w=[p_, lc])
                S.op("dve", lambda e, i=i: e.tensor_single_scalar(out=rlc[:, i:i + 1], in_=lc[:, i:i + 1], scalar=1e-30, op=ALU.max), r=[lc], w=[rlc])
                S.op("dve", lambda e, i=i: e.reciprocal(out=rlc[:, i:i + 1], in_=rlc[:, i:i + 1]), r=[rlc], w=[rlc])
                if i == 0:
                    S.op("dve", lambda e, p_=p_, i=i: e.tensor_scalar(out=Ps4[:], in0=p_[:], scalar1=rlc[:, i:i + 1], scalar2=None, op0=ALU.mult, op1=ALU.bypass), r=[p_, rlc], w=[Ps4])
                else:
                    S.op("dve", lambda e, p_=p_, i=i: e.scalar_tensor_tensor(out=Ps4[:], in0=p_[:], scalar=rlc[:, i:i + 1], in1=Ps4[:], op0=ALU.mult, op1=ALU.add), r=[p_, rlc, Ps4], w=[Ps4])
                if i < 2:
                    pb = Pb[i]; pt = PT[i]
                    S.op("pool", lambda e, p_=p_, pb=pb: e.tensor_copy(out=pb[:], in_=p_[:]), r=[p_], w=[pb])
                    for cb in range(NCB):
                        S.op("pe", lambda e, pb=pb, cb=cb: e.transpose(out=ps_t[:, cb * 128:(cb + 1) * 128], in_=pb[:, cb * 128:(cb + 1) * 128], identity=ident[:]), r=[pb, ident], w=[ps_t])
                    S.op("act", lambda e, pt=pt: e.activation(out=pt[:, 0:NCB * 128], in_=ps_t[:, 0:NCB * 128], func=AF.Copy), r=[ps_t], w=[pt])
                    for cb in range(NCB):
                        S.op("pe", lambda e, pt=pt, cb=cb, i=i: e.matmul(ps_oc[:, i, :], lhsT=pt[:, cb * 128:(cb + 1) * 128], rhs=vcmp[:, cb, :], start=(cb == 0), stop=(cb == NCB - 1)), r=[pt, vcmp], w=[ps_oc])
                    S.op("act", lambda e, i=i, sub=sub, oc_t=oc_t: e.activation(out=oc_t[:, sub, i, :], in_=ps_oc[:, i, :], func=AF.Copy), r=[ps_oc], w=[oc_t])
            S.op("dve", lambda e: e.tensor_reduce(out=imp[:], in_=Ps4[:].rearrange("p (n f) -> p n f", f=4), axis=AX.X, op=ALU.add), r=[Ps4], w=[imp])
            S.op("dve", lambda e: e.tensor_add(out=imp[:, 1:128], in0=imp[:, 1:128], in1=Ps4[:, 3:508:4]), r=[imp, Ps4], w=[imp])
            S.op("dve", lambda e, qs=qs: e.tensor_single_scalar(out=v_[:], in_=dmat[:], scalar=float(2 * qs), op=ALU.is_le), r=[dmat], w=[v_])
            S.op("dve", lambda e, qs=qs: e.scalar_tensor_tensor(out=f_[:], in0=dmat[:], scalar=float(2 * qs - 1), in1=v_[:], op0=ALU.is_ge, op1=ALU.mult), r=[dmat, v_], w=[f_])
            S.op("dve", lambda e: e.tensor_max(out=f_[:], in0=f_[:], in1=col0[:]), r=[f_, col0], w=[f_])
            S.op("dve", lambda e: e.tensor_sub(out=vf_[:], in0=v_[:], in1=f_[:]), r=[v_, f_], w=[vf_])
            S.op("dve", lambda e: e.scalar_tensor_tensor(out=ad_[:], in0=f_[:], scalar=-1.0, in1=v_[:], op0=ALU.add, op1=ALU.add), r=[f_, v_], w=[ad_])
            S.op("dve", lambda e: e.tensor_mul(out=score[:], in0=imp[:], in1=vf_[:]), r=[imp, vf_], w=[score])
            S.op("dve", lambda e: e.scalar_tensor_tensor(out=score[:], in0=ad_[:], scalar=1e9, in1=score[:], op0=ALU.mult, op1=ALU.add), r=[ad_, score], w=[score])
            S.op("dve", lambda e: e.max(out=m8[:, 0:8], in_=score[:]), r=[score], w=[m8])
            S.op("dve", lambda e: e.match_replace(out=sc2[:], in_to_replace=m8[:, 0:8], in_values=score[:], imm_value=-3e9), r=[score, m8], w=[sc2])
            S.op("dve", lambda e: e.max(out=m8[:, 8:16], in_=sc2[:]), r=[sc2, m8], w=[m8])
            S.op("dve", lambda e: e.tensor_scalar(out=sc2[:], in0=score[:], scalar1=m8[:, 15:16], scalar2=None, op0=ALU.is_ge, op1=ALU.bypass), r=[score, m8], w=[sc2])
            S.op("dve", lambda e: e.tensor_scalar(out=selb[:], in0=sc2[:], scalar1=-1.0, scalar2=-NEG, op0=ALU.add, op1=ALU.mult), r=[sc2], w=[selb])
            if "dbg_kcmp" in A and qs == A["dbg_qs"]:
                S.dma("sp", A["dbg_imp"], imp[:], r=[imp], is_output=True)
                S.dma("sp", A["dbg_score"], score[:], r=[score], is_output=True)
                S.dma("sp", A["dbg_sel"], sc2[:], r=[sc2], is_output=True)
                S.dma("sp", A["dbg_m8"], m8[:], r=[m8], is_output=True)
                S.dma("sp", A["dbg_ps4"], Ps4[:], r=[Ps4], is_output=True)
            S.op("pe", lambda e: e.transpose(out=ps_t[:, 0:128], in_=selb[:], identity=ident[:]), r=[selb, ident], w=[ps_t])
            S.op("act", lambda e, sub=sub, sT=sT: e.activation(out=sT[:, sub * 128:(sub + 1) * 128], in_=ps_t[:, 0:128], func=AF.Copy), r=[ps_t], w=[sT])
        for _s in range(4):
            _sub(_s)
        S.dma("sp", glb[:], A["gl"][qsl, :].rearrange("(s p) c -> p s c", p=128), w=[glb], allow_slow_non_contiguous=True)
        S.op("act", lambda e: e.activation(out=gg[:], in_=glb[:], func=AF.Exp, scale=-1.0), r=[glb], w=[gg])
        S.op("dve", lambda e: e.tensor_single_scalar(out=gg[:], in_=gg[:], scalar=1.0, op=ALU.add), r=[gg], w=[gg])
        S.op("dve", lambda e: e.reciprocal(out=gg[:], in_=gg[:]), r=[gg], w=[gg])
        def _head(i):
            fo = [True]
            def s1_sel(kb):
                d = kb - 4 * qt
                itc[0] += 1
                ps = ps_s[itc[0] % 2]; p_t = pT[itc[0] % 3]
                S.op("pe", lambda e: e.matmul(ps[:], lhsT=ksT[:, kb * 128:(kb + 1) * 128], rhs=qn[i][:, qsl], start=True, stop=False), r=[ksT, qn[i]], w=[ps])
                S.op("pe", lambda e: e.matmul(ps[:], lhsT=eall[:, kb * 128:(kb + 1) * 128], rhs=sT[:], start=False, stop=(d < 0)), r=[eall, sT], w=[ps])
                if d >= 0:
                    S.op("pe", lambda e: e.matmul(ps[:], lhsT=ident[:], rhs=mle[:, d, :], start=False, stop=True), r=[ident, mle], w=[ps])
                S.op("act", lambda e: e.activation(out=p_t[:], in_=ps[:], func=AF.Exp, scale=0.125), r=[ps], w=[p_t])
                return p_t
            def s2_sel(kb, p_t):
                d = kb - 4 * qt
                for sub in range(max(d, 0), 4):
                    st = fo[0]; fo[0] = False
                    S.op("pe", lambda e, sub=sub, st=st: e.matmul(ps_os[:, sub, :], lhsT=p_t[:, sub * 128:(sub + 1) * 128], rhs=vsa[:, kb, :], start=st, stop=(kb == 4 * qt + sub), skip_group_check=True), r=[p_t, vsa], w=[ps_os])
            kbs = list(range(4 * qt + 4))
            nxt = s1_sel(kbs[0])
            for n_, kb in enumerate(kbs):
                cur = nxt
                if n_ + 1 < len(kbs):
                    nxt = s1_sel(kbs[n_ + 1])
                s2_sel(kb, cur)
            S.op("act", lambda e: e.activation(out=oss[:], in_=ps_os[:], func=AF.Copy), r=[ps_os], w=[oss])
            fw_ = [True]
            def s1_win(kb):
                d = kb - 4 * qt
                itc[0] += 1
                ps = ps_s[itc[0] % 2]; p_t = pT[itc[0] % 3]
                S.op("pe", lambda e: e.matmul(ps[:], lhsT=kwT[:, kb * 128:(kb + 1) * 128], rhs=qn[i][:, qsl], start=True, stop=False), r=[kwT, qn[i]], w=[ps])
                mk = mle[:, d, :] if d >= 0 else mwin[:, d + 4, :]
                S.op("pe", lambda e: e.matmul(ps[:], lhsT=ident[:], rhs=mk, start=False, stop=True), r=[ident, mle, mwin], w=[ps])
                S.op("act", lambda e: e.activation(out=p_t[:], in_=ps[:], func=AF.Exp, scale=0.125), r=[ps], w=[p_t])
                return p_t
            def s2_win(kb, p_t):
                d = kb - 4 * qt
                subs = range(d, 4) if d >= 0 else range(0, d + 5)
                for sub in subs:
                    st = fw_[0]; fw_[0] = False
                    S.op("pe", lambda e, sub=sub, st=st: e.matmul(ps_ow[:, sub, :], lhsT=p_t[:, sub * 128:(sub + 1) * 128], rhs=vwa[:, kb, :], start=st, stop=(kb == 4 * qt + sub), skip_group_check=True), r=[p_t, vwa], w=[ps_ow])
            kbs = list(range(max(0, 4 * qt - 4), 4 * qt + 4))
            nxt = s1_win(kbs[0])
            for n_, kb in enumerate(kbs):
                cur = nxt
                if n_ + 1 < len(kbs):
                    nxt = s1_win(kbs[n_ + 1])
                s2_win(kb, cur)
            S.op("dve", lambda e, i=i: e.tensor_single_scalar(out=ww[:, :, 0], in_=oc_t[:, :, i, 64], scalar=1e-30, op=ALU.max), r=[oc_t], w=[ww])
            S.op("dve", lambda e: e.tensor_copy(out=ww[:, :, 1], in_=oss[:, :, 64]), r=[oss, ww], w=[ww])
            S.op("dve", lambda e: e.tensor_copy(out=ww[:, :, 2], in_=ps_ow[:, :, 64]), r=[ps_ow, ww], w=[ww])
            S.op("dve", lambda e: e.reciprocal(out=ww[:], in_=ww[:]), r=[ww], w=[ww])
            S.op("dve", lambda e, i=i: e.tensor_mul(out=ww[:], in0=ww[:], in1=gg[:, :, i * 3:(i + 1) * 3]), r=[ww, gg], w=[ww])
            if "dbg_kcmp" in A and qt == A["dbg_qs"] // 4 and i == 0:
                owf = S.sb([128, 4, 65], F32, "owf")
                S.op("dve", lambda e: e.tensor_copy(out=owf[:], in_=ps_ow[:]), r=[ps_ow], w=[owf])
                S.dma("sp", A["dbg_ow"], owf[:], r=[owf], is_output=True)
                S.dma("sp", A["dbg_os"], oss[:], r=[oss], is_output=True)
                S.dma("sp", A["dbg_oc"], oc_t[:], r=[oc_t], is_output=True)
                S.dma("sp", A["dbg_ww"], ww[:], r=[ww], is_output=True)
            for sub in range(4):
                S.op("dve", lambda e, sub=sub, i=i: e.tensor_scalar(out=oacc[:, sub, :], in0=oc_t[:, sub, i, 0:64], scalar1=ww[:, sub, 0:1], scalar2=None, op0=ALU.mult, op1=ALU.bypass), r=[oc_t, ww], w=[oacc])
                S.op("dve", lambda e, sub=sub: e.scalar_tensor_tensor(out=oacc[:, sub, :], in0=oss[:, sub, 0:64], scalar=ww[:, sub, 1:2], in1=oacc[:, sub, :], op0=ALU.mult, op1=ALU.add), r=[oss, ww, oacc], w=[oacc])
                S.op("dve", lambda e, sub=sub: e.scalar_tensor_tensor(out=ob[:, sub, :], in0=ps_ow[:, sub, 0:64], scalar=ww[:, sub, 2:3], in1=oacc[:, sub, :], op0=ALU.mult, op1=ALU.add), r=[ps_ow, ww, oacc], w=[ob])
            for sub in range(4):
                S.op("pe", lambda e, sub=sub: e.transpose(out=ps_t[0:64, sub * 128:(sub + 1) * 128], in_=ob[:, sub, :], identity=ident[:]), r=[ob, ident], w=[ps_t])
            o_s = ost[(qt * 2 + i) % 2]
            S.op("act", lambda e, o_s=o_s: e.activation(out=o_s[:], in_=ps_t[0:64, :], func=AF.Copy), r=[ps_t], w=[o_s])
            S.dma("sp", A["oT"][i * 64:(i + 1) * 64, qsl], o_s[:], r=[o_s], is_output=True)
        for _i in range(2):
            _head(_i)
    for _q in range(NQT):
        _qt(_q)


NSA_IN = dict(qnT=lambda T: ([4, 64, T], BF16), kcT=lambda T: ([64, T], BF16), vcT=lambda T: ([64, T], BF16),
              ksT=lambda T: ([64, T], BF16), kwT=lambda T: ([64, T], BF16), vs=lambda T: ([T, 64], BF16), vw=lambda T: ([T, 64], BF16),
              gl=lambda T: ([T, 6], BF16), posc=lambda T: ([1, 512], I32),
              w1k=lambda T: ([2048, 256], F32), w1v=lambda T: ([2048, 256], F32), w2k=lambda T: ([256, 64], F32), w2v=lambda T: ([256, 64], F32),
              posk=lambda T: ([32, 64], F32), posv=lambda T: ([32, 64], F32))


def load_csts_extra(S, nc, arrs):
    out = {}
    for n, arr in arrs.items():
        dt = BF16 if arr.dtype == NPBF else F32
        ap = din(nc, "c_" + n, arr.shape, dt)
        t = S.sb(list(arr.shape), dt, n)
        S.dma("sp", t[:], ap, w=[t])
        out[n] = t
    return out, {"c_" + n: a for n, a in arrs.items()}


def build_nsa(T, dbg_qs=None):
    nc = new_nc()
    A = {}
    if dbg_qs is not None:
        A["dbg_qs"] = dbg_qs
        for n, shp in dict(dbg_kcmp=[64, 512], dbg_vcmp=[128, 4, 65], dbg_imp=[128, 128], dbg_score=[128, 128], dbg_sel=[128, 128], dbg_m8=[128, 16], dbg_ps4=[128, 512],
                           dbg_hid=[2, 128, 2, 512], dbg_b1=[2, 128, 2], dbg_u1=[64, 512], dbg_u2=[64, 512], dbg_cc=[64, 512], dbg_sc=[64, 512], dbg_ow=[128, 4, 65], dbg_os=[128, 4, 65], dbg_oc=[128, 4, 2, 65], dbg_ww=[128, 4, 3]).items():
            A[n] = dout(nc, n, shp, F32)
    for n, f in NSA_IN.items():
        shp, dt = f(T)
        A[n] = din(nc, n, shp, dt)
    A["oT"] = dout(nc, "oT", [128, T], BF16)
    with ExitStack() as st:
        S = Sched(nc, st)
        cst, cmap = load_csts(S, nc, ["ident", "pswap", "ropec", "mle", "mwin"])
        c2, cmap2 = load_csts_extra(S, nc, nsa_consts(T))
        cst.update(c2); cmap.update(cmap2)
        emit_nsa(S, T, A, cst)
        S.finish(); S.emit()
    return nc, cmap


def build_mod():
    nc = new_nc()
    cT = din(nc, "cT", [128, 8, 2], F32); w = din(nc, "w", [2, D, 768], F32); bias = din(nc, "bias", [2, 768], F32)
    out = dout(nc, "modp", [2, 2, 768], F32)
    with ExitStack() as st:
        S = Sched(nc, st)
        ct = S.sb([128, 8, 2], F32, "ct"); cond = S.sb([128, 8, 2], F32, "cond")
        S.dma("sp", ct[:], cT, w=[ct])
        S.op("act", lambda e: e.activation(out=cond[:], in_=ct[:], func=AF.Silu), r=[ct], w=[cond])
        ps = [S.ps([128, 512], F32, "ps") for _ in range(2)]
        for l in range(2):
            W = S.sb([128, 8, 768], F32, "W")
            S.dma("sp", W[:], w[l].rearrange("(k p) c -> p k c", p=128), w=[W])
            bt = S.sb([2, 768], F32, "bt")
            S.dma("sp", bt[:], bias[l:l + 1, :].partition_broadcast(2), w=[bt])
            ot = S.sb([2, 768], F32, "ot")
            for half in range(2):
                p = ps[half]
                for k in range(8):
                    S.op("pe", lambda e, p=p, k=k, half=half, W=W: e.matmul(p[0:2, 0:384], lhsT=cond[:, k, :], rhs=W[:, k, half * 384:(half + 1) * 384], start=(k == 0), stop=(k == 7)), r=[cond, W], w=[p])
                S.op("dve", lambda e, p=p, half=half, ot=ot, bt=bt: e.tensor_add(out=ot[:, half * 384:(half + 1) * 384], in0=p[0:2, 0:384], in1=bt[:, half * 384:(half + 1) * 384]), r=[p, bt], w=[ot])
            S.dma("sp", out[l], ot[:], r=[ot], is_output=True)
        S.finish(); S.emit()
    return nc, {}


def _run(nc, in_maps):
    res = run_bass_kernel_spmd(nc, in_maps, core_ids=list(range(8)))
    return res.results


def kernel(x, c, positions, mod_w, mod_b, norm_mix, norm_ffn, ffn_w_gate, ffn_w_up, ffn_conv_w, ffn_conv_b, ffn_w_down,
           hyb_w_in, nsa_pos_k, nsa_pos_v, nsa_ck_w1, nsa_ck_w2, nsa_cv_w1, nsa_cv_w2, hyb_w_out, diff_w_qkv,
           diff_lq1, diff_lk1, diff_lq2, diff_lk2, diff_subln, diff_w_out, norm_f):
    f32 = lambda a: np.ascontiguousarray(np.asarray(a), dtype=np.float32)
    x = f32(x); c = f32(c); positions = np.ascontiguousarray(np.asarray(positions), dtype=np.int32)
    B, T, _ = x.shape
    TL = T // 4
    ca = np.ascontiguousarray
    nc, cm = build_mod()
    cT = ca(c.T.reshape(8, 128, 2).transpose(1, 0, 2))
    mw = f32(mod_w); mb = f32(mod_b)
    r = _run(nc, [dict(cT=cT, w=ca(mw[:, :, i * 768:(i + 1) * 768]), bias=ca(mb[:, i * 768:(i + 1) * 768])) for i in range(8)])
    mod = np.concatenate([r[i]["modp"] for i in range(8)], axis=-1)
    sh_m, sc_m, g_m, sh_f, sc_f, g_f = [mod[..., k * D:(k + 1) * D] for k in range(6)]
    nmix = f32(norm_mix); nffn = f32(norm_ffn)

    def run_pre(layer, xin, w):
        nc, cm = build_pre(TL, layer)
        maps = []
        for i in range(8):
            b, j = i // 4, i % 4
            maps.append(dict(x=ca(xin[b, j * TL:(j + 1) * TL]), pos=ca(positions[b:b + 1, j * TL:(j + 1) * TL]),
                             mv=ca(np.stack([nmix[layer], sc_m[layer, b], sh_m[layer, b]])), w=w, **cm))
        r = _run(nc, maps)
        FM = [np.concatenate([r[b * 4 + j]["fm"] for j in range(4)], axis=2) for b in range(B)]
        TM = [np.concatenate([r[b * 4 + j]["tm"] for j in range(4)], axis=0) for b in range(B)]
        return FM, TM

    def run_post(layer, OT, xin, wo, final):
        nc, cm = build_post(TL, final)
        maps = []
        for i in range(8):
            b, j = i // 4, i % 4
            oT = np.zeros((D, TL + 2), NPBF); xe = np.zeros((TL + 2, D), np.float32)
            lo = j * TL - 2
            if j > 0:
                oT[:] = OT[b][:, lo:lo + TL + 2]; xe[:] = xin[b, lo:lo + TL + 2]
            else:
                oT[:, 2:] = OT[b][:, 0:TL]; xe[2:] = xin[b, 0:TL]
            m = dict(oT=oT, x=xe, wo=wo, mvec=ca(np.stack([g_m[layer, b], g_f[layer, b]])),
                     mvf=ca(np.stack([nffn[layer], sc_f[layer, b], sh_f[layer, b]])),
                     wg=f32(ffn_w_gate[layer]), wu=f32(ffn_w_up[layer]), wd=f32(ffn_w_down[layer]),
                     cw=f32(ffn_conv_w[layer]), cb=f32(ffn_conv_b[layer])[None, :], flag=np.array([[1.0 if j > 0 else 0.0]], np.float32), **cm)
            if final:
                m["fg"] = f32(norm_f)[None, :]
            maps.append(m)
        r = _run(nc, maps)
        return np.stack([np.concatenate([r[b * 4 + j]["xo"] for j in range(4)], axis=0) for b in range(B)])

    FM, TM = run_pre(0, x, f32(hyb_w_in[0]))
    nc, cm = build_nsa(T)
    maps = []
    NCc = (T - 32) // 16 + 1
    for i in range(8):
        b, hg = i // 4, i % 4
        g = hg // 2
        own = [2 * hg, 2 * hg + 1]
        order = own + [h for h in range(4 * g, 4 * g + 4) if h not in own]
        posc = np.zeros((1, 512), np.int32); posc[0, :NCc] = positions[b, 31::16][:NCc]
        gs = slice(64 * g, 64 * g + 64)
        maps.append(dict(qnT=ca(np.stack([FM[b][h // 2, (h % 2) * 64:(h % 2) * 64 + 64] for h in order])),
                         kcT=ca(FM[b][4, gs]), vcT=ca(FM[b][5, gs]), ksT=ca(FM[b][6, gs]), kwT=ca(FM[b][7, gs]),
                         vs=ca(TM[b][:, 64 * g:64 * g + 64]), vw=ca(TM[b][:, 128 + 64 * g:128 + 64 * g + 64]),
                         gl=ca(TM[b][:, 256 + 3 * own[0]:256 + 3 * own[0] + 6]), posc=posc,
                         w1k=f32(nsa_ck_w1[0]), w1v=f32(nsa_cv_w1[0]), w2k=f32(nsa_ck_w2[0]), w2v=f32(nsa_cv_w2[0]),
                         posk=f32(nsa_pos_k[0]), posv=f32(nsa_pos_v[0]), **cm))
    r_nsa = _run(nc, maps)
    nc, cm = build_sb(T)
    maps = []
    for i in range(8):
        b, hg = i // 4, i % 4
        maps.append(dict(qT=ca(FM[b][8 + hg].reshape(2, 64, T)), kT=ca(FM[b][12 + hg].reshape(2, 64, T)),
                         v=ca(TM[b][:, 280 + 128 * hg:280 + 128 * hg + 128]), **cm))
    r_sb = _run(nc, maps)
    OT = []
    for b in range(B):
        OT.append(np.concatenate([r_nsa[b * 4 + hg]["oT"] for hg in range(4)] + [r_sb[b * 4 + hg]["oT"] for hg in range(4)], axis=0))
    x1 = run_post(0, OT, x, f32(hyb_w_out[0]), False)
    FM, TM = run_pre(1, x1, f32(diff_w_qkv[0]))
    nc, cm = build_diff(T)
    lam = ca(np.stack([f32(diff_lq1[0]), f32(diff_lk1[0]), f32(diff_lq2[0]), f32(diff_lk2[0])]))
    maps = []
    for i in range(8):
        b, hg = i // 4, i % 4
        maps.append(dict(qT=ca(FM[b][2 * hg:2 * hg + 2].reshape(4, 64, T)), kT=ca(FM[b][8 + 2 * hg:8 + 2 * hg + 2].reshape(4, 64, T)),
                         v=ca(TM[b][:, 256 * hg:256 * hg + 256]), lam=lam, subln=f32(diff_subln[0])[None, :], **cm))
    r_d = _run(nc, maps)
    OT = [np.concatenate([r_d[b * 4 + hg]["oT"] for hg in range(4)], axis=0) for b in range(B)]
    out = run_post(1, OT, x1, f32(diff_w_out[0]), True)
    return out.astype(np.float32)
```
